# Optimizing a Trainium2 kernel written in Bass

```python
import jax, jax.numpy as jnp
from jax import lax
import numpy as np

D_MODEL = 2048
BATCH = 2
SEQ = 8192
DEPTH = 2

D_MIX = D_MODEL
CONV_WIDTH = D_MIX // 2
LRU_WIDTH = D_MIX - CONV_WIDTH
LRU_HEADS = 8
LRU_HEAD_DIM = LRU_WIDTH // LRU_HEADS
D_IN = 2 * CONV_WIDTH + 2 * LRU_WIDTH
CONV_K = 31
LRU_CONV_K = 4
LRU_C = 8.0
D_FF = 3 * D_MODEL
N_EXPERTS = 8
TOP_K = 2
D_EXPERT = 3 * D_MODEL
MOE_BLOCK = 512
N_DENSE = (DEPTH + 1) // 2
N_MOE = DEPTH // 2
EPS = 1e-6

kernel_name = "hybrid_conformer_rglru_moe_trunk"


def rms_norm(x, g):
    xf = x.astype(jnp.float32)
    y = xf * lax.rsqrt(jnp.mean(xf * xf, axis=-1, keepdims=True) + EPS)
    return (y * g.astype(jnp.float32)).astype(x.dtype)


def causal_depthwise_conv(x, w, b):
    width, ch = w.shape
    y = lax.conv_general_dilated(
        x, w[:, None, :].astype(x.dtype), window_strides=(1,), padding=[(width - 1, 0)],
        dimension_numbers=("NWC", "WIO", "NWC"), feature_group_count=ch)
    return y + b.astype(x.dtype)


def conformer_conv(val, gate, conv_w, conv_b, ln_g, ln_b):
    c = val * jax.nn.sigmoid(gate)
    c = causal_depthwise_conv(c, conv_w, conv_b)
    cf = c.astype(jnp.float32)
    mu = jnp.mean(cf, axis=-1, keepdims=True)
    var = jnp.mean(jnp.square(cf - mu), axis=-1, keepdims=True)
    cn = (cf - mu) * lax.rsqrt(var + EPS) * ln_g.astype(jnp.float32) + ln_b.astype(jnp.float32)
    return jax.nn.silu(cn).astype(val.dtype)


def _linear_recurrence_combine(c1, c2):
    a1, b1 = c1
    a2, b2 = c2
    return a1 * a2, a2 * b1 + b2


def rg_lru(x, wa, ba, wx, bx, lam):
    bsz, seq, width = x.shape
    xh = x.reshape(bsz, seq, LRU_HEADS, LRU_HEAD_DIM)
    gate_a = jax.nn.sigmoid(jnp.einsum("bshi,hij->bshj", xh, wa.astype(x.dtype)).reshape(bsz, seq, width) + ba.astype(x.dtype))
    gate_x = jax.nn.sigmoid(jnp.einsum("bshi,hij->bshj", xh, wx.astype(x.dtype)).reshape(bsz, seq, width) + bx.astype(x.dtype))
    log_a = -LRU_C * gate_a.astype(jnp.float32) * jax.nn.softplus(-lam.astype(jnp.float32))
    a = jnp.exp(log_a)
    mult = jnp.sqrt(-jnp.expm1(2.0 * log_a))
    is_first = (jnp.arange(seq) == 0)[None, :, None]
    mult = jnp.where(is_first, jnp.ones_like(mult), mult)
    b = mult * (gate_x * x).astype(jnp.float32)
    _, h = lax.associative_scan(_linear_recurrence_combine, (a, b), axis=1)
    return h.astype(x.dtype)


def hybrid_mixer(h, w_in, conv_w, conv_b, conv_ln_g, conv_ln_b,
                 lru_conv_w, lru_conv_b, lru_wa, lru_ba, lru_wx, lru_bx, lru_lambda, w_out):
    u = jnp.einsum("bsd,de->bse", h, w_in.astype(h.dtype))
    splits = [CONV_WIDTH, 2 * CONV_WIDTH, 2 * CONV_WIDTH + LRU_WIDTH]
    c_val, c_gate, r_x, r_gate = jnp.split(u, splits, axis=-1)
    y_conv = conformer_conv(c_val, c_gate, conv_w, conv_b, conv_ln_g, conv_ln_b)
    r_x = causal_depthwise_conv(r_x, lru_conv_w, lru_conv_b)
    y_lru = rg_lru(r_x, lru_wa, lru_ba, lru_wx, lru_bx, lru_lambda) * jax.nn.gelu(r_gate)
    y = jnp.concatenate([y_conv, y_lru], axis=-1)
    return jnp.einsum("bse,ed->bsd", y, w_out.astype(h.dtype))


def swiglu(h, wg, wu, wd):
    g = jnp.einsum("bsd,df->bsf", h, wg.astype(h.dtype))
    v = jnp.einsum("bsd,df->bsf", h, wu.astype(h.dtype))
    return jnp.einsum("bsf,fd->bsd", jax.nn.silu(g) * v, wd.astype(h.dtype))


def moe_swiglu(h, w_router, wg, wu, wd):
    bsz, seq, d = h.shape
    n_tok = bsz * seq
    n_asg = n_tok * TOP_K
    xf = h.reshape(n_tok, d)
    logits = jnp.einsum("td,de->te", xf, w_router.astype(xf.dtype)).astype(jnp.float32)
    probs = jax.nn.softmax(logits, axis=-1)
    top_p, top_e = lax.top_k(probs, TOP_K)
    top_p = top_p / jnp.sum(top_p, axis=-1, keepdims=True)
    flat_e = top_e.reshape(-1).astype(jnp.int32)
    flat_tok = jnp.arange(n_asg, dtype=jnp.int32) // TOP_K
    flat_w = top_p.reshape(-1)
    order = jnp.argsort(flat_e)
    s_e, s_tok, s_w = flat_e[order], flat_tok[order], flat_w[order]
    counts = jnp.bincount(flat_e, length=N_EXPERTS).astype(jnp.int32)
    starts = jnp.cumsum(counts) - counts
    padded = ((counts + MOE_BLOCK - 1) // MOE_BLOCK) * MOE_BLOCK
    p_ends = jnp.cumsum(padded)
    p_starts = p_ends - padded
    pos = p_starts[s_e] + (jnp.arange(n_asg, dtype=jnp.int32) - starts[s_e])
    n_blocks = -(-n_asg // MOE_BLOCK) + N_EXPERTS
    n_rows = n_blocks * MOE_BLOCK
    x_buf = jnp.zeros((n_rows, d), xf.dtype).at[pos].set(xf[s_tok])
    block_start = jnp.arange(n_blocks, dtype=jnp.int32) * MOE_BLOCK
    block_e = jnp.minimum(jnp.searchsorted(p_ends, block_start, side="right"), N_EXPERTS - 1)

    def expert_block(args):
        xb, e = args
        g = xb @ wg[e].astype(xb.dtype)
        v = xb @ wu[e].astype(xb.dtype)
        return (jax.nn.silu(g) * v) @ wd[e].astype(xb.dtype)

    y_buf = lax.map(expert_block, (x_buf.reshape(n_blocks, MOE_BLOCK, d), block_e)).reshape(n_rows, d)
    y = jnp.zeros((n_tok, d), xf.dtype).at[s_tok].add(y_buf[pos] * s_w[:, None].astype(xf.dtype))
    return y.reshape(bsz, seq, d)


def setup_inputs(seed: int = 0) -> dict:
    key = jax.random.key(seed)
    ks = jax.random.split(key, 24)
    f32 = jnp.float32

    def nrm(k, shape, scale):
        return jax.random.normal(k, shape, f32) * scale

    lam_v = jax.random.uniform(ks[13], (DEPTH, LRU_WIDTH), f32, minval=0.9, maxval=0.999)
    lam_s = lam_v ** (1.0 / LRU_C)
    return {
        "x": nrm(ks[0], (BATCH, SEQ, D_MODEL), 1.0),
        "mix_norm": 1.0 + nrm(ks[1], (DEPTH, D_MODEL), 0.02),
        "w_in": nrm(ks[2], (DEPTH, D_MODEL, D_IN), D_MODEL ** -0.5),
        "conv_w": nrm(ks[3], (DEPTH, CONV_K, CONV_WIDTH), CONV_K ** -0.5),
        "conv_b": nrm(ks[4], (DEPTH, CONV_WIDTH), 0.02),
        "conv_ln_g": 1.0 + nrm(ks[5], (DEPTH, CONV_WIDTH), 0.02),
        "conv_ln_b": nrm(ks[6], (DEPTH, CONV_WIDTH), 0.02),
        "lru_conv_w": nrm(ks[7], (DEPTH, LRU_CONV_K, LRU_WIDTH), LRU_CONV_K ** -0.5),
        "lru_conv_b": nrm(ks[8], (DEPTH, LRU_WIDTH), 0.02),
        "lru_wa": nrm(ks[9], (DEPTH, LRU_HEADS, LRU_HEAD_DIM, LRU_HEAD_DIM), LRU_HEAD_DIM ** -0.5),
        "lru_ba": nrm(ks[10], (DEPTH, LRU_WIDTH), 0.02),
        "lru_wx": nrm(ks[11], (DEPTH, LRU_HEADS, LRU_HEAD_DIM, LRU_HEAD_DIM), LRU_HEAD_DIM ** -0.5),
        "lru_bx": nrm(ks[12], (DEPTH, LRU_WIDTH), 0.02),
        "lru_lambda": jnp.log(lam_s) - jnp.log1p(-lam_s),
        "w_out": nrm(ks[14], (DEPTH, D_MIX, D_MODEL), D_MIX ** -0.5),
        "ffn_norm": 1.0 + nrm(ks[15], (DEPTH, D_MODEL), 0.02),
        "dense_wg": nrm(ks[16], (N_DENSE, D_MODEL, D_FF), D_MODEL ** -0.5),
        "dense_wu": nrm(ks[17], (N_DENSE, D_MODEL, D_FF), D_MODEL ** -0.5),
        "dense_wd": nrm(ks[18], (N_DENSE, D_FF, D_MODEL), D_FF ** -0.5),
        "w_router": nrm(ks[19], (N_MOE, D_MODEL, N_EXPERTS), D_MODEL ** -0.5),
        "moe_wg": nrm(ks[20], (N_MOE, N_EXPERTS, D_MODEL, D_EXPERT), D_MODEL ** -0.5),
        "moe_wu": nrm(ks[21], (N_MOE, N_EXPERTS, D_MODEL, D_EXPERT), D_MODEL ** -0.5),
        "moe_wd": nrm(ks[22], (N_MOE, N_EXPERTS, D_EXPERT, D_MODEL), D_EXPERT ** -0.5),
        "final_norm": 1.0 + nrm(ks[23], (D_MODEL,), 0.02),
    }


def reference(x, mix_norm, w_in, conv_w, conv_b, conv_ln_g, conv_ln_b,
              lru_conv_w, lru_conv_b, lru_wa, lru_ba, lru_wx, lru_bx, lru_lambda,
              w_out, ffn_norm, dense_wg, dense_wu, dense_wd,
              w_router, moe_wg, moe_wu, moe_wd, final_norm):
    for layer in range(DEPTH):
        h = rms_norm(x, mix_norm[layer])
        x = x + hybrid_mixer(h, w_in[layer], conv_w[layer], conv_b[layer], conv_ln_g[layer], conv_ln_b[layer],
                             lru_conv_w[layer], lru_conv_b[layer], lru_wa[layer], lru_ba[layer],
                             lru_wx[layer], lru_bx[layer], lru_lambda[layer], w_out[layer])
        h = rms_norm(x, ffn_norm[layer])
        j = layer // 2
        if layer % 2 == 0:
            x = x + swiglu(h, dense_wg[j], dense_wu[j], dense_wd[j])
        else:
            x = x + moe_swiglu(h, w_router[j], moe_wg[j], moe_wu[j], moe_wd[j])
    return rms_norm(x, final_norm)
```

```python
import numpy as np
from contextlib import ExitStack
import ml_dtypes
import concourse.bass as bass
import concourse.mybir as mybir
from concourse.bass_utils import run_bass_kernel_spmd

F32 = mybir.dt.float32
BF16 = mybir.dt.bfloat16
AF = mybir.ActivationFunctionType
ALU = mybir.AluOpType
EPS = 1e-6
HALO = 32
CK = 31
LK = 4


def make_cfg(D=2048, T=2048, F=6144, FE=6144, NE=8, W=512):
    c = dict(D=D, KC=D // 128, CW=D // 2, NJ=D // 256, EIN=2 * D, T=T, W=W, F=F, FE=FE, NE=NE,
             TH=T // 2)
    c["PP"] = min(2, c["NJ"])
    c["XG"] = min(4, c["KC"])
    c["DGW"] = min(512, D)
    KC, NJ = c["KC"], c["NJ"]
    off = {}
    p = 0
    for name, n in (("mixn", KC), ("ffnn", KC), ("finn", KC), ("cb", NJ), ("lng", NJ), ("lnb", NJ),
                    ("lcb", NJ), ("ba", NJ), ("bx", NJ), ("lam", NJ), ("cw", NJ * CK), ("lw", NJ * LK)):
        off[name] = p
        p += n
    c["off"] = off
    c["NV"] = p
    return c


class Buf:
    _n = 0

    def __init__(self, ap, kind, name):
        self.ap, self.kind, self.name = ap, kind, name
        self.last_w = None
        self.reads = {}
        self.dcnt = 0
        Buf._n += 1
        self.id = Buf._n

    def __getitem__(self, idx):
        return V(self, self.ap[idx])

    def v(self):
        return V(self, self.ap)


class V:
    def __init__(self, buf, ap):
        self.buf, self.ap = buf, ap

    def __getitem__(self, idx):
        return V(self.buf, self.ap[idx])

    def re(self, s, **kw):
        return V(self.buf, self.ap.rearrange(s, **kw))


class Rot:
    def __init__(self, bufs):
        self.bufs, self.i = bufs, 0

    def next(self):
        b = self.bufs[self.i % len(self.bufs)]
        self.i += 1
        return b


COMPUTE = ("pe", "act", "dve", "pool")
STREAMS = ("pe", "act", "dve", "pool", "sp")


class K:
    def __init__(self, nc, st, arena_words):
        self.nc = nc
        self.arena = st.enter_context(nc.sbuf_tensor("arena", [128, arena_words], F32))[:]
        self.arena_words = arena_words
        self.ptr = 0
        self.ops = []
        self.cnt = {e: 0 for e in COMPUTE}
        self.waited = {e: {} for e in STREAMS}
        self.slot_cnt = []
        self.free_slots = []
        self.live_dbufs = []
        self.cbufs = {}
        self.banks = [Buf(st.enter_context(nc.psum_tensor(f"ps{i}", [128, 512], F32))[:], "psum", f"ps{i}")
                      for i in range(8)]
        self.rot = 0
        self.peak = 0

    def alloc(self, name, shape, dtype):
        n = 1
        for s in shape[1:]:
            n *= s
        words = (n + 1) // 2 if dtype == BF16 else n
        words = (words + 7) // 8 * 8
        off = self.ptr
        self.ptr += words
        self.peak = max(self.peak, self.ptr)
        assert self.ptr <= self.arena_words, f"SBUF arena overflow at {name}: {self.ptr} > {self.arena_words}"
        ap = self.arena[:, off:off + words]
        if dtype == BF16:
            ap = ap.bitcast(BF16)
        ap = ap[:, :n]
        if shape[0] < 128:
            ap = ap[0:shape[0]]
        if len(shape) == 3:
            ap = ap.rearrange("p (a b) -> p a b", a=shape[1])
        elif len(shape) == 4:
            ap = ap.rearrange("p (a b c) -> p a b c", a=shape[1], b=shape[2])
        return Buf(ap, "sbuf", name)

    def bank(self):
        b = self.banks[self.rot % 6]
        self.rot += 1
        return b

    def dram(self, name, shape, dtype, kind):
        return Buf(self.nc.dram_tensor(name, list(shape), dtype, kind=kind).ap(), "dram", name)

    def _collect(self, eng, reads, writes):
        need = {}

        def add(tok):
            if tok is not None:
                need[tok[0]] = max(need.get(tok[0], 0), tok[1])

        for b in reads:
            add(b.last_w)
        for b in writes:
            if b.kind != "dram":
                add(b.last_w)
            for kk, v in b.reads.items():
                add((kk, v))
        out = []
        for kk, v in need.items():
            if kk == eng and eng == "pe":
                continue
            if self.waited[eng].get(kk, 0) >= v:
                continue
            self.waited[eng][kk] = v
            out.append((kk, v))
        return out

    def op(self, eng, fn, reads, writes):
        reads = [r.buf for r in reads if isinstance(r, V)]
        writes = [w.buf for w in writes]
        waits = self._collect(eng, reads, writes)
        n = self.cnt[eng] + 1
        self.cnt[eng] = n
        self.ops.append((eng, fn, waits, (eng, 1)))
        for b in reads:
            b.reads[eng] = max(b.reads.get(eng, 0), n)
        for b in writes:
            b.last_w = (eng, n)
            b.reads = {}

    def dma(self, q, out, in_):
        waits = self._collect(q, [in_.buf], [out.buf])
        dst = out.buf
        if getattr(dst, "dslot", None) is None:
            if self.free_slots:
                dst.dslot = self.free_slots.pop()
            else:
                dst.dslot = len(self.slot_cnt)
                self.slot_cnt.append(0)
            self.live_dbufs.append(dst)
        slot = dst.dslot
        key = ("d", slot)
        self.slot_cnt[slot] += 16
        cntv = self.slot_cnt[slot]
        o_ap, i_ap = out.ap, in_.ap
        self.ops.append((q, lambda e: e.dma_start(out=o_ap, in_=i_ap), waits, (key, 16)))
        in_.buf.reads[key] = max(in_.buf.reads.get(key, 0), cntv)
        dst.last_w = (key, cntv)
        dst.reads = {}

    def collective(self, out_buf, in_buf, groups):
        waits = self._collect("pool", [in_buf], [out_buf])
        key = ("c", out_buf.id)
        self.cbufs[out_buf.id] = out_buf
        o_ap, i_ap = out_buf.ap, in_buf.ap
        self.ops.append(("pool", lambda e: e.collective_compute("AllGather", ALU.bypass, replica_groups=groups,
                                                                  ins=[i_ap], outs=[o_ap]), waits, (key, None)))
        in_buf.reads[key] = 1
        out_buf.last_w = (key, 1)
        out_buf.reads = {}

    def barrier(self):
        allw = [(e, self.cnt[e]) for e in COMPUTE if self.cnt[e] > 0]
        allw += [(("d", i), v) for i, v in enumerate(self.slot_cnt) if v > 0]
        allw += [(("c", b.id), 1) for b in self.cbufs.values()]
        for b in self.live_dbufs:
            self.free_slots.append(b.dslot)
            b.dslot = None
        self.live_dbufs = []
        for e in STREAMS:
            waits = []
            for kk, v in allw:
                if self.waited[e].get(kk, 0) < v:
                    self.waited[e][kk] = v
                    waits.append((kk, v))
            self.ops.append((e, None, waits, None))

    @staticmethod
    def _a(x):
        return x.ap if isinstance(x, V) else x

    def mm(self, out, lhsT, rhs, start, stop):
        o, l, r = out.ap, lhsT.ap, rhs.ap
        self.op("pe", lambda e: e.matmul(o, l, r, start=start, stop=stop), [lhsT, rhs], [out])

    def act(self, out, in_, func, bias=None, scale=None):
        o, i = out.ap, in_.ap
        kw = {}
        if bias is not None:
            kw["bias"] = self._a(bias)
        if scale is not None:
            kw["scale"] = self._a(scale)
        self.op("act", lambda e: e.activation(out=o, in_=i, func=func, **kw), [in_, bias, scale], [out])

    def tt(self, out, in0, in1, op, eng="dve"):
        o, a, b = out.ap, in0.ap, in1.ap
        self.op(eng, lambda e: e.tensor_tensor(out=o, in0=a, in1=b, op=op), [in0, in1], [out])

    def stt(self, out, in0, scalar, in1, op0, op1):
        o, a, s, b = out.ap, in0.ap, self._a(scalar), in1.ap
        self.op("dve", lambda e: e.scalar_tensor_tensor(out=o, in0=a, scalar=s, in1=b, op0=op0, op1=op1),
                [in0, scalar, in1], [out])

    def ts(self, out, in0, s1, s2, op0, op1=None, eng="dve"):
        o, a, x1, x2 = out.ap, in0.ap, self._a(s1), self._a(s2)
        if op1 is None:
            fn = lambda e: e.tensor_scalar(out=o, in0=a, scalar1=x1, scalar2=None, op0=op0)
        else:
            fn = lambda e: e.tensor_scalar(out=o, in0=a, scalar1=x1, scalar2=x2, op0=op0, op1=op1)
        self.op(eng, fn, [in0, s1, s2], [out])

    def scan(self, out, d0, d1, initial, op0, op1):
        o, a, b, i = out.ap, d0.ap, d1.ap, self._a(initial)
        self.op("dve", lambda e: e.tensor_tensor_scan(out=o, data0=a, data1=b, initial=i, op0=op0, op1=op1),
                [d0, d1, initial], [out])

    def copy(self, out, in_, eng="dve"):
        o, i = out.ap, in_.ap
        self.op(eng, lambda e: e.tensor_copy(out=o, in_=i), [in_], [out])

    def recip(self, out, in_):
        o, i = out.ap, in_.ap
        self.op("dve", lambda e: e.reciprocal(out=o, in_=i), [in_], [out])

    def memset(self, v, val, eng="dve"):
        a = v.ap
        self.op(eng, lambda e: e.memset(a, val), [], [v])

    def vmax(self, out, in_):
        o, i = out.ap, in_.ap
        self.op("dve", lambda e: e.max(out=o, in_=i), [in_], [out])

    def emit(self, final_bufs):
        nc = self.nc
        waits = [b.last_w for b in final_bufs if b.last_w is not None]
        self.ops.append(("sp", None, waits, None))
        with ExitStack() as st:
            sems = {}
            for e in COMPUTE:
                sems[e] = st.enter_context(nc.semaphore("s_" + e))
            print("free sems", nc.free_len(), "dma slots", len(self.slot_cnt))
            for i in range(len(self.slot_cnt)):
                sems[("d", i)] = st.enter_context(nc.semaphore(f"d{i}"))
            for b in self.cbufs.values():
                sems[("c", b.id)] = st.enter_context(nc.semaphore(f"c{b.id}"))
            block = st.enter_context(nc.Block())
            ops = self.ops

            def mk(name):
                def body(eng):
                    for (e, fn, waits, inc) in ops:
                        if e != name:
                            continue
                        for (kk, v) in waits:
                            eng.wait_ge(sems[kk], v)
                        if fn is None:
                            continue
                        ins = fn(eng)
                        if inc[1] is None:
                            ins.then_inc(sems[inc[0]])
                        else:
                            ins.then_inc(sems[inc[0]], inc[1])
                return body

            block.tensor(mk("pe"))
            block.scalar(mk("act"))
            block.vector(mk("dve"))
            block.gpsimd(mk("pool"))
            block.sync(mk("sp"))


def setup_consts(k, cfg, din):
    c = {}
    c["ones"] = k.alloc("ones", [128, 128], BF16)
    k.memset(c["ones"].v(), 1.0)
    c["zeros"] = k.alloc("zeros", [128, 512], F32)
    k.memset(c["zeros"].v(), 0.0)
    c["ident"] = k.alloc("ident", [128, 128], F32)
    k.dma("sp", c["ident"].v(), din["ident"].v())
    c["sel"] = k.alloc("sel", [8, 8 * 128], F32)
    k.dma("sp", c["sel"].v(), din["sel"].v())
    c["flags"] = k.alloc("flags", [128, 8], F32)
    k.dma("sp", c["flags"].v(), din["flags"].v())
    k.persist = k.ptr
    return c


def rows(buf, r0, nr, c0, nc_):
    return V(buf, buf.ap[r0:r0 + nr, c0:c0 + nc_].rearrange("(k p) t -> p k t", p=128))


def stage_A(k, cfg, c, dw, xmain, xoff, xhalo, xhoff, yT, YL, PG, cst, halo_gathered=False):
    D, KC, NJ, T, W, CW = cfg["D"], cfg["KC"], cfg["NJ"], cfg["T"], cfg["W"], cfg["CW"]
    PP, XG, off = cfg["PP"], cfg["XG"], cfg["off"]
    k.ptr = k.persist
    vec = k.alloc("vec", [128, cfg["NV"]], F32)
    k.dma("sp", vec.v(), dw["vecs"].v())
    wa = k.alloc("wa", [128, NJ, 128], BF16)
    wx = k.alloc("wx", [128, NJ, 128], BF16)
    k.dma("pool", wa.v(), dw["wa"].v().re("h i j -> i h j"))
    k.dma("pool", wx.v(), dw["wx"].v().re("h i j -> i h j"))

    def vcol(name, i, n=1):
        return vec[:, off[name] + i: off[name] + i + n]

    sm = k.alloc("sm", [128, 10 * NJ], F32)
    s_ = [sm[:, i * NJ:(i + 1) * NJ] for i in range(10)]
    e_, ln_, t1, msk, t2, spv, nsp, nsp2 = s_[0], s_[1], s_[2], s_[3], s_[4], s_[5], s_[6], s_[7]
    lam = vcol("lam", 0, NJ)
    k.act(e_, lam, AF.Exp, scale=-1.0)
    k.act(ln_, e_, AF.Ln, bias=1.0)
    k.ts(t1, e_, -1.0 / 3.0, 0.5, ALU.mult, ALU.add)
    k.tt(t1, t1, e_, ALU.mult)
    k.ts(t1, t1, -1.0, 1.0, ALU.mult, ALU.add)
    k.tt(t1, t1, e_, ALU.mult)
    k.ts(msk, e_, 0.05, None, ALU.is_lt)
    k.tt(t2, t1, ln_, ALU.subtract)
    k.tt(t2, t2, msk, ALU.mult)
    k.tt(spv, t2, ln_, ALU.add)
    k.ts(nsp, spv, -8.0, None, ALU.mult)
    k.ts(nsp2, spv, -16.0, None, ALU.mult)

    hst = k.alloc("hst", [128, NJ], F32)
    pst = k.alloc("pst", [128, NJ], F32)
    k.memset(hst.v(), 0.0)
    k.memset(pst.v(), 1.0)
    chalo = [k.alloc(f"chalo{j}", [128, HALO], F32) for j in range(NJ)]
    rhalo = [k.alloc(f"rhalo{j}", [128, HALO], F32) for j in range(NJ)]

    xt = [k.alloc(f"xt{g}", [128, XG, W], F32) for g in range(KC // XG)]
    if halo_gathered:
        xhp = [k.alloc(f"xhp{i}", [128, XG, HALO], F32) for i in range(3)]
    hT = [k.alloc(f"hT{i}", [128, W], BF16) for i in range(KC)]
    sqp = Rot([k.alloc(f"sq{i}", [128, W], BF16) for i in range(2)])
    bfp = Rot([k.alloc(f"bfp{i}", [128, W], BF16) for i in range(4)])
    wtA = Rot([k.alloc(f"wtA{i}", [128, KC, PP * 128], BF16) for i in range(2)])
    wtB = Rot([k.alloc(f"wtB{i}", [128, KC, PP * 128], BF16) for i in range(2)])
    cwp = Rot([k.alloc(f"cw{i}", [128, HALO + W], F32) for i in range(2)])
    rwp = Rot([k.alloc(f"rw{i}", [128, HALO + W], F32) for i in range(2)])
    ccb = [k.alloc(f"cc{j}", [128, W], F32) for j in range(NJ)]
    fp = Rot([k.alloc(f"fp{i}", [128, W], F32) for i in range(14)])
    rstd = k.alloc("rstd", [128, W], F32)
    mu = k.alloc("mu", [128, W], F32)
    rs2 = k.alloc("rs2", [128, W], F32)
    ycv = Rot([k.alloc(f"ycv{i}", [128, NJ, W], BF16) for i in range(2)])
    ps_ss, ps_s1, ps_s2 = k.banks[6], k.banks[6], k.banks[7]
    ones = c["ones"].v()
    flags = c["flags"]
    w_in = dw["w_in"]

    tiles = [("halo", 0, HALO)] + [("main", i, W) for i in range(T // W)]
    for kind, ti, w in tiles:
        for g in range(KC // XG):
            if kind == "halo" and halo_gathered:
                for jj in range(3):
                    k.dma("sp", xhp[jj].v(), rows(xhalo, jj * D + g * XG * 128, XG * 128, 0, w))
                k.ts(xt[g][:, :, 0:w], xhp[0].v(), flags[:, 5:6], None, ALU.mult)
                k.stt(xt[g][:, :, 0:w], xhp[1].v(), flags[:, 6:7], xt[g][:, :, 0:w], ALU.mult, ALU.add)
                k.stt(xt[g][:, :, 0:w], xhp[2].v(), flags[:, 7:8], xt[g][:, :, 0:w], ALU.mult, ALU.add)
                continue
            if kind == "halo":
                src = rows(xhalo, g * XG * 128, XG * 128, xhoff, w)
            else:
                src = rows(xmain, g * XG * 128, XG * 128, xoff + ti * W, w)
            k.dma("sp", xt[g][:, :, 0:w], src)
        for kc in range(KC):
            sq = sqp.next()
            k.act(sq[:, :w], xt[kc // XG][:, kc % XG, 0:w], AF.Square)
            k.mm(ps_ss[:, :w], ones, sq[:, :w], kc == 0, kc == KC - 1)
        rt = fp.next()
        k.act(rt[:, :w], ps_ss[:, :w], AF.Sqrt, bias=EPS, scale=1.0 / D)
        k.recip(rstd[:, :w], rt[:, :w])
        for kc in range(KC):
            k.stt(hT[kc][:, :w], xt[kc // XG][:, kc % XG, 0:w], vcol("mixn", kc), rstd[:, :w],
                  ALU.mult, ALU.mult)

        for branch in (0, 1):
            for q in range(NJ // PP):
                A, B = wtA.next(), wtB.next()
                ca0 = branch * 2 * CW + q * PP * 128
                cb0 = ca0 + CW
                k.dma("pool", A.v(), rows(w_in, 0, D, ca0, PP * 128))
                k.dma("pool", B.v(), rows(w_in, 0, D, cb0, PP * 128))
                for pr in range(PP):
                    j = q * PP + pr
                    psa, psb = k.bank(), k.bank()
                    for kc in range(KC):
                        k.mm(psa[:, :w], A[:, kc, pr * 128:(pr + 1) * 128], hT[kc][:, :w], kc == 0, kc == KC - 1)
                    need_b = not (branch == 1 and kind == "halo")
                    if need_b:
                        for kc in range(KC):
                            k.mm(psb[:, :w], B[:, kc, pr * 128:(pr + 1) * 128], hT[kc][:, :w], kc == 0,
                                 kc == KC - 1)
                    if branch == 0:
                        sgt = fp.next()
                        k.act(sgt[:, :w], psb[:, :w], AF.Sigmoid)
                        if kind == "halo":
                            k.tt(chalo[j][:, :], psa[:, :w], sgt[:, :w], ALU.mult)
                            continue
                        cw = cwp.next()
                        k.copy(cw[:, 0:HALO], chalo[j][:, :])
                        k.tt(cw[:, HALO:HALO + W], psa[:, :W], sgt[:, :W], ALU.mult)
                        cc = ccb[j]
                        k.ts(cc[:, :], cw[:, 2:2 + W], vcol("cw", j * CK), vcol("cb", j), ALU.mult, ALU.add)
                        for kk in range(1, CK):
                            k.stt(cc[:, :], cw[:, 2 + kk:2 + kk + W], vcol("cw", j * CK + kk), cc[:, :],
                                  ALU.mult, ALU.add)
                        k.copy(chalo[j][:, :], cw[:, W:W + HALO])
                        b1, b2 = bfp.next(), bfp.next()
                        k.act(b1[:, :], cc[:, :], AF.Identity)
                        k.act(b2[:, :], cc[:, :], AF.Square)
                        k.mm(ps_s1[:, :W], ones, b1[:, :], j == 0, j == NJ - 1)
                        k.mm(ps_s2[:, :W], ones, b2[:, :], j == 0, j == NJ - 1)
                    else:
                        if kind == "halo":
                            k.act(rhalo[j][:, :], psa[:, :w], AF.Identity)
                            continue
                        rw = rwp.next()
                        k.copy(rw[:, 0:HALO], rhalo[j][:, :])
                        k.act(rw[:, HALO:HALO + W], psa[:, :W], AF.Identity)
                        G = fp.next()
                        k.act(G[:, :], psb[:, :W], AF.Gelu_apprx_tanh)
                        rc = fp.next()
                        k.ts(rc[:, :], rw[:, HALO - 3:HALO - 3 + W], vcol("lw", j * LK), vcol("lcb", j),
                             ALU.mult, ALU.add)
                        for kk in range(1, LK):
                            k.stt(rc[:, :], rw[:, HALO - 3 + kk:HALO - 3 + kk + W], vcol("lw", j * LK + kk),
                                  rc[:, :], ALU.mult, ALU.add)
                        k.copy(rhalo[j][:, :], rw[:, W:W + HALO])
                        rcb = bfp.next()
                        k.act(rcb[:, :], rc[:, :], AF.Identity)
                        pga, pgx = k.bank(), k.bank()
                        k.mm(pga[:, :W], wa[:, j, :], rcb[:, :], True, True)
                        k.mm(pgx[:, :W], wx[:, j, :], rcb[:, :], True, True)
                        ga, gx, a_, a2, ml = fp.next(), fp.next(), fp.next(), fp.next(), fp.next()
                        k.act(ga[:, :], pga[:, :W], AF.Sigmoid, bias=vcol("ba", j))
                        k.act(gx[:, :], pgx[:, :W], AF.Sigmoid, bias=vcol("bx", j))
                        k.act(a_[:, :], ga[:, :], AF.Exp, scale=nsp[:, j:j + 1])
                        k.act(a2[:, :], ga[:, :], AF.Exp, scale=nsp2[:, j:j + 1])
                        k.act(ml[:, :], a2[:, :], AF.Sqrt, bias=1.0, scale=-1.0)
                        if ti == 0:
                            k.ts(ml[:, 0:1], ml[:, 0:1], flags[:, 4:5], flags[:, 3:4], ALU.mult, ALU.add)
                        bb = fp.next()
                        k.tt(bb[:, :], gx[:, :], rc[:, :], ALU.mult)
                        k.tt(bb[:, :], bb[:, :], ml[:, :], ALU.mult)
                        hl, Pc = fp.next(), fp.next()
                        k.scan(hl[:, :], a_[:, :], bb[:, :], hst[:, j:j + 1], ALU.mult, ALU.add)
                        k.copy(hst[:, j:j + 1], hl[:, W - 1:W])
                        k.scan(Pc[:, :], a_[:, :], c["zeros"][:, :W], pst[:, j:j + 1], ALU.mult, ALU.add)
                        k.copy(pst[:, j:j + 1], Pc[:, W - 1:W])
                        k.tt(hl[:, :], hl[:, :], G[:, :], ALU.mult)
                        k.tt(Pc[:, :], Pc[:, :], G[:, :], ALU.mult)
                        k.dma("sp", YL[j * 128:(j + 1) * 128, ti * W:(ti + 1) * W], hl[:, :])
                        k.dma("sp", PG[j * 128:(j + 1) * 128, ti * W:(ti + 1) * W], Pc[:, :])
            if branch == 0 and kind == "main":
                mu2, var, sd = fp.next(), fp.next(), fp.next()
                k.act(mu[:, :], ps_s1[:, :W], AF.Identity, scale=1.0 / CW)
                k.tt(mu2[:, :], mu[:, :], mu[:, :], ALU.mult)
                k.stt(var[:, :], ps_s2[:, :W], 1.0 / CW, mu2[:, :], ALU.mult, ALU.subtract)
                k.act(sd[:, :], var[:, :], AF.Sqrt, bias=EPS)
                k.recip(rs2[:, :], sd[:, :])
                yc = ycv.next()
                for j in range(NJ):
                    t_ = fp.next()
                    k.tt(t_[:, :], ccb[j][:, :], mu[:, :], ALU.subtract)
                    k.tt(t_[:, :], t_[:, :], rs2[:, :], ALU.mult)
                    k.act(yc[:, j, :], t_[:, :], AF.Silu, bias=vcol("lnb", j), scale=vcol("lng", j))
                k.dma("sp", rows(yT, 0, CW, ti * W, W), yc.v())
    k.dma("sp", cst[0:128, :], pst.v())
    k.dma("sp", cst[128:256, :], hst.v())


def stage_B(k, cfg, c, dw, xres, xroff, yT, YL, PG, carr, xout, xlast, outT, is_moe, is_last):
    D, KC, NJ, T, W, CW, TH = cfg["D"], cfg["KC"], cfg["NJ"], cfg["T"], cfg["W"], cfg["CW"], cfg["TH"]
    off, DGW = cfg["off"], cfg["DGW"]
    NE = cfg["NE"] if is_moe else 1
    F = cfg["FE"] if is_moe else cfg["F"]
    NTH = TH // W
    k.ptr = k.persist
    vec = k.alloc("vec", [128, cfg["NV"]], F32)
    k.dma("sp", vec.v(), dw["vecs"].v())

    def vcol(name, i, n=1):
        return vec[:, off[name] + i: off[name] + i + n]

    flags = c["flags"]
    ones = c["ones"].v()
    ca = k.alloc("ca", [128, 8, NJ], F32)
    k.dma("sp", ca.v(), carr.v().re("(sa p) j -> p sa j", p=128))
    carry = k.alloc("carry", [128, NJ], F32)
    ctmp = k.alloc("ctmp", [128, NJ], F32)
    k.memset(carry.v(), 0.0)
    for s in range(3):
        k.tt(ctmp[:, :], ca[:, 2 * s, :], carry[:, :], ALU.mult)
        k.tt(ctmp[:, :], ctmp[:, :], ca[:, 2 * s + 1, :], ALU.add)
        k.tt(ctmp[:, :], ctmp[:, :], carry[:, :], ALU.subtract)
        k.stt(carry[:, :], ctmp[:, :], flags[:, s:s + 1], carry[:, :], ALU.mult, ALU.add)

    if is_moe:
        wr = k.alloc("wr", [128, KC, 8], F32)
        k.dma("sp", wr.v(), dw["w_router"].v().re("(kc p) e -> p kc e", p=128))
        lgT = k.alloc("lgT", [8, TH], F32)
        gwT = k.alloc("gwT", [8, TH], F32)
        smal = k.alloc("smal", [128, 64], F32)
    h2T = [k.alloc(f"h2T{i}", [128, TH], BF16) for i in range(KC)]
    acc = [k.alloc(f"acc{i}", [128, TH], F32) for i in range(KC)]
    base = k.ptr
    ps_ss = k.banks[6]

    for hh in range(2):
        k.barrier()
        k.ptr = base
        yt = k.alloc("yt", [128, 2 * NJ, W], BF16)
        ylp = Rot([k.alloc(f"yl{i}", [128, W], F32) for i in range(2)])
        pgp = Rot([k.alloc(f"pg{i}", [128, W], F32) for i in range(2)])
        wop = Rot([k.alloc(f"wo{i}", [128, 2 * NJ, DGW], BF16) for i in range(2)])
        sqp = Rot([k.alloc(f"sqb{i}", [128, W], BF16) for i in range(2)])
        rt = k.alloc("rtb", [128, W], F32)
        rstd = k.alloc("rstdb", [128, W], F32)
        hfp = Rot([k.alloc(f"hf{i}", [128, W], F32) for i in range(2)])
        for tt in range(NTH):
            col = hh * TH + tt * W
            lc = tt * W
            for d in range(KC):
                k.dma("sp", acc[d][:, lc:lc + W], xres[d * 128:(d + 1) * 128, xroff + col:xroff + col + W])
            k.dma("sp", yt[:, 0:NJ, :], rows(yT, 0, CW, col, W))
            for j in range(NJ):
                yl, pg = ylp.next(), pgp.next()
                k.dma("sp", yl[:, :], YL[j * 128:(j + 1) * 128, col:col + W])
                k.dma("sp", pg[:, :], PG[j * 128:(j + 1) * 128, col:col + W])
                k.stt(yt[:, NJ + j, :], pg[:, :], carry[:, j:j + 1], yl[:, :], ALU.mult, ALU.add)
            for dg in range(D // DGW):
                wo = wop.next()
                k.dma("pool", wo.v(), rows(dw["w_out"], 0, D, dg * DGW, DGW))
                for dc in range(DGW // 128):
                    d = dg * (DGW // 128) + dc
                    ps = k.bank()
                    for e in range(2 * NJ):
                        k.mm(ps[:, :W], wo[:, e, dc * 128:(dc + 1) * 128], yt[:, e, :], e == 0, e == 2 * NJ - 1)
                    k.tt(acc[d][:, lc:lc + W], ps[:, :W], acc[d][:, lc:lc + W], ALU.add)
                    sq = sqp.next()
                    k.act(sq[:, :], acc[d][:, lc:lc + W], AF.Square)
                    k.mm(ps_ss[:, :W], ones, sq[:, :], d == 0, d == KC - 1)
            k.act(rt[:, :], ps_ss[:, :W], AF.Sqrt, bias=EPS, scale=1.0 / D)
            k.recip(rstd[:, :], rt[:, :])
            for kc in range(KC):
                k.stt(h2T[kc][:, lc:lc + W], acc[kc][:, lc:lc + W], vcol("ffnn", kc), rstd[:, :],
                      ALU.mult, ALU.mult)
            if is_moe:
                pl = k.bank()
                for kc in range(KC):
                    hf = hfp.next()
                    k.stt(hf[:, :], acc[kc][:, lc:lc + W], vcol("ffnn", kc), rstd[:, :], ALU.mult, ALU.mult)
                    k.mm(pl[0:8, :W], wr[:, kc, :], hf[:, :], kc == 0, kc == KC - 1)
                k.act(lgT[0:8, lc:lc + W], pl[0:8, :W], AF.Identity)
        if is_moe:
            for blk in range(TH // 128):
                sl = slice(blk * 128, (blk + 1) * 128)
                lg, m8, msk, nl1 = smal[:, 0:8], smal[:, 8:16], smal[:, 16:24], smal[:, 24:25]
                ex, e2, den, rden, gw = smal[:, 32:40], smal[:, 25:26], smal[:, 26:27], smal[:, 27:28], smal[:, 40:48]
                pl = k.bank()
                k.mm(pl[:, 0:8], lgT[0:8, sl], c["ident"][0:8, 0:8], True, True)
                k.act(lg, pl[:, 0:8], AF.Identity)
                k.vmax(m8, lg)
                k.ts(msk, lg, m8[:, 1:2], None, ALU.is_ge)
                k.ts(nl1, m8[:, 0:1], -1.0, None, ALU.mult)
                k.act(ex, lg, AF.Exp, bias=nl1)
                k.act(e2, m8[:, 1:2], AF.Exp, bias=nl1)
                k.ts(den, e2, 1.0, None, ALU.add)
                k.recip(rden, den)
                k.stt(gw, ex, rden, msk, ALU.mult, ALU.mult)
                pt = k.bank()
                k.mm(pt[0:8, 0:128], gw, c["ident"][:, :], True, True)
                k.act(gwT[0:8, sl], pt[0:8, 0:128], AF.Identity)

        k.barrier()
        k.ptr = base
        wgp = Rot([k.alloc(f"wg{i}", [128, KC, 256], BF16) for i in range(2)])
        wup = Rot([k.alloc(f"wu{i}", [128, KC, 256], BF16) for i in range(2)])
        wdp = Rot([k.alloc(f"wd{i}", [128, 4, D], BF16) for i in range(2)])
        actT = [k.alloc(f"actT{i}", [128, TH], BF16) for i in range(4)]
        sgp = Rot([k.alloc(f"sg{i}", [128, W], F32) for i in range(2)])
        tmpp = Rot([k.alloc(f"tm{i}", [128, W], F32) for i in range(2)])
        if is_moe:
            gwBp = Rot([k.alloc(f"gwB{i}", [128, TH], F32) for i in range(2)])
        for ex_i in range(NE):
            if is_moe:
                wg_d, wu_d, wd_d = (V(dw[n], dw[n].ap[ex_i]) for n in ("wg", "wu", "wd"))
                gwB = gwBp.next()
                for tt in range(NTH):
                    pb = k.bank()
                    k.mm(pb[:, :W], c["sel"][0:8, ex_i * 128:(ex_i + 1) * 128], gwT[0:8, tt * W:(tt + 1) * W],
                         True, True)
                    k.act(gwB[:, tt * W:(tt + 1) * W], pb[:, :W], AF.Identity)
            else:
                wg_d, wu_d, wd_d = (dw[n].v() for n in ("wg", "wu", "wd"))
            for g in range(F // 512):
                wd_t = wdp.next()
                k.dma("pool", wd_t.v(), wd_d[g * 512:(g + 1) * 512, :].re("(f p) d -> p f d", p=128))
                for pr in range(2):
                    wg_t, wu_t = wgp.next(), wup.next()
                    f0 = g * 512 + pr * 256
                    k.dma("pool", wg_t.v(), wg_d[:, f0:f0 + 256].re("(kc p) f -> p kc f", p=128))
                    k.dma("pool", wu_t.v(), wu_d[:, f0:f0 + 256].re("(kc p) f -> p kc f", p=128))
                    for fc2 in range(2):
                        fc = pr * 2 + fc2
                        for tt in range(NTH):
                            cs = slice(tt * W, (tt + 1) * W)
                            pg_, pu_ = k.bank(), k.bank()
                            for kc in range(KC):
                                k.mm(pg_[:, :W], wg_t[:, kc, fc2 * 128:(fc2 + 1) * 128], h2T[kc][:, cs], kc == 0,
                                     kc == KC - 1)
                            for kc in range(KC):
                                k.mm(pu_[:, :W], wu_t[:, kc, fc2 * 128:(fc2 + 1) * 128], h2T[kc][:, cs], kc == 0,
                                     kc == KC - 1)
                            sg = sgp.next()
                            k.act(sg[:, :], pg_[:, :W], AF.Silu)
                            if is_moe:
                                tm = tmpp.next()
                                k.tt(tm[:, :], pu_[:, :W], sg[:, :], ALU.mult)
                                k.tt(actT[fc][:, cs], tm[:, :], gwB[:, cs], ALU.mult)
                            else:
                                k.tt(actT[fc][:, cs], pu_[:, :W], sg[:, :], ALU.mult)
                for d in range(KC):
                    for tt in range(NTH):
                        cs = slice(tt * W, (tt + 1) * W)
                        pd = k.bank()
                        for fc in range(4):
                            k.mm(pd[:, :W], wd_t[:, fc, d * 128:(d + 1) * 128], actT[fc][:, cs], fc == 0, fc == 3)
                        k.tt(acc[d][:, cs], pd[:, :W], acc[d][:, cs], ALU.add)
        if not is_last:
            for d in range(KC):
                k.dma("sp", xout[d * 128:(d + 1) * 128, hh * TH:(hh + 1) * TH], acc[d][:, :])
                if hh == 1:
                    k.dma("sp", xlast[d * 128:(d + 1) * 128, :], acc[d][:, TH - HALO:TH])
        else:
            k.barrier()
            k.ptr = base
            sqf = Rot([k.alloc(f"sqf{i}", [128, W], BF16) for i in range(2)])
            otp = Rot([k.alloc(f"ot{i}", [128, W], F32) for i in range(2)])
            rtf = k.alloc("rtf", [128, W], F32)
            rsf = k.alloc("rsf", [128, W], F32)
            for tt in range(NTH):
                cs = slice(tt * W, (tt + 1) * W)
                for d in range(KC):
                    sq = sqf.next()
                    k.act(sq[:, :], acc[d][:, cs], AF.Square)
                    k.mm(ps_ss[:, :W], ones, sq[:, :], d == 0, d == KC - 1)
                k.act(rtf[:, :], ps_ss[:, :W], AF.Sqrt, bias=EPS, scale=1.0 / D)
                k.recip(rsf[:, :], rtf[:, :])
                for d in range(KC):
                    ot = otp.next()
                    k.stt(ot[:, :], acc[d][:, cs], vcol("finn", d), rsf[:, :], ALU.mult, ALU.mult)
                    k.dma("sp", outT[d * 128:(d + 1) * 128, hh * TH + tt * W: hh * TH + (tt + 1) * W], ot[:, :])


ARENA_WORDS = 52800


def build_program(cfg, stages):
    nc = bass.Bass("TRN2", target_bir_lowering=False)
    D, KC, NJ, T, CW = cfg["D"], cfg["KC"], cfg["NJ"], cfg["T"], cfg["CW"]
    fused = len(stages) == 4
    groups = [[0, 1, 2, 3], [4, 5, 6, 7]]
    final_bufs = []
    with ExitStack() as st:
        k = K(nc, st, ARENA_WORDS)

        def ext_in(name, shape, dtype=F32):
            return k.dram(name, shape, dtype, "ExternalInput")

        def inter(name, shape, dtype, producer_stage, consumer_stages):
            if producer_stage in stages:
                if all(s in stages for s in consumer_stages):
                    b = k.dram(name, shape, dtype, "Internal")
                else:
                    b = k.dram(name, shape, dtype, "ExternalOutput")
                    final_bufs.append(b)
                return b
            if any(s in stages for s in consumer_stages):
                return ext_in(name, shape, dtype)
            return None

        din = {"ident": ext_in("ident", [128, 128]), "sel": ext_in("sel", [8, 1024]),
               "flags": ext_in("flags", [128, 8])}
        c = setup_consts(k, cfg, din)
        xTin = ext_in("xTin", [D, HALO + T]) if ("A0" in stages or "B0" in stages) else None
        t = {}
        for l in (0, 1):
            t[f"yT{l}"] = inter(f"yT{l}", [CW, T], BF16, f"A{l}", [f"B{l}"])
            t[f"YL{l}"] = inter(f"YL{l}", [CW, T], F32, f"A{l}", [f"B{l}"])
            t[f"PG{l}"] = inter(f"PG{l}", [CW, T], F32, f"A{l}", [f"B{l}"])
            if fused:
                t[f"cst{l}"] = k.dram(f"cst{l}", [256, NJ], F32, "Internal")
                t[f"carr{l}"] = k.dram(f"carr{l}", [1024, NJ], F32, "Internal")
            else:
                t[f"cst{l}"] = inter(f"cst{l}", [256, NJ], F32, f"A{l}", ["host"])
                t[f"carr{l}"] = ext_in(f"carr{l}", [1024, NJ]) if f"B{l}" in stages else None
        t["xT1"] = inter("xT1", [D, T], F32, "B0", ["A1", "B1"])
        if fused:
            t["xlast"] = k.dram("xlast", [D, HALO], F32, "Internal")
            t["xh1"] = k.dram("xhall", [4 * D, HALO], F32, "Internal")
        else:
            t["xlast"] = inter("xlast", [D, HALO], F32, "B0", ["host"])
            t["xh1"] = ext_in("xh1", [D, HALO]) if "A1" in stages else None
        if "B1" in stages:
            outT = k.dram("outT", [D, T], F32, "ExternalOutput")
            final_bufs.append(outT)

        def layer_w(l, names):
            return {n: ext_in(f"{n}{l}", shp, F32) for n, shp in names}

        for sname in stages:
            l = int(sname[1])
            k.barrier()
            if sname[0] == "A":
                dw = layer_w(l, [("vecs", [128, cfg["NV"]]), ("w_in", [D, cfg["EIN"]]),
                                 ("wa", [NJ, 128, 128]), ("wx", [NJ, 128, 128])])
                if l == 0:
                    stage_A(k, cfg, c, dw, xTin, HALO, xTin, 0, t["yT0"], t["YL0"], t["PG0"], t["cst0"])
                else:
                    stage_A(k, cfg, c, dw, t["xT1"], 0, t["xh1"], 0, t["yT1"], t["YL1"], t["PG1"], t["cst1"],
                            halo_gathered=fused)
                if fused:
                    k.collective(t[f"carr{l}"], t[f"cst{l}"], groups)
            else:
                names = [("vecsB", [128, cfg["NV"]]), ("w_out", [D, D])]
                if l == 0:
                    names += [("wg", [D, cfg["F"]]), ("wu", [D, cfg["F"]]), ("wd", [cfg["F"], D])]
                else:
                    names += [("w_router", [D, 8]), ("wg", [cfg["NE"], D, cfg["FE"]]),
                              ("wu", [cfg["NE"], D, cfg["FE"]]), ("wd", [cfg["NE"], cfg["FE"], D])]
                dw = layer_w(l, names)
                dw["vecs"] = dw["vecsB"]
                if l == 0:
                    stage_B(k, cfg, c, dw, xTin, HALO, t["yT0"], t["YL0"], t["PG0"], t["carr0"],
                            t["xT1"], t["xlast"], None, False, False)
                    if fused:
                        k.collective(t["xh1"], t["xlast"], groups)
                else:
                    stage_B(k, cfg, c, dw, t["xT1"], 0, t["yT1"], t["YL1"], t["PG1"], t["carr1"],
                            None, None, outT, True, True)
        k.emit(final_bufs)
        build_program.last_peak = k.peak
    return nc


def vec2d(v, n):
    return np.ascontiguousarray(np.asarray(v, np.float32).reshape(n, 128).T)


def pack_vecs(cfg, inp, l):
    KC, NJ = cfg["KC"], cfg["NJ"]
    cw = np.asarray(inp["conv_w"][l], np.float32)
    lw = np.asarray(inp["lru_conv_w"][l], np.float32)
    parts = [vec2d(inp["mix_norm"][l], KC), vec2d(inp["ffn_norm"][l], KC), vec2d(inp["final_norm"], KC),
             vec2d(inp["conv_b"][l], NJ), vec2d(inp["conv_ln_g"][l], NJ), vec2d(inp["conv_ln_b"][l], NJ),
             vec2d(inp["lru_conv_b"][l], NJ), vec2d(inp["lru_ba"][l], NJ), vec2d(inp["lru_bx"][l], NJ),
             vec2d(inp["lru_lambda"][l], NJ),
             cw.reshape(CK, NJ, 128).transpose(2, 1, 0).reshape(128, NJ * CK),
             lw.reshape(LK, NJ, 128).transpose(2, 1, 0).reshape(128, NJ * LK)]
    return np.ascontiguousarray(np.concatenate(parts, axis=1).astype(np.float32))


def host_consts():
    ident = np.eye(128, dtype=np.float32)
    sel = np.zeros((8, 8, 128), np.float32)
    for e in range(8):
        sel[e, e, :] = 1.0
    return ident, sel.reshape(8, 1024)


def core_flags(s):
    f = np.zeros((128, 8), np.float32)
    for j in range(3):
        f[:, j] = 1.0 if j < s else 0.0
    f[:, 3] = 1.0 if s == 0 else 0.0
    f[:, 4] = 0.0 if s == 0 else 1.0
    for j in range(3):
        f[:, 5 + j] = 1.0 if j == s - 1 else 0.0
    return f


_PROG_CACHE = {}


def get_prog(cfg, stages):
    key = (cfg["D"], cfg["T"], cfg["F"], cfg["FE"], cfg["W"], tuple(stages))
    if key not in _PROG_CACHE:
        _PROG_CACHE[key] = build_program(cfg, list(stages))
    return _PROG_CACHE[key]


def run_unfused(cfg, inp, n_cores=8):
    D, T, NJ = cfg["D"], cfg["T"], cfg["NJ"]
    x = np.asarray(inp["x"], np.float32)
    B, S, _ = x.shape
    nseg = S // T
    assert B * nseg == n_cores and nseg == 4
    ident, sel = host_consts()
    common = []
    for cidx in range(n_cores):
        b, s = divmod(cidx, nseg)
        xT = np.zeros((D, HALO + T), np.float32)
        lo = s * T - HALO
        if s == 0:
            xT[:, HALO:] = x[b, 0:T, :].T
        else:
            xT[:, :] = x[b, lo:lo + HALO + T, :].T
        common.append({"ident": ident, "sel": sel, "flags": core_flags(s), "xTin": xT})
    vecs = [pack_vecs(cfg, inp, l) for l in (0, 1)]
    f32 = lambda a: np.ascontiguousarray(np.asarray(a, np.float32))

    def wA(l):
        return {f"vecs{l}": vecs[l], f"w_in{l}": f32(inp["w_in"][l]), f"wa{l}": f32(inp["lru_wa"][l]),
                f"wx{l}": f32(inp["lru_wx"][l])}

    def wB(l):
        d = {f"vecsB{l}": vecs[l], f"w_out{l}": f32(inp["w_out"][l])}
        if l == 0:
            d.update({"wg0": f32(inp["dense_wg"][0]), "wu0": f32(inp["dense_wu"][0]), "wd0": f32(inp["dense_wd"][0])})
        else:
            d.update({"w_router1": f32(inp["w_router"][0]), "wg1": f32(inp["moe_wg"][0]),
                      "wu1": f32(inp["moe_wu"][0]), "wd1": f32(inp["moe_wd"][0])})
        return d

    def launch(stage, maps):
        nc = get_prog(cfg, [stage])
        res = run_bass_kernel_spmd(nc, maps, core_ids=list(range(n_cores)))
        return res.results

    def gather_carry(res, l):
        out = []
        for cidx in range(n_cores):
            b = cidx // nseg
            out.append(np.ascontiguousarray(np.concatenate([res[b * nseg + j][f"cst{l}"] for j in range(nseg)], 0)))
        return out

    keepA = ("ident", "sel", "flags")
    rA0 = launch("A0", [dict(common[i], **wA(0)) for i in range(n_cores)])
    carr0 = gather_carry(rA0, 0)
    mB0 = [dict(common[i], **wB(0), yT0=rA0[i]["yT0"], YL0=rA0[i]["YL0"], PG0=rA0[i]["PG0"], carr0=carr0[i])
           for i in range(n_cores)]
    rB0 = launch("B0", mB0)
    mA1 = []
    for i in range(n_cores):
        s = i % nseg
        xh = rB0[i - 1]["xlast"] if s > 0 else np.zeros((D, HALO), np.float32)
        m = {kk: common[i][kk] for kk in keepA}
        m.update(wA(1))
        m.update(xT1=rB0[i]["xT1"], xh1=np.ascontiguousarray(xh))
        mA1.append(m)
    rA1 = launch("A1", mA1)
    carr1 = gather_carry(rA1, 1)
    mB1 = []
    for i in range(n_cores):
        m = {kk: common[i][kk] for kk in keepA}
        m.update(wB(1))
        m.update(xT1=rB0[i]["xT1"], yT1=rA1[i]["yT1"], YL1=rA1[i]["YL1"], PG1=rA1[i]["PG1"], carr1=carr1[i])
        mB1.append(m)
    rB1 = launch("B1", mB1)
    out = np.empty((B, S, D), np.float32)
    for i in range(n_cores):
        b, s = divmod(i, nseg)
        out[b, s * T:(s + 1) * T, :] = rB1[i]["outT"].T
    return out


def run_fused(cfg, inp, n_cores=8):
    D, T = cfg["D"], cfg["T"]
    x = np.asarray(inp["x"], np.float32)
    B, S, _ = x.shape
    nseg = S // T
    assert B * nseg == n_cores and nseg == 4
    ident, sel = host_consts()
    f32 = lambda a: np.ascontiguousarray(np.asarray(a, np.float32))
    vecs = [pack_vecs(cfg, inp, l) for l in (0, 1)]
    shared = {"ident": ident, "sel": sel}
    for l in (0, 1):
        shared.update({f"vecs{l}": vecs[l], f"vecsB{l}": vecs[l], f"w_in{l}": f32(inp["w_in"][l]),
                       f"wa{l}": f32(inp["lru_wa"][l]), f"wx{l}": f32(inp["lru_wx"][l]),
                       f"w_out{l}": f32(inp["w_out"][l])})
    shared.update({"wg0": f32(inp["dense_wg"][0]), "wu0": f32(inp["dense_wu"][0]), "wd0": f32(inp["dense_wd"][0]),
                   "w_router1": f32(inp["w_router"][0]), "wg1": f32(inp["moe_wg"][0]),
                   "wu1": f32(inp["moe_wu"][0]), "wd1": f32(inp["moe_wd"][0])})
    maps = []
    for cidx in range(n_cores):
        b, s = divmod(cidx, nseg)
        xT = np.zeros((D, HALO + T), np.float32)
        if s == 0:
            xT[:, HALO:] = x[b, 0:T, :].T
        else:
            xT[:, :] = x[b, s * T - HALO:(s + 1) * T, :].T
        maps.append(dict(shared, flags=core_flags(s), xTin=xT))
    nc = get_prog(cfg, ["A0", "B0", "A1", "B1"])
    res = run_bass_kernel_spmd(nc, maps, core_ids=list(range(n_cores))).results
    out = np.empty((B, S, D), np.float32)
    for i in range(n_cores):
        b, s = divmod(i, nseg)
        out[b, s * T:(s + 1) * T, :] = res[i]["outT"].T
    return out


FULL_CFG = make_cfg()


def kernel(**inputs):
    return run_fused(FULL_CFG, inputs)
```

```python
import numpy as np
from contextlib import ExitStack
import ml_dtypes
import concourse.bass as bass
import concourse.mybir as mybir
from concourse.bass_utils import run_bass_kernel_spmd

F32 = mybir.dt.float32
BF16 = mybir.dt.bfloat16
AF = mybir.ActivationFunctionType
ALU = mybir.AluOpType
EPS = 1e-6
HALO = 32
CK = 31
LK = 4


def make_cfg(D=2048, T=2048, F=6144, FE=6144, NE=8, W=512):
    c = dict(D=D, KC=D // 128, CW=D // 2, NJ=D // 256, EIN=2 * D, T=T, W=W, F=F, FE=FE, NE=NE,
             TH=T // 2)
    c["PP"] = min(2, c["NJ"])
    c["XG"] = min(4, c["KC"])
    c["DGW"] = min(512, D)
    KC, NJ = c["KC"], c["NJ"]
    off = {}
    p = 0
    for name, n in (("mixn", KC), ("ffnn", KC), ("finn", KC), ("cb", NJ), ("lng", NJ), ("lnb", NJ),
                    ("lcb", NJ), ("ba", NJ), ("bx", NJ), ("lam", NJ), ("cw", NJ * CK), ("lw", NJ * LK)):
        off[name] = p
        p += n
    c["off"] = off
    c["NV"] = p
    return c


class Buf:
    _n = 0

    def __init__(self, ap, kind, name):
        self.ap, self.kind, self.name = ap, kind, name
        self.last_w = None
        self.reads = {}
        self.dcnt = 0
        Buf._n += 1
        self.id = Buf._n

    def __getitem__(self, idx):
        return V(self, self.ap[idx])

    def v(self):
        return V(self, self.ap)


class V:
    def __init__(self, buf, ap):
        self.buf, self.ap = buf, ap

    def __getitem__(self, idx):
        return V(self.buf, self.ap[idx])

    def re(self, s, **kw):
        return V(self.buf, self.ap.rearrange(s, **kw))


class Rot:
    def __init__(self, bufs):
        self.bufs, self.i = bufs, 0

    def next(self):
        b = self.bufs[self.i % len(self.bufs)]
        self.i += 1
        return b


COMPUTE = ("pe", "act", "dve", "pool")
STREAMS = ("pe", "act", "dve", "pool", "sp")


class K:
    def __init__(self, nc, st, arena_words):
        self.nc = nc
        self.arena = st.enter_context(nc.sbuf_tensor("arena", [128, arena_words], F32))[:]
        self.arena_words = arena_words
        self.ptr = 0
        self.ops = []
        self.cnt = {e: 0 for e in COMPUTE}
        self.waited = {e: {} for e in STREAMS}
        self.slot_cnt = []
        self.free_slots = []
        self.live_dbufs = []
        self.cbufs = {}
        self.banks = [Buf(st.enter_context(nc.psum_tensor(f"ps{i}", [128, 512], F32))[:], "psum", f"ps{i}")
                      for i in range(8)]
        self.rot = 0
        self.peak = 0

    def alloc(self, name, shape, dtype):
        n = 1
        for s in shape[1:]:
            n *= s
        words = (n + 1) // 2 if dtype == BF16 else n
        words = (words + 7) // 8 * 8
        off = self.ptr
        self.ptr += words
        self.peak = max(self.peak, self.ptr)
        assert self.ptr <= self.arena_words, f"SBUF arena overflow at {name}: {self.ptr} > {self.arena_words}"
        ap = self.arena[:, off:off + words]
        if dtype == BF16:
            ap = ap.bitcast(BF16)
        ap = ap[:, :n]
        if shape[0] < 128:
            ap = ap[0:shape[0]]
        if len(shape) == 3:
            ap = ap.rearrange("p (a b) -> p a b", a=shape[1])
        elif len(shape) == 4:
            ap = ap.rearrange("p (a b c) -> p a b c", a=shape[1], b=shape[2])
        return Buf(ap, "sbuf", name)

    def bank(self):
        b = self.banks[self.rot % 6]
        self.rot += 1
        return b

    def dram(self, name, shape, dtype, kind):
        return Buf(self.nc.dram_tensor(name, list(shape), dtype, kind=kind).ap(), "dram", name)

    def _collect(self, eng, reads, writes):
        need = {}

        def add(tok):
            if tok is not None:
                need[tok[0]] = max(need.get(tok[0], 0), tok[1])

        for b in reads:
            add(b.last_w)
        for b in writes:
            if b.kind != "dram":
                add(b.last_w)
            for kk, v in b.reads.items():
                add((kk, v))
        out = []
        for kk, v in need.items():
            if kk == eng and eng == "pe":
                continue
            if self.waited[eng].get(kk, 0) >= v:
                continue
            self.waited[eng][kk] = v
            out.append((kk, v))
        return out

    def op(self, eng, fn, reads, writes):
        reads = [r.buf for r in reads if isinstance(r, V)]
        writes = [w.buf for w in writes]
        waits = self._collect(eng, reads, writes)
        n = self.cnt[eng] + 1
        self.cnt[eng] = n
        self.ops.append((eng, fn, waits, (eng, 1)))
        for b in reads:
            b.reads[eng] = max(b.reads.get(eng, 0), n)
        for b in writes:
            b.last_w = (eng, n)
            b.reads = {}

    def dma(self, q, out, in_):
        waits = self._collect(q, [in_.buf], [out.buf])
        dst = out.buf
        if getattr(dst, "dslot", None) is None:
            if self.free_slots:
                dst.dslot = self.free_slots.pop()
            else:
                dst.dslot = len(self.slot_cnt)
                self.slot_cnt.append(0)
            self.live_dbufs.append(dst)
        slot = dst.dslot
        key = ("d", slot)
        self.slot_cnt[slot] += 16
        cntv = self.slot_cnt[slot]
        o_ap, i_ap = out.ap, in_.ap
        self.ops.append((q, lambda e: e.dma_start(out=o_ap, in_=i_ap), waits, (key, 16)))
        in_.buf.reads[key] = max(in_.buf.reads.get(key, 0), cntv)
        dst.last_w = (key, cntv)
        dst.reads = {}

    def collective(self, out_buf, in_buf, groups):
        waits = self._collect("pool", [in_buf], [out_buf])
        key = ("c", out_buf.id)
        self.cbufs[out_buf.id] = out_buf
        o_ap, i_ap = out_buf.ap, in_buf.ap
        self.ops.append(("pool", lambda e: e.collective_compute("AllGather", ALU.bypass, replica_groups=groups,
                                                                  ins=[i_ap], outs=[o_ap]), waits, (key, None)))
        in_buf.reads[key] = 1
        out_buf.last_w = (key, 1)
        out_buf.reads = {}

    def barrier(self):
        allw = [(e, self.cnt[e]) for e in COMPUTE if self.cnt[e] > 0]
        allw += [(("d", i), v) for i, v in enumerate(self.slot_cnt) if v > 0]
        allw += [(("c", b.id), 1) for b in self.cbufs.values()]
        for b in self.live_dbufs:
            self.free_slots.append(b.dslot)
            b.dslot = None
        self.live_dbufs = []
        for e in STREAMS:
            waits = []
            for kk, v in allw:
                if self.waited[e].get(kk, 0) < v:
                    self.waited[e][kk] = v
                    waits.append((kk, v))
            self.ops.append((e, None, waits, None))

    @staticmethod
    def _a(x):
        return x.ap if isinstance(x, V) else x

    def mm(self, out, lhsT, rhs, start, stop):
        o, l, r = out.ap, lhsT.ap, rhs.ap
        self.op("pe", lambda e: e.matmul(o, l, r, start=start, stop=stop), [lhsT, rhs], [out])

    def act(self, out, in_, func, bias=None, scale=None):
        o, i = out.ap, in_.ap
        kw = {}
        if bias is not None:
            kw["bias"] = self._a(bias)
        if scale is not None:
            kw["scale"] = self._a(scale)
        self.op("act", lambda e: e.activation(out=o, in_=i, func=func, **kw), [in_, bias, scale], [out])

    def tt(self, out, in0, in1, op, eng="dve"):
        o, a, b = out.ap, in0.ap, in1.ap
        self.op(eng, lambda e: e.tensor_tensor(out=o, in0=a, in1=b, op=op), [in0, in1], [out])

    def stt(self, out, in0, scalar, in1, op0, op1):
        o, a, s, b = out.ap, in0.ap, self._a(scalar), in1.ap
        self.op("dve", lambda e: e.scalar_tensor_tensor(out=o, in0=a, scalar=s, in1=b, op0=op0, op1=op1),
                [in0, scalar, in1], [out])

    def ts(self, out, in0, s1, s2, op0, op1=None, eng="dve"):
        o, a, x1, x2 = out.ap, in0.ap, self._a(s1), self._a(s2)
        if op1 is None:
            fn = lambda e: e.tensor_scalar(out=o, in0=a, scalar1=x1, scalar2=None, op0=op0)
        else:
            fn = lambda e: e.tensor_scalar(out=o, in0=a, scalar1=x1, scalar2=x2, op0=op0, op1=op1)
        self.op(eng, fn, [in0, s1, s2], [out])

    def scan(self, out, d0, d1, initial, op0, op1):
        o, a, b, i = out.ap, d0.ap, d1.ap, self._a(initial)
        self.op("dve", lambda e: e.tensor_tensor_scan(out=o, data0=a, data1=b, initial=i, op0=op0, op1=op1),
                [d0, d1, initial], [out])

    def copy(self, out, in_, eng="dve"):
        o, i = out.ap, in_.ap
        self.op(eng, lambda e: e.tensor_copy(out=o, in_=i), [in_], [out])

    def recip(self, out, in_):
        o, i = out.ap, in_.ap
        self.op("dve", lambda e: e.reciprocal(out=o, in_=i), [in_], [out])

    def memset(self, v, val, eng="dve"):
        a = v.ap
        self.op(eng, lambda e: e.memset(a, val), [], [v])

    def vmax(self, out, in_):
        o, i = out.ap, in_.ap
        self.op("dve", lambda e: e.max(out=o, in_=i), [in_], [out])

    def emit(self, final_bufs):
        nc = self.nc
        waits = [b.last_w for b in final_bufs if b.last_w is not None]
        self.ops.append(("sp", None, waits, None))
        with ExitStack() as st:
            sems = {}
            for e in COMPUTE:
                sems[e] = st.enter_context(nc.semaphore("s_" + e))
            print("free sems", nc.free_len(), "dma slots", len(self.slot_cnt))
            for i in range(len(self.slot_cnt)):
                sems[("d", i)] = st.enter_context(nc.semaphore(f"d{i}"))
            for b in self.cbufs.values():
                sems[("c", b.id)] = st.enter_context(nc.semaphore(f"c{b.id}"))
            block = st.enter_context(nc.Block())
            ops = self.ops

            def mk(name):
                def body(eng):
                    for (e, fn, waits, inc) in ops:
                        if e != name:
                            continue
                        for (kk, v) in waits:
                            eng.wait_ge(sems[kk], v)
                        if fn is None:
                            continue
                        ins = fn(eng)
                        if inc[1] is None:
                            ins.then_inc(sems[inc[0]])
                        else:
                            ins.then_inc(sems[inc[0]], inc[1])
                return body

            block.tensor(mk("pe"))
            block.scalar(mk("act"))
            block.vector(mk("dve"))
            block.gpsimd(mk("pool"))
            block.sync(mk("sp"))


def setup_consts(k, cfg, din):
    c = {}
    c["ones"] = k.alloc("ones", [128, 128], BF16)
    k.memset(c["ones"].v(), 1.0)
    c["zeros"] = k.alloc("zeros", [128, 512], F32)
    k.memset(c["zeros"].v(), 0.0)
    c["ident"] = k.alloc("ident", [128, 128], F32)
    k.dma("sp", c["ident"].v(), din["ident"].v())
    c["sel"] = k.alloc("sel", [8, 8 * 128], F32)
    k.dma("sp", c["sel"].v(), din["sel"].v())
    c["flags"] = k.alloc("flags", [128, 8], F32)
    k.dma("sp", c["flags"].v(), din["flags"].v())
    k.persist = k.ptr
    return c


def rows(buf, r0, nr, c0, nc_):
    return V(buf, buf.ap[r0:r0 + nr, c0:c0 + nc_].rearrange("(k p) t -> p k t", p=128))


def stage_A(k, cfg, c, dw, xmain, xoff, xhalo, xhoff, yT, YL, PG, cst, halo_gathered=False):
    D, KC, NJ, T, W, CW = cfg["D"], cfg["KC"], cfg["NJ"], cfg["T"], cfg["W"], cfg["CW"]
    PP, XG, off = cfg["PP"], cfg["XG"], cfg["off"]
    k.ptr = k.persist
    vec = k.alloc("vec", [128, cfg["NV"]], F32)
    k.dma("sp", vec.v(), dw["vecs"].v())
    wa = k.alloc("wa", [128, NJ, 128], BF16)
    wx = k.alloc("wx", [128, NJ, 128], BF16)
    k.dma("pool", wa.v(), dw["wa"].v().re("h i j -> i h j"))
    k.dma("pool", wx.v(), dw["wx"].v().re("h i j -> i h j"))

    def vcol(name, i, n=1):
        return vec[:, off[name] + i: off[name] + i + n]

    sm = k.alloc("sm", [128, 10 * NJ], F32)
    s_ = [sm[:, i * NJ:(i + 1) * NJ] for i in range(10)]
    e_, ln_, t1, msk, t2, spv, nsp, nsp2 = s_[0], s_[1], s_[2], s_[3], s_[4], s_[5], s_[6], s_[7]
    lam = vcol("lam", 0, NJ)
    k.act(e_, lam, AF.Exp, scale=-1.0)
    k.act(ln_, e_, AF.Ln, bias=1.0)
    k.ts(t1, e_, -1.0 / 3.0, 0.5, ALU.mult, ALU.add)
    k.tt(t1, t1, e_, ALU.mult)
    k.ts(t1, t1, -1.0, 1.0, ALU.mult, ALU.add)
    k.tt(t1, t1, e_, ALU.mult)
    k.ts(msk, e_, 0.05, None, ALU.is_lt)
    k.tt(t2, t1, ln_, ALU.subtract)
    k.tt(t2, t2, msk, ALU.mult)
    k.tt(spv, t2, ln_, ALU.add)
    k.ts(nsp, spv, -8.0, None, ALU.mult)
    k.ts(nsp2, spv, -16.0, None, ALU.mult)

    hst = k.alloc("hst", [128, NJ], F32)
    pst = k.alloc("pst", [128, NJ], F32)
    k.memset(hst.v(), 0.0)
    k.memset(pst.v(), 1.0)
    chalo = [k.alloc(f"chalo{j}", [128, HALO], F32) for j in range(NJ)]
    rhalo = [k.alloc(f"rhalo{j}", [128, HALO], F32) for j in range(NJ)]

    xt = [k.alloc(f"xt{g}", [128, XG, W], F32) for g in range(KC // XG)]
    if halo_gathered:
        xhp = [k.alloc(f"xhp{i}", [128, XG, HALO], F32) for i in range(3)]
    hT = [k.alloc(f"hT{i}", [128, W], BF16) for i in range(KC)]
    sqp = Rot([k.alloc(f"sq{i}", [128, W], BF16) for i in range(2)])
    bfp = Rot([k.alloc(f"bfp{i}", [128, W], BF16) for i in range(4)])
    wtA = Rot([k.alloc(f"wtA{i}", [128, KC, PP * 128], BF16) for i in range(2)])
    wtB = Rot([k.alloc(f"wtB{i}", [128, KC, PP * 128], BF16) for i in range(2)])
    cwp = Rot([k.alloc(f"cw{i}", [128, HALO + W], F32) for i in range(2)])
    rwp = Rot([k.alloc(f"rw{i}", [128, HALO + W], F32) for i in range(2)])
    ccb = [k.alloc(f"cc{j}", [128, W], F32) for j in range(NJ)]
    fp = Rot([k.alloc(f"fp{i}", [128, W], F32) for i in range(22)])
    rstd = k.alloc("rstd", [128, W], F32)
    mu = k.alloc("mu", [128, W], F32)
    rs2 = k.alloc("rs2", [128, W], F32)
    ycv = Rot([k.alloc(f"ycv{i}", [128, NJ, W], BF16) for i in range(2)])
    ps_ss, ps_s1, ps_s2 = k.banks[6], k.banks[6], k.banks[7]
    ones = c["ones"].v()
    flags = c["flags"]
    w_in = dw["w_in"]

    def lru_part2(j, ti, gx, rc, ml, a_, G):
        bb = fp.next()
        k.tt(bb[:, :], gx[:, :], rc[:, :], ALU.mult)
        k.tt(bb[:, :], bb[:, :], ml[:, :], ALU.mult)
        hl, Pc = fp.next(), fp.next()
        k.scan(hl[:, :], a_[:, :], bb[:, :], hst[:, j:j + 1], ALU.mult, ALU.add)
        k.copy(hst[:, j:j + 1], hl[:, W - 1:W])
        k.scan(Pc[:, :], a_[:, :], c["zeros"][:, :W], pst[:, j:j + 1], ALU.mult, ALU.add)
        k.copy(pst[:, j:j + 1], Pc[:, W - 1:W])
        k.tt(hl[:, :], hl[:, :], G[:, :], ALU.mult)
        k.tt(Pc[:, :], Pc[:, :], G[:, :], ALU.mult)
        k.dma("sp", YL[j * 128:(j + 1) * 128, ti * W:(ti + 1) * W], hl[:, :])
        k.dma("sp", PG[j * 128:(j + 1) * 128, ti * W:(ti + 1) * W], Pc[:, :])

    pend = None
    tiles = [("halo", 0, HALO)] + [("main", i, W) for i in range(T // W)]
    for kind, ti, w in tiles:
        for g in range(KC // XG):
            if kind == "halo" and halo_gathered:
                for jj in range(3):
                    k.dma("sp", xhp[jj].v(), rows(xhalo, jj * D + g * XG * 128, XG * 128, 0, w))
                k.ts(xt[g][:, :, 0:w], xhp[0].v(), flags[:, 5:6], None, ALU.mult)
                k.stt(xt[g][:, :, 0:w], xhp[1].v(), flags[:, 6:7], xt[g][:, :, 0:w], ALU.mult, ALU.add)
                k.stt(xt[g][:, :, 0:w], xhp[2].v(), flags[:, 7:8], xt[g][:, :, 0:w], ALU.mult, ALU.add)
                continue
            if kind == "halo":
                src = rows(xhalo, g * XG * 128, XG * 128, xhoff, w)
            else:
                src = rows(xmain, g * XG * 128, XG * 128, xoff + ti * W, w)
            k.dma("sp", xt[g][:, :, 0:w], src)
        for kc in range(KC):
            sq = sqp.next()
            k.act(sq[:, :w], xt[kc // XG][:, kc % XG, 0:w], AF.Square)
            k.mm(ps_ss[:, :w], ones, sq[:, :w], kc == 0, kc == KC - 1)
        rt = fp.next()
        k.act(rt[:, :w], ps_ss[:, :w], AF.Sqrt, bias=EPS, scale=1.0 / D)
        k.recip(rstd[:, :w], rt[:, :w])
        for kc in range(KC):
            k.stt(hT[kc][:, :w], xt[kc // XG][:, kc % XG, 0:w], vcol("mixn", kc), rstd[:, :w],
                  ALU.mult, ALU.mult)

        for branch in (0, 1):
            for q in range(NJ // PP):
                A, B = wtA.next(), wtB.next()
                ca0 = branch * 2 * CW + q * PP * 128
                cb0 = ca0 + CW
                k.dma("pool", A.v(), rows(w_in, 0, D, ca0, PP * 128))
                k.dma("pool", B.v(), rows(w_in, 0, D, cb0, PP * 128))
                for pr in range(PP):
                    j = q * PP + pr
                    psa, psb = k.bank(), k.bank()
                    for kc in range(KC):
                        k.mm(psa[:, :w], A[:, kc, pr * 128:(pr + 1) * 128], hT[kc][:, :w], kc == 0, kc == KC - 1)
                    need_b = not (branch == 1 and kind == "halo")
                    if need_b:
                        for kc in range(KC):
                            k.mm(psb[:, :w], B[:, kc, pr * 128:(pr + 1) * 128], hT[kc][:, :w], kc == 0,
                                 kc == KC - 1)
                    if branch == 0:
                        sgt = fp.next()
                        k.act(sgt[:, :w], psb[:, :w], AF.Sigmoid)
                        if kind == "halo":
                            k.tt(chalo[j][:, :], psa[:, :w], sgt[:, :w], ALU.mult)
                            continue
                        cw = cwp.next()
                        k.copy(cw[:, 0:HALO], chalo[j][:, :])
                        k.tt(cw[:, HALO:HALO + W], psa[:, :W], sgt[:, :W], ALU.mult)
                        cc = ccb[j]
                        cc2 = fp.next()
                        HK = 16
                        k.ts(cc[:, :], cw[:, 2:2 + W], vcol("cw", j * CK), vcol("cb", j), ALU.mult, ALU.add)
                        k.ts(cc2[:, :], cw[:, 2 + HK:2 + HK + W], vcol("cw", j * CK + HK), None, ALU.mult)
                        for kk in range(1, HK):
                            k.stt(cc[:, :], cw[:, 2 + kk:2 + kk + W], vcol("cw", j * CK + kk), cc[:, :],
                                  ALU.mult, ALU.add)
                            if HK + kk < CK:
                                k.stt(cc2[:, :], cw[:, 2 + HK + kk:2 + HK + kk + W], vcol("cw", j * CK + HK + kk),
                                      cc2[:, :], ALU.mult, ALU.add)
                        k.tt(cc[:, :], cc[:, :], cc2[:, :], ALU.add)
                        k.copy(chalo[j][:, :], cw[:, W:W + HALO])
                        b1, b2 = bfp.next(), bfp.next()
                        k.act(b1[:, :], cc[:, :], AF.Identity)
                        k.act(b2[:, :], cc[:, :], AF.Square)
                        k.mm(ps_s1[:, :W], ones, b1[:, :], j == 0, j == NJ - 1)
                        k.mm(ps_s2[:, :W], ones, b2[:, :], j == 0, j == NJ - 1)
                    else:
                        if kind == "halo":
                            k.act(rhalo[j][:, :], psa[:, :w], AF.Identity)
                            continue
                        rw = rwp.next()
                        k.copy(rw[:, 0:HALO], rhalo[j][:, :])
                        k.act(rw[:, HALO:HALO + W], psa[:, :W], AF.Identity)
                        G = fp.next()
                        k.act(G[:, :], psb[:, :W], AF.Gelu_apprx_tanh)
                        rc = fp.next()
                        k.ts(rc[:, :], rw[:, HALO - 3:HALO - 3 + W], vcol("lw", j * LK), vcol("lcb", j),
                             ALU.mult, ALU.add)
                        for kk in range(1, LK):
                            k.stt(rc[:, :], rw[:, HALO - 3 + kk:HALO - 3 + kk + W], vcol("lw", j * LK + kk),
                                  rc[:, :], ALU.mult, ALU.add)
                        k.copy(rhalo[j][:, :], rw[:, W:W + HALO])
                        rcb = bfp.next()
                        k.act(rcb[:, :], rc[:, :], AF.Identity)
                        pga, pgx = k.bank(), k.bank()
                        k.mm(pga[:, :W], wa[:, j, :], rcb[:, :], True, True)
                        k.mm(pgx[:, :W], wx[:, j, :], rcb[:, :], True, True)
                        ga, gx, a_, a2, ml = fp.next(), fp.next(), fp.next(), fp.next(), fp.next()
                        k.act(ga[:, :], pga[:, :W], AF.Sigmoid, bias=vcol("ba", j))
                        k.act(gx[:, :], pgx[:, :W], AF.Sigmoid, bias=vcol("bx", j))
                        k.act(a_[:, :], ga[:, :], AF.Exp, scale=nsp[:, j:j + 1])
                        k.act(a2[:, :], ga[:, :], AF.Exp, scale=nsp2[:, j:j + 1])
                        k.act(ml[:, :], a2[:, :], AF.Sqrt, bias=1.0, scale=-1.0)
                        if ti == 0:
                            k.ts(ml[:, 0:1], ml[:, 0:1], flags[:, 4:5], flags[:, 3:4], ALU.mult, ALU.add)
                        if pend is not None:
                            lru_part2(*pend)
                        pend = (j, ti, gx, rc, ml, a_, G)
            if branch == 1 and pend is not None:
                lru_part2(*pend)
                pend = None
            if branch == 0 and kind == "main":
                mu2, var, sd = fp.next(), fp.next(), fp.next()
                k.act(mu[:, :], ps_s1[:, :W], AF.Identity, scale=1.0 / CW)
                k.tt(mu2[:, :], mu[:, :], mu[:, :], ALU.mult)
                k.stt(var[:, :], ps_s2[:, :W], 1.0 / CW, mu2[:, :], ALU.mult, ALU.subtract)
                k.act(sd[:, :], var[:, :], AF.Sqrt, bias=EPS)
                k.recip(rs2[:, :], sd[:, :])
                yc = ycv.next()
                for j in range(NJ):
                    t_ = fp.next()
                    k.tt(t_[:, :], ccb[j][:, :], mu[:, :], ALU.subtract)
                    k.tt(t_[:, :], t_[:, :], rs2[:, :], ALU.mult)
                    k.act(yc[:, j, :], t_[:, :], AF.Silu, bias=vcol("lnb", j), scale=vcol("lng", j))
                k.dma("sp", rows(yT, 0, CW, ti * W, W), yc.v())
    k.dma("sp", cst[0:128, :], pst.v())
    k.dma("sp", cst[128:256, :], hst.v())


def stage_B(k, cfg, c, dw, xres, xroff, yT, YL, PG, carr, xout, xlast, outT, is_moe, is_last):
    D, KC, NJ, T, W, CW, TH = cfg["D"], cfg["KC"], cfg["NJ"], cfg["T"], cfg["W"], cfg["CW"], cfg["TH"]
    off, DGW = cfg["off"], cfg["DGW"]
    NE = cfg["NE"] if is_moe else 1
    F = cfg["FE"] if is_moe else cfg["F"]
    NTH = TH // W
    k.ptr = k.persist
    vec = k.alloc("vec", [128, cfg["NV"]], F32)
    k.dma("sp", vec.v(), dw["vecs"].v())

    def vcol(name, i, n=1):
        return vec[:, off[name] + i: off[name] + i + n]

    flags = c["flags"]
    ones = c["ones"].v()
    ca = k.alloc("ca", [128, 8, NJ], F32)
    k.dma("sp", ca.v(), carr.v().re("(sa p) j -> p sa j", p=128))
    carry = k.alloc("carry", [128, NJ], F32)
    ctmp = k.alloc("ctmp", [128, NJ], F32)
    k.memset(carry.v(), 0.0)
    for s in range(3):
        k.tt(ctmp[:, :], ca[:, 2 * s, :], carry[:, :], ALU.mult)
        k.tt(ctmp[:, :], ctmp[:, :], ca[:, 2 * s + 1, :], ALU.add)
        k.tt(ctmp[:, :], ctmp[:, :], carry[:, :], ALU.subtract)
        k.stt(carry[:, :], ctmp[:, :], flags[:, s:s + 1], carry[:, :], ALU.mult, ALU.add)

    if is_moe:
        wr = k.alloc("wr", [128, KC, 8], F32)
        k.dma("sp", wr.v(), dw["w_router"].v().re("(kc p) e -> p kc e", p=128))
        lgT = k.alloc("lgT", [8, TH], F32)
        gwT = k.alloc("gwT", [8, TH], F32)
        smal = k.alloc("smal", [128, 64], F32)
    h2T = [k.alloc(f"h2T{i}", [128, TH], BF16) for i in range(KC)]
    acc = [k.alloc(f"acc{i}", [128, TH], F32) for i in range(KC)]
    base = k.ptr
    ps_ss = k.banks[6]

    for hh in range(2):
        k.barrier()
        k.ptr = base
        yt = k.alloc("yt", [128, 2 * NJ, W], BF16)
        ylp = Rot([k.alloc(f"yl{i}", [128, W], F32) for i in range(2)])
        pgp = Rot([k.alloc(f"pg{i}", [128, W], F32) for i in range(2)])
        wop = Rot([k.alloc(f"wo{i}", [128, 2 * NJ, DGW], BF16) for i in range(2)])
        sqp = Rot([k.alloc(f"sqb{i}", [128, W], BF16) for i in range(2)])
        rt = k.alloc("rtb", [128, W], F32)
        rstd = k.alloc("rstdb", [128, W], F32)
        hfp = Rot([k.alloc(f"hf{i}", [128, W], F32) for i in range(2)])
        for tt in range(NTH):
            col = hh * TH + tt * W
            lc = tt * W
            for d in range(KC):
                k.dma("sp", acc[d][:, lc:lc + W], xres[d * 128:(d + 1) * 128, xroff + col:xroff + col + W])
            k.dma("sp", yt[:, 0:NJ, :], rows(yT, 0, CW, col, W))
            for j in range(NJ):
                yl, pg = ylp.next(), pgp.next()
                k.dma("sp", yl[:, :], YL[j * 128:(j + 1) * 128, col:col + W])
                k.dma("sp", pg[:, :], PG[j * 128:(j + 1) * 128, col:col + W])
                k.stt(yt[:, NJ + j, :], pg[:, :], carry[:, j:j + 1], yl[:, :], ALU.mult, ALU.add)
            for dg in range(D // DGW):
                wo = wop.next()
                k.dma("pool", wo.v(), rows(dw["w_out"], 0, D, dg * DGW, DGW))
                for dc in range(DGW // 128):
                    d = dg * (DGW // 128) + dc
                    ps = k.bank()
                    for e in range(2 * NJ):
                        k.mm(ps[:, :W], wo[:, e, dc * 128:(dc + 1) * 128], yt[:, e, :], e == 0, e == 2 * NJ - 1)
                    k.tt(acc[d][:, lc:lc + W], ps[:, :W], acc[d][:, lc:lc + W], ALU.add)
                    sq = sqp.next()
                    k.act(sq[:, :], acc[d][:, lc:lc + W], AF.Square)
                    k.mm(ps_ss[:, :W], ones, sq[:, :], d == 0, d == KC - 1)
            k.act(rt[:, :], ps_ss[:, :W], AF.Sqrt, bias=EPS, scale=1.0 / D)
            k.recip(rstd[:, :], rt[:, :])
            for kc in range(KC):
                k.stt(h2T[kc][:, lc:lc + W], acc[kc][:, lc:lc + W], vcol("ffnn", kc), rstd[:, :],
                      ALU.mult, ALU.mult)
            if is_moe:
                pl = k.bank()
                for kc in range(KC):
                    hf = hfp.next()
                    k.stt(hf[:, :], acc[kc][:, lc:lc + W], vcol("ffnn", kc), rstd[:, :], ALU.mult, ALU.mult)
                    k.mm(pl[0:8, :W], wr[:, kc, :], hf[:, :], kc == 0, kc == KC - 1)
                k.act(lgT[0:8, lc:lc + W], pl[0:8, :W], AF.Identity)
        if is_moe:
            for blk in range(TH // 128):
                sl = slice(blk * 128, (blk + 1) * 128)
                lg, m8, msk, nl1 = smal[:, 0:8], smal[:, 8:16], smal[:, 16:24], smal[:, 24:25]
                ex, e2, den, rden, gw = smal[:, 32:40], smal[:, 25:26], smal[:, 26:27], smal[:, 27:28], smal[:, 40:48]
                pl = k.bank()
                k.mm(pl[:, 0:8], lgT[0:8, sl], c["ident"][0:8, 0:8], True, True)
                k.act(lg, pl[:, 0:8], AF.Identity)
                k.vmax(m8, lg)
                k.ts(msk, lg, m8[:, 1:2], None, ALU.is_ge)
                k.ts(nl1, m8[:, 0:1], -1.0, None, ALU.mult)
                k.act(ex, lg, AF.Exp, bias=nl1)
                k.act(e2, m8[:, 1:2], AF.Exp, bias=nl1)
                k.ts(den, e2, 1.0, None, ALU.add)
                k.recip(rden, den)
                k.stt(gw, ex, rden, msk, ALU.mult, ALU.mult)
                pt = k.bank()
                k.mm(pt[0:8, 0:128], gw, c["ident"][:, :], True, True)
                k.act(gwT[0:8, sl], pt[0:8, 0:128], AF.Identity)

        k.barrier()
        k.ptr = base
        wgp = Rot([k.alloc(f"wg{i}", [128, KC, 256], BF16) for i in range(2)])
        wup = Rot([k.alloc(f"wu{i}", [128, KC, 256], BF16) for i in range(2)])
        wdp = Rot([k.alloc(f"wd{i}", [128, 4, D], BF16) for i in range(2)])
        actT = [k.alloc(f"actT{i}", [128, TH], BF16) for i in range(4)]
        sgp = Rot([k.alloc(f"sg{i}", [128, W], F32) for i in range(2)])
        tmpp = Rot([k.alloc(f"tm{i}", [128, W], F32) for i in range(2)])
        if is_moe:
            gwBp = Rot([k.alloc(f"gwB{i}", [128, TH], F32) for i in range(2)])
        for ex_i in range(NE):
            if is_moe:
                wg_d, wu_d, wd_d = (V(dw[n], dw[n].ap[ex_i]) for n in ("wg", "wu", "wd"))
                gwB = gwBp.next()
                for tt in range(NTH):
                    pb = k.bank()
                    k.mm(pb[:, :W], c["sel"][0:8, ex_i * 128:(ex_i + 1) * 128], gwT[0:8, tt * W:(tt + 1) * W],
                         True, True)
                    k.act(gwB[:, tt * W:(tt + 1) * W], pb[:, :W], AF.Identity)
            else:
                wg_d, wu_d, wd_d = (dw[n].v() for n in ("wg", "wu", "wd"))
            for g in range(F // 512):
                wd_t = wdp.next()
                k.dma("pool", wd_t.v(), wd_d[g * 512:(g + 1) * 512, :].re("(f p) d -> p f d", p=128))
                for pr in range(2):
                    wg_t, wu_t = wgp.next(), wup.next()
                    f0 = g * 512 + pr * 256
                    k.dma("pool", wg_t.v(), wg_d[:, f0:f0 + 256].re("(kc p) f -> p kc f", p=128))
                    k.dma("pool", wu_t.v(), wu_d[:, f0:f0 + 256].re("(kc p) f -> p kc f", p=128))
                    for fc2 in range(2):
                        fc = pr * 2 + fc2
                        for tt in range(NTH):
                            cs = slice(tt * W, (tt + 1) * W)
                            pg_, pu_ = k.bank(), k.bank()
                            for kc in range(KC):
                                k.mm(pg_[:, :W], wg_t[:, kc, fc2 * 128:(fc2 + 1) * 128], h2T[kc][:, cs], kc == 0,
                                     kc == KC - 1)
                            for kc in range(KC):
                                k.mm(pu_[:, :W], wu_t[:, kc, fc2 * 128:(fc2 + 1) * 128], h2T[kc][:, cs], kc == 0,
                                     kc == KC - 1)
                            sg = sgp.next()
                            k.act(sg[:, :], pg_[:, :W], AF.Silu)
                            if is_moe:
                                tm = tmpp.next()
                                k.tt(tm[:, :], pu_[:, :W], sg[:, :], ALU.mult)
                                k.tt(actT[fc][:, cs], tm[:, :], gwB[:, cs], ALU.mult)
                            else:
                                k.tt(actT[fc][:, cs], pu_[:, :W], sg[:, :], ALU.mult)
                for d in range(KC):
                    for tt in range(NTH):
                        cs = slice(tt * W, (tt + 1) * W)
                        pd = k.bank()
                        for fc in range(4):
                            k.mm(pd[:, :W], wd_t[:, fc, d * 128:(d + 1) * 128], actT[fc][:, cs], fc == 0, fc == 3)
                        k.tt(acc[d][:, cs], pd[:, :W], acc[d][:, cs], ALU.add)
        if not is_last:
            for d in range(KC):
                k.dma("sp", xout[d * 128:(d + 1) * 128, hh * TH:(hh + 1) * TH], acc[d][:, :])
                if hh == 1:
                    k.dma("sp", xlast[d * 128:(d + 1) * 128, :], acc[d][:, TH - HALO:TH])
        else:
            k.barrier()
            k.ptr = base
            sqf = Rot([k.alloc(f"sqf{i}", [128, W], BF16) for i in range(2)])
            otp = Rot([k.alloc(f"ot{i}", [128, W], F32) for i in range(2)])
            rtf = k.alloc("rtf", [128, W], F32)
            rsf = k.alloc("rsf", [128, W], F32)
            for tt in range(NTH):
                cs = slice(tt * W, (tt + 1) * W)
                for d in range(KC):
                    sq = sqf.next()
                    k.act(sq[:, :], acc[d][:, cs], AF.Square)
                    k.mm(ps_ss[:, :W], ones, sq[:, :], d == 0, d == KC - 1)
                k.act(rtf[:, :], ps_ss[:, :W], AF.Sqrt, bias=EPS, scale=1.0 / D)
                k.recip(rsf[:, :], rtf[:, :])
                for d in range(KC):
                    ot = otp.next()
                    k.stt(ot[:, :], acc[d][:, cs], vcol("finn", d), rsf[:, :], ALU.mult, ALU.mult)
                    k.dma("sp", outT[d * 128:(d + 1) * 128, hh * TH + tt * W: hh * TH + (tt + 1) * W], ot[:, :])


ARENA_WORDS = 52800


def build_program(cfg, stages):
    nc = bass.Bass("TRN2", target_bir_lowering=False)
    D, KC, NJ, T, CW = cfg["D"], cfg["KC"], cfg["NJ"], cfg["T"], cfg["CW"]
    fused = len(stages) == 4
    groups = [[0, 1, 2, 3], [4, 5, 6, 7]]
    final_bufs = []
    with ExitStack() as st:
        k = K(nc, st, ARENA_WORDS)

        def ext_in(name, shape, dtype=F32):
            return k.dram(name, shape, dtype, "ExternalInput")

        def inter(name, shape, dtype, producer_stage, consumer_stages):
            if producer_stage in stages:
                if all(s in stages for s in consumer_stages):
                    b = k.dram(name, shape, dtype, "Internal")
                else:
                    b = k.dram(name, shape, dtype, "ExternalOutput")
                    final_bufs.append(b)
                return b
            if any(s in stages for s in consumer_stages):
                return ext_in(name, shape, dtype)
            return None

        din = {"ident": ext_in("ident", [128, 128]), "sel": ext_in("sel", [8, 1024]),
               "flags": ext_in("flags", [128, 8])}
        c = setup_consts(k, cfg, din)
        xTin = ext_in("xTin", [D, HALO + T]) if ("A0" in stages or "B0" in stages) else None
        t = {}
        for l in (0, 1):
            t[f"yT{l}"] = inter(f"yT{l}", [CW, T], BF16, f"A{l}", [f"B{l}"])
            t[f"YL{l}"] = inter(f"YL{l}", [CW, T], F32, f"A{l}", [f"B{l}"])
            t[f"PG{l}"] = inter(f"PG{l}", [CW, T], F32, f"A{l}", [f"B{l}"])
            if fused:
                t[f"cst{l}"] = k.dram(f"cst{l}", [256, NJ], F32, "Internal")
                t[f"carr{l}"] = k.dram(f"carr{l}", [1024, NJ], F32, "Internal")
            else:
                t[f"cst{l}"] = inter(f"cst{l}", [256, NJ], F32, f"A{l}", ["host"])
                t[f"carr{l}"] = ext_in(f"carr{l}", [1024, NJ]) if f"B{l}" in stages else None
        t["xT1"] = inter("xT1", [D, T], F32, "B0", ["A1", "B1"])
        if fused:
            t["xlast"] = k.dram("xlast", [D, HALO], F32, "Internal")
            t["xh1"] = k.dram("xhall", [4 * D, HALO], F32, "Internal")
        else:
            t["xlast"] = inter("xlast", [D, HALO], F32, "B0", ["host"])
            t["xh1"] = ext_in("xh1", [D, HALO]) if "A1" in stages else None
        if "B1" in stages:
            outT = k.dram("outT", [D, T], F32, "ExternalOutput")
            final_bufs.append(outT)

        def layer_w(l, names):
            return {n: ext_in(f"{n}{l}", shp, F32) for n, shp in names}

        for sname in stages:
            l = int(sname[1])
            k.barrier()
            if sname[0] == "A":
                dw = layer_w(l, [("vecs", [128, cfg["NV"]]), ("w_in", [D, cfg["EIN"]]),
                                 ("wa", [NJ, 128, 128]), ("wx", [NJ, 128, 128])])
                if l == 0:
                    stage_A(k, cfg, c, dw, xTin, HALO, xTin, 0, t["yT0"], t["YL0"], t["PG0"], t["cst0"])
                else:
                    stage_A(k, cfg, c, dw, t["xT1"], 0, t["xh1"], 0, t["yT1"], t["YL1"], t["PG1"], t["cst1"],
                            halo_gathered=fused)
                if fused:
                    k.collective(t[f"carr{l}"], t[f"cst{l}"], groups)
            else:
                names = [("vecsB", [128, cfg["NV"]]), ("w_out", [D, D])]
                if l == 0:
                    names += [("wg", [D, cfg["F"]]), ("wu", [D, cfg["F"]]), ("wd", [cfg["F"], D])]
                else:
                    names += [("w_router", [D, 8]), ("wg", [cfg["NE"], D, cfg["FE"]]),
                              ("wu", [cfg["NE"], D, cfg["FE"]]), ("wd", [cfg["NE"], cfg["FE"], D])]
                dw = layer_w(l, names)
                dw["vecs"] = dw["vecsB"]
                if l == 0:
                    stage_B(k, cfg, c, dw, xTin, HALO, t["yT0"], t["YL0"], t["PG0"], t["carr0"],
                            t["xT1"], t["xlast"], None, False, False)
                    if fused:
                        k.collective(t["xh1"], t["xlast"], groups)
                else:
                    stage_B(k, cfg, c, dw, t["xT1"], 0, t["yT1"], t["YL1"], t["PG1"], t["carr1"],
                            None, None, outT, True, True)
        k.emit(final_bufs)
        build_program.last_peak = k.peak
    return nc


def vec2d(v, n):
    return np.ascontiguousarray(np.asarray(v, np.float32).reshape(n, 128).T)


def pack_vecs(cfg, inp, l):
    KC, NJ = cfg["KC"], cfg["NJ"]
    cw = np.asarray(inp["conv_w"][l], np.float32)
    lw = np.asarray(inp["lru_conv_w"][l], np.float32)
    parts = [vec2d(inp["mix_norm"][l], KC), vec2d(inp["ffn_norm"][l], KC), vec2d(inp["final_norm"], KC),
             vec2d(inp["conv_b"][l], NJ), vec2d(inp["conv_ln_g"][l], NJ), vec2d(inp["conv_ln_b"][l], NJ),
             vec2d(inp["lru_conv_b"][l], NJ), vec2d(inp["lru_ba"][l], NJ), vec2d(inp["lru_bx"][l], NJ),
             vec2d(inp["lru_lambda"][l], NJ),
             cw.reshape(CK, NJ, 128).transpose(2, 1, 0).reshape(128, NJ * CK),
             lw.reshape(LK, NJ, 128).transpose(2, 1, 0).reshape(128, NJ * LK)]
    return np.ascontiguousarray(np.concatenate(parts, axis=1).astype(np.float32))


def host_consts():
    ident = np.eye(128, dtype=np.float32)
    sel = np.zeros((8, 8, 128), np.float32)
    for e in range(8):
        sel[e, e, :] = 1.0
    return ident, sel.reshape(8, 1024)


def core_flags(s):
    f = np.zeros((128, 8), np.float32)
    for j in range(3):
        f[:, j] = 1.0 if j < s else 0.0
    f[:, 3] = 1.0 if s == 0 else 0.0
    f[:, 4] = 0.0 if s == 0 else 1.0
    for j in range(3):
        f[:, 5 + j] = 1.0 if j == s - 1 else 0.0
    return f


_PROG_CACHE = {}


def get_prog(cfg, stages):
    key = (cfg["D"], cfg["T"], cfg["F"], cfg["FE"], cfg["W"], tuple(stages))
    if key not in _PROG_CACHE:
        _PROG_CACHE[key] = build_program(cfg, list(stages))
    return _PROG_CACHE[key]


def run_unfused(cfg, inp, n_cores=8):
    D, T, NJ = cfg["D"], cfg["T"], cfg["NJ"]
    x = np.asarray(inp["x"], np.float32)
    B, S, _ = x.shape
    nseg = S // T
    assert B * nseg == n_cores and nseg == 4
    ident, sel = host_consts()
    common = []
    for cidx in range(n_cores):
        b, s = divmod(cidx, nseg)
        xT = np.zeros((D, HALO + T), np.float32)
        lo = s * T - HALO
        if s == 0:
            xT[:, HALO:] = x[b, 0:T, :].T
        else:
            xT[:, :] = x[b, lo:lo + HALO + T, :].T
        common.append({"ident": ident, "sel": sel, "flags": core_flags(s), "xTin": xT})
    vecs = [pack_vecs(cfg, inp, l) for l in (0, 1)]
    f32 = lambda a: np.ascontiguousarray(np.asarray(a, np.float32))

    def wA(l):
        return {f"vecs{l}": vecs[l], f"w_in{l}": f32(inp["w_in"][l]), f"wa{l}": f32(inp["lru_wa"][l]),
                f"wx{l}": f32(inp["lru_wx"][l])}

    def wB(l):
        d = {f"vecsB{l}": vecs[l], f"w_out{l}": f32(inp["w_out"][l])}
        if l == 0:
            d.update({"wg0": f32(inp["dense_wg"][0]), "wu0": f32(inp["dense_wu"][0]), "wd0": f32(inp["dense_wd"][0])})
        else:
            d.update({"w_router1": f32(inp["w_router"][0]), "wg1": f32(inp["moe_wg"][0]),
                      "wu1": f32(inp["moe_wu"][0]), "wd1": f32(inp["moe_wd"][0])})
        return d

    def launch(stage, maps):
        nc = get_prog(cfg, [stage])
        res = run_bass_kernel_spmd(nc, maps, core_ids=list(range(n_cores)))
        return res.results

    def gather_carry(res, l):
        out = []
        for cidx in range(n_cores):
            b = cidx // nseg
            out.append(np.ascontiguousarray(np.concatenate([res[b * nseg + j][f"cst{l}"] for j in range(nseg)], 0)))
        return out

    keepA = ("ident", "sel", "flags")
    rA0 = launch("A0", [dict(common[i], **wA(0)) for i in range(n_cores)])
    carr0 = gather_carry(rA0, 0)
    mB0 = [dict(common[i], **wB(0), yT0=rA0[i]["yT0"], YL0=rA0[i]["YL0"], PG0=rA0[i]["PG0"], carr0=carr0[i])
           for i in range(n_cores)]
    rB0 = launch("B0", mB0)
    mA1 = []
    for i in range(n_cores):
        s = i % nseg
        xh = rB0[i - 1]["xlast"] if s > 0 else np.zeros((D, HALO), np.float32)
        m = {kk: common[i][kk] for kk in keepA}
        m.update(wA(1))
        m.update(xT1=rB0[i]["xT1"], xh1=np.ascontiguousarray(xh))
        mA1.append(m)
    rA1 = launch("A1", mA1)
    carr1 = gather_carry(rA1, 1)
    mB1 = []
    for i in range(n_cores):
        m = {kk: common[i][kk] for kk in keepA}
        m.update(wB(1))
        m.update(xT1=rB0[i]["xT1"], yT1=rA1[i]["yT1"], YL1=rA1[i]["YL1"], PG1=rA1[i]["PG1"], carr1=carr1[i])
        mB1.append(m)
    rB1 = launch("B1", mB1)
    out = np.empty((B, S, D), np.float32)
    for i in range(n_cores):
        b, s = divmod(i, nseg)
        out[b, s * T:(s + 1) * T, :] = rB1[i]["outT"].T
    return out


def run_fused(cfg, inp, n_cores=8):
    D, T = cfg["D"], cfg["T"]
    x = np.asarray(inp["x"], np.float32)
    B, S, _ = x.shape
    nseg = S // T
    assert B * nseg == n_cores and nseg == 4
    ident, sel = host_consts()
    f32 = lambda a: np.ascontiguousarray(np.asarray(a, np.float32))
    vecs = [pack_vecs(cfg, inp, l) for l in (0, 1)]
    shared = {"ident": ident, "sel": sel}
    for l in (0, 1):
        shared.update({f"vecs{l}": vecs[l], f"vecsB{l}": vecs[l], f"w_in{l}": f32(inp["w_in"][l]),
                       f"wa{l}": f32(inp["lru_wa"][l]), f"wx{l}": f32(inp["lru_wx"][l]),
                       f"w_out{l}": f32(inp["w_out"][l])})
    shared.update({"wg0": f32(inp["dense_wg"][0]), "wu0": f32(inp["dense_wu"][0]), "wd0": f32(inp["dense_wd"][0]),
                   "w_router1": f32(inp["w_router"][0]), "wg1": f32(inp["moe_wg"][0]),
                   "wu1": f32(inp["moe_wu"][0]), "wd1": f32(inp["moe_wd"][0])})
    maps = []
    for cidx in range(n_cores):
        b, s = divmod(cidx, nseg)
        xT = np.zeros((D, HALO + T), np.float32)
        if s == 0:
            xT[:, HALO:] = x[b, 0:T, :].T
        else:
            xT[:, :] = x[b, s * T - HALO:(s + 1) * T, :].T
        maps.append(dict(shared, flags=core_flags(s), xTin=xT))
    nc = get_prog(cfg, ["A0", "B0", "A1", "B1"])
    res = run_bass_kernel_spmd(nc, maps, core_ids=list(range(n_cores))).results
    out = np.empty((B, S, D), np.float32)
    for i in range(n_cores):
        b, s = divmod(i, nseg)
        out[b, s * T:(s + 1) * T, :] = res[i]["outT"].T
    return out


FULL_CFG = make_cfg()


def kernel(**inputs):
    return run_fused(FULL_CFG, inputs)
```

```python
import numpy as np
from contextlib import ExitStack
import ml_dtypes
import concourse.bass as bass
import concourse.mybir as mybir
from concourse.bass_utils import run_bass_kernel_spmd

F32 = mybir.dt.float32
BF16 = mybir.dt.bfloat16
AF = mybir.ActivationFunctionType
ALU = mybir.AluOpType
EPS = 1e-6
HALO = 32
CK = 31
LK = 4


def make_cfg(D=2048, T=2048, F=6144, FE=6144, NE=8, W=512):
    c = dict(D=D, KC=D // 128, CW=D // 2, NJ=D // 256, EIN=2 * D, T=T, W=W, F=F, FE=FE, NE=NE,
             TH=T // 2)
    c["PP"] = min(2, c["NJ"])
    c["XG"] = min(4, c["KC"])
    c["DGW"] = min(512, D)
    KC, NJ = c["KC"], c["NJ"]
    off = {}
    p = 0
    for name, n in (("mixn", KC), ("ffnn", KC), ("finn", KC), ("cb", NJ), ("lng", NJ), ("lnb", NJ),
                    ("lcb", NJ), ("ba", NJ), ("bx", NJ), ("lam", NJ), ("cw", NJ * CK), ("lw", NJ * LK)):
        off[name] = p
        p += n
    c["off"] = off
    c["NV"] = p
    return c


class Buf:
    _n = 0

    def __init__(self, ap, kind, name):
        self.ap, self.kind, self.name = ap, kind, name
        self.last_w = None
        self.reads = {}
        self.dcnt = 0
        Buf._n += 1
        self.id = Buf._n

    def __getitem__(self, idx):
        return V(self, self.ap[idx])

    def v(self):
        return V(self, self.ap)


class V:
    def __init__(self, buf, ap):
        self.buf, self.ap = buf, ap

    def __getitem__(self, idx):
        return V(self.buf, self.ap[idx])

    def re(self, s, **kw):
        return V(self.buf, self.ap.rearrange(s, **kw))


class Rot:
    def __init__(self, bufs):
        self.bufs, self.i = bufs, 0

    def next(self):
        b = self.bufs[self.i % len(self.bufs)]
        self.i += 1
        return b


COMPUTE = ("pe", "act", "dve", "pool")
STREAMS = ("pe", "act", "dve", "pool", "sp")


class K:
    def __init__(self, nc, st, arena_words):
        self.nc = nc
        self.arena = st.enter_context(nc.sbuf_tensor("arena", [128, arena_words], F32))[:]
        self.arena_words = arena_words
        self.ptr = 0
        self.ops = []
        self.cnt = {e: 0 for e in COMPUTE}
        self.waited = {e: {} for e in STREAMS}
        self.slot_cnt = []
        self.free_slots = []
        self.live_dbufs = []
        self.cbufs = {}
        self.banks = [Buf(st.enter_context(nc.psum_tensor(f"ps{i}", [128, 512], F32))[:], "psum", f"ps{i}")
                      for i in range(8)]
        self.rot = 0
        self.peak = 0

    def alloc(self, name, shape, dtype):
        n = 1
        for s in shape[1:]:
            n *= s
        words = (n + 1) // 2 if dtype == BF16 else n
        words = (words + 7) // 8 * 8
        off = self.ptr
        self.ptr += words
        self.peak = max(self.peak, self.ptr)
        assert self.ptr <= self.arena_words, f"SBUF arena overflow at {name}: {self.ptr} > {self.arena_words}"
        ap = self.arena[:, off:off + words]
        if dtype == BF16:
            ap = ap.bitcast(BF16)
        ap = ap[:, :n]
        if shape[0] < 128:
            ap = ap[0:shape[0]]
        if len(shape) == 3:
            ap = ap.rearrange("p (a b) -> p a b", a=shape[1])
        elif len(shape) == 4:
            ap = ap.rearrange("p (a b c) -> p a b c", a=shape[1], b=shape[2])
        return Buf(ap, "sbuf", name)

    def bank(self):
        b = self.banks[self.rot % 6]
        self.rot += 1
        return b

    def dram(self, name, shape, dtype, kind):
        return Buf(self.nc.dram_tensor(name, list(shape), dtype, kind=kind).ap(), "dram", name)

    def _collect(self, eng, reads, writes):
        need = {}

        def add(tok):
            if tok is not None:
                need[tok[0]] = max(need.get(tok[0], 0), tok[1])

        for b in reads:
            add(b.last_w)
        for b in writes:
            if b.kind != "dram":
                add(b.last_w)
            for kk, v in b.reads.items():
                add((kk, v))
        out = []
        for kk, v in need.items():
            if kk == eng and eng == "pe":
                continue
            if self.waited[eng].get(kk, 0) >= v:
                continue
            self.waited[eng][kk] = v
            out.append((kk, v))
        return out

    def op(self, eng, fn, reads, writes):
        reads = [r.buf for r in reads if isinstance(r, V)]
        writes = [w.buf for w in writes]
        waits = self._collect(eng, reads, writes)
        n = self.cnt[eng] + 1
        self.cnt[eng] = n
        self.ops.append((eng, fn, waits, (eng, 1)))
        for b in reads:
            b.reads[eng] = max(b.reads.get(eng, 0), n)
        for b in writes:
            b.last_w = (eng, n)
            b.reads = {}

    def dma(self, q, out, in_):
        waits = self._collect(q, [in_.buf], [out.buf])
        dst = out.buf
        if getattr(dst, "dslot", None) is None:
            if self.free_slots:
                dst.dslot = self.free_slots.pop()
            else:
                dst.dslot = len(self.slot_cnt)
                self.slot_cnt.append(0)
            self.live_dbufs.append(dst)
        slot = dst.dslot
        key = ("d", slot)
        self.slot_cnt[slot] += 16
        cntv = self.slot_cnt[slot]
        o_ap, i_ap = out.ap, in_.ap
        self.ops.append((q, lambda e: e.dma_start(out=o_ap, in_=i_ap), waits, (key, 16)))
        in_.buf.reads[key] = max(in_.buf.reads.get(key, 0), cntv)
        dst.last_w = (key, cntv)
        dst.reads = {}

    def collective(self, out_buf, in_buf, groups):
        waits = self._collect("pool", [in_buf], [out_buf])
        key = ("c", out_buf.id)
        self.cbufs[out_buf.id] = out_buf
        o_ap, i_ap = out_buf.ap, in_buf.ap
        self.ops.append(("pool", lambda e: e.collective_compute("AllGather", ALU.bypass, replica_groups=groups,
                                                                  ins=[i_ap], outs=[o_ap]), waits, (key, None)))
        in_buf.reads[key] = 1
        out_buf.last_w = (key, 1)
        out_buf.reads = {}

    def barrier(self):
        allw = [(e, self.cnt[e]) for e in COMPUTE if self.cnt[e] > 0]
        allw += [(("d", i), v) for i, v in enumerate(self.slot_cnt) if v > 0]
        allw += [(("c", b.id), 1) for b in self.cbufs.values()]
        for b in self.live_dbufs:
            self.free_slots.append(b.dslot)
            b.dslot = None
        self.live_dbufs = []
        for e in STREAMS:
            waits = []
            for kk, v in allw:
                if self.waited[e].get(kk, 0) < v:
                    self.waited[e][kk] = v
                    waits.append((kk, v))
            self.ops.append((e, None, waits, None))

    @staticmethod
    def _a(x):
        return x.ap if isinstance(x, V) else x

    def mm(self, out, lhsT, rhs, start, stop):
        o, l, r = out.ap, lhsT.ap, rhs.ap
        self.op("pe", lambda e: e.matmul(o, l, r, start=start, stop=stop), [lhsT, rhs], [out])

    def act(self, out, in_, func, bias=None, scale=None):
        o, i = out.ap, in_.ap
        kw = {}
        if bias is not None:
            kw["bias"] = self._a(bias)
        if scale is not None:
            kw["scale"] = self._a(scale)
        self.op("act", lambda e: e.activation(out=o, in_=i, func=func, **kw), [in_, bias, scale], [out])

    def tt(self, out, in0, in1, op, eng="dve"):
        o, a, b = out.ap, in0.ap, in1.ap
        self.op(eng, lambda e: e.tensor_tensor(out=o, in0=a, in1=b, op=op), [in0, in1], [out])

    def stt(self, out, in0, scalar, in1, op0, op1):
        o, a, s, b = out.ap, in0.ap, self._a(scalar), in1.ap
        self.op("dve", lambda e: e.scalar_tensor_tensor(out=o, in0=a, scalar=s, in1=b, op0=op0, op1=op1),
                [in0, scalar, in1], [out])

    def ts(self, out, in0, s1, s2, op0, op1=None, eng="dve"):
        o, a, x1, x2 = out.ap, in0.ap, self._a(s1), self._a(s2)
        if op1 is None:
            fn = lambda e: e.tensor_scalar(out=o, in0=a, scalar1=x1, scalar2=None, op0=op0)
        else:
            fn = lambda e: e.tensor_scalar(out=o, in0=a, scalar1=x1, scalar2=x2, op0=op0, op1=op1)
        self.op(eng, fn, [in0, s1, s2], [out])

    def scan(self, out, d0, d1, initial, op0, op1):
        o, a, b, i = out.ap, d0.ap, d1.ap, self._a(initial)
        self.op("dve", lambda e: e.tensor_tensor_scan(out=o, data0=a, data1=b, initial=i, op0=op0, op1=op1),
                [d0, d1, initial], [out])

    def copy(self, out, in_, eng="dve"):
        o, i = out.ap, in_.ap
        self.op(eng, lambda e: e.tensor_copy(out=o, in_=i), [in_], [out])

    def recip(self, out, in_):
        o, i = out.ap, in_.ap
        self.op("dve", lambda e: e.reciprocal(out=o, in_=i), [in_], [out])

    def memset(self, v, val, eng="dve"):
        a = v.ap
        self.op(eng, lambda e: e.memset(a, val), [], [v])

    def vmax(self, out, in_):
        o, i = out.ap, in_.ap
        self.op("dve", lambda e: e.max(out=o, in_=i), [in_], [out])

    def emit(self, final_bufs):
        nc = self.nc
        waits = [b.last_w for b in final_bufs if b.last_w is not None]
        self.ops.append(("sp", None, waits, None))
        with ExitStack() as st:
            sems = {}
            for e in COMPUTE:
                sems[e] = st.enter_context(nc.semaphore("s_" + e))
            print("free sems", nc.free_len(), "dma slots", len(self.slot_cnt))
            for i in range(len(self.slot_cnt)):
                sems[("d", i)] = st.enter_context(nc.semaphore(f"d{i}"))
            for b in self.cbufs.values():
                sems[("c", b.id)] = st.enter_context(nc.semaphore(f"c{b.id}"))
            block = st.enter_context(nc.Block())
            ops = self.ops

            def mk(name):
                def body(eng):
                    for (e, fn, waits, inc) in ops:
                        if e != name:
                            continue
                        for (kk, v) in waits:
                            eng.wait_ge(sems[kk], v)
                        if fn is None:
                            continue
                        ins = fn(eng)
                        if inc[1] is None:
                            ins.then_inc(sems[inc[0]])
                        else:
                            ins.then_inc(sems[inc[0]], inc[1])
                return body

            block.tensor(mk("pe"))
            block.scalar(mk("act"))
            block.vector(mk("dve"))
            block.gpsimd(mk("pool"))
            block.sync(mk("sp"))


def setup_consts(k, cfg, din):
    c = {}
    c["ones"] = k.alloc("ones", [128, 128], BF16)
    k.memset(c["ones"].v(), 1.0)
    c["zeros"] = k.alloc("zeros", [128, 512], F32)
    k.memset(c["zeros"].v(), 0.0)
    c["ident"] = k.alloc("ident", [128, 128], F32)
    k.dma("sp", c["ident"].v(), din["ident"].v())
    c["sel"] = k.alloc("sel", [8, 8 * 128], F32)
    k.dma("sp", c["sel"].v(), din["sel"].v())
    c["flags"] = k.alloc("flags", [128, 8], F32)
    k.dma("sp", c["flags"].v(), din["flags"].v())
    k.persist = k.ptr
    return c


def rows(buf, r0, nr, c0, nc_):
    return V(buf, buf.ap[r0:r0 + nr, c0:c0 + nc_].rearrange("(k p) t -> p k t", p=128))


def stage_A(k, cfg, c, dw, xmain, xoff, xhalo, xhoff, yT, YL, PG, cst, halo_gathered=False):
    D, KC, NJ, T, W, CW = cfg["D"], cfg["KC"], cfg["NJ"], cfg["T"], cfg["W"], cfg["CW"]
    PP, XG, off = cfg["PP"], cfg["XG"], cfg["off"]
    k.ptr = k.persist
    vec = k.alloc("vec", [128, cfg["NV"]], F32)
    k.dma("sp", vec.v(), dw["vecs"].v())
    wa = k.alloc("wa", [128, NJ, 128], BF16)
    wx = k.alloc("wx", [128, NJ, 128], BF16)
    k.dma("pool", wa.v(), dw["wa"].v().re("h i j -> i h j"))
    k.dma("pool", wx.v(), dw["wx"].v().re("h i j -> i h j"))

    def vcol(name, i, n=1):
        return vec[:, off[name] + i: off[name] + i + n]

    sm = k.alloc("sm", [128, 10 * NJ], F32)
    s_ = [sm[:, i * NJ:(i + 1) * NJ] for i in range(10)]
    e_, ln_, t1, msk, t2, spv, nsp, nsp2 = s_[0], s_[1], s_[2], s_[3], s_[4], s_[5], s_[6], s_[7]
    lam = vcol("lam", 0, NJ)
    k.act(e_, lam, AF.Exp, scale=-1.0)
    k.act(ln_, e_, AF.Ln, bias=1.0)
    k.ts(t1, e_, -1.0 / 3.0, 0.5, ALU.mult, ALU.add)
    k.tt(t1, t1, e_, ALU.mult)
    k.ts(t1, t1, -1.0, 1.0, ALU.mult, ALU.add)
    k.tt(t1, t1, e_, ALU.mult)
    k.ts(msk, e_, 0.05, None, ALU.is_lt)
    k.tt(t2, t1, ln_, ALU.subtract)
    k.tt(t2, t2, msk, ALU.mult)
    k.tt(spv, t2, ln_, ALU.add)
    k.ts(nsp, spv, -8.0, None, ALU.mult)
    k.ts(nsp2, spv, -16.0, None, ALU.mult)

    hst = k.alloc("hst", [128, NJ], F32)
    pst = k.alloc("pst", [128, NJ], F32)
    k.memset(hst.v(), 0.0)
    k.memset(pst.v(), 1.0)
    chalo = [k.alloc(f"chalo{j}", [128, HALO], BF16) for j in range(NJ)]
    rhalo = [k.alloc(f"rhalo{j}", [128, HALO], F32) for j in range(NJ)]

    xt = [k.alloc(f"xt{g}", [128, XG, W], F32) for g in range(KC // XG)]
    if halo_gathered:
        xhp = [k.alloc(f"xhp{i}", [128, XG, HALO], F32) for i in range(3)]
    hT = [k.alloc(f"hT{i}", [128, W], BF16) for i in range(KC)]
    xth = [k.alloc(f"xth{g}", [128, XG, HALO], F32) for g in range(KC // XG)]
    hTh = [k.alloc(f"hTh{i}", [128, HALO], BF16) for i in range(KC)]
    sqp = Rot([k.alloc(f"sq{i}", [128, W], BF16) for i in range(2)])
    bfp = Rot([k.alloc(f"bfp{i}", [128, W], BF16) for i in range(4)])
    wtA = Rot([k.alloc(f"wtA{i}", [128, KC, PP * 128], BF16) for i in range(2)])
    wtB = Rot([k.alloc(f"wtB{i}", [128, KC, PP * 128], BF16) for i in range(2)])
    cwp = Rot([k.alloc(f"cw{i}", [128, HALO + W], BF16) for i in range(2)])
    dgp = Rot([k.alloc(f"dg{i}", [128, CK, 128], BF16) for i in range(2)])
    identb = k.alloc("identb", [128, 128], BF16)
    k.copy(identb.v(), c["ident"].v())
    rwp = Rot([k.alloc(f"rw{i}", [128, HALO + W], F32) for i in range(2)])
    ccb = [k.alloc(f"cc{j}", [128, W], F32) for j in range(NJ)]
    fp = Rot([k.alloc(f"fp{i}", [128, W], F32) for i in range(20)])
    rstd = k.alloc("rstd", [128, W], F32)
    mu = k.alloc("mu", [128, W], F32)
    rs2 = k.alloc("rs2", [128, W], F32)
    ycv = Rot([k.alloc(f"ycv{i}", [128, NJ, W], BF16) for i in range(2)])
    ps_ss, ps_s1, ps_s2 = k.banks[6], k.banks[6], k.banks[7]
    ones = c["ones"].v()
    flags = c["flags"]
    w_in = dw["w_in"]

    def lru_part2(j, ti, gx, rc, ml, a_, G):
        bb = fp.next()
        k.tt(bb[:, :], gx[:, :], rc[:, :], ALU.mult)
        k.tt(bb[:, :], bb[:, :], ml[:, :], ALU.mult)
        hl, Pc = fp.next(), fp.next()
        k.scan(hl[:, :], a_[:, :], bb[:, :], hst[:, j:j + 1], ALU.mult, ALU.add)
        k.copy(hst[:, j:j + 1], hl[:, W - 1:W])
        k.scan(Pc[:, :], a_[:, :], c["zeros"][:, :W], pst[:, j:j + 1], ALU.mult, ALU.add)
        k.copy(pst[:, j:j + 1], Pc[:, W - 1:W])
        k.tt(hl[:, :], hl[:, :], G[:, :], ALU.mult)
        k.tt(Pc[:, :], Pc[:, :], G[:, :], ALU.mult)
        k.dma("sp", YL[j * 128:(j + 1) * 128, ti * W:(ti + 1) * W], hl[:, :])
        k.dma("sp", PG[j * 128:(j + 1) * 128, ti * W:(ti + 1) * W], Pc[:, :])

    pend = None
    grps = [[("halo", 0, HALO), ("main", 0, W)]] + [[("main", i, W)] for i in range(1, T // W)]
    for grp in grps:
      for kind, ti, w in grp:
        xt_, hT_ = (xth, hTh) if kind == "halo" else (xt, hT)
        for g in range(KC // XG):
            if kind == "halo" and halo_gathered:
                for jj in range(3):
                    k.dma("sp", xhp[jj].v(), rows(xhalo, jj * D + g * XG * 128, XG * 128, 0, w))
                k.ts(xt_[g][:, :, 0:w], xhp[0].v(), flags[:, 5:6], None, ALU.mult)
                k.stt(xt_[g][:, :, 0:w], xhp[1].v(), flags[:, 6:7], xt_[g][:, :, 0:w], ALU.mult, ALU.add)
                k.stt(xt_[g][:, :, 0:w], xhp[2].v(), flags[:, 7:8], xt_[g][:, :, 0:w], ALU.mult, ALU.add)
                continue
            if kind == "halo":
                src = rows(xhalo, g * XG * 128, XG * 128, xhoff, w)
            else:
                src = rows(xmain, g * XG * 128, XG * 128, xoff + ti * W, w)
            k.dma("sp", xt_[g][:, :, 0:w], src)
        for kc in range(KC):
            sq = sqp.next()
            k.act(sq[:, :w], xt_[kc // XG][:, kc % XG, 0:w], AF.Square)
            k.mm(ps_ss[:, :w], ones, sq[:, :w], kc == 0, kc == KC - 1)
        rt = fp.next()
        k.act(rt[:, :w], ps_ss[:, :w], AF.Sqrt, bias=EPS, scale=1.0 / D)
        k.recip(rstd[:, :w], rt[:, :w])
        for kc in range(KC):
            k.stt(hT_[kc][:, :w], xt_[kc // XG][:, kc % XG, 0:w], vcol("mixn", kc), rstd[:, :w],
                  ALU.mult, ALU.mult)

      for branch in (0, 1):
            for q in range(NJ // PP):
                A, B = wtA.next(), wtB.next()
                ca0 = branch * 2 * CW + q * PP * 128
                cb0 = ca0 + CW
                k.dma("pool", A.v(), rows(w_in, 0, D, ca0, PP * 128))
                k.dma("pool", B.v(), rows(w_in, 0, D, cb0, PP * 128))
                for pr in range(PP):
                    j = q * PP + pr
                    for kind, ti, w in grp:
                        hT_ = hTh if kind == "halo" else hT
                        psa, psb = k.bank(), k.bank()
                        for kc in range(KC):
                            k.mm(psa[:, :w], A[:, kc, pr * 128:(pr + 1) * 128], hT_[kc][:, :w], kc == 0, kc == KC - 1)
                        need_b = not (branch == 1 and kind == "halo")
                        if need_b:
                            for kc in range(KC):
                                k.mm(psb[:, :w], B[:, kc, pr * 128:(pr + 1) * 128], hT_[kc][:, :w], kc == 0,
                                     kc == KC - 1)
                        if branch == 0:
                            sgt = fp.next()
                            k.act(sgt[:, :w], psb[:, :w], AF.Sigmoid)
                            if kind == "halo":
                                k.tt(chalo[j][:, :], psa[:, :w], sgt[:, :w], ALU.mult)
                                continue
                            cw = cwp.next()
                            k.copy(cw[:, 0:HALO], chalo[j][:, :])
                            k.tt(cw[:, HALO:HALO + W], psa[:, :W], sgt[:, :W], ALU.mult)
                            dg = dgp.next()
                            for kk in range(CK):
                                k.ts(dg[:, kk, :], identb[:, :], vcol("cw", j * CK + kk), None, ALU.mult)
                            pcv = k.bank()
                            for kk in range(CK):
                                k.mm(pcv[:, :W], dg[:, kk, :], cw[:, 2 + kk:2 + kk + W], kk == 0, kk == CK - 1)
                            k.copy(chalo[j][:, :], cw[:, W:W + HALO])
                            cc = ccb[j]
                            k.act(cc[:, :], pcv[:, :W], AF.Identity, bias=vcol("cb", j))
                            b1, b2 = bfp.next(), bfp.next()
                            k.act(b1[:, :], cc[:, :], AF.Identity)
                            k.act(b2[:, :], cc[:, :], AF.Square)
                            k.mm(ps_s1[:, :W], ones, b1[:, :], j == 0, j == NJ - 1)
                            k.mm(ps_s2[:, :W], ones, b2[:, :], j == 0, j == NJ - 1)
                        else:
                            if kind == "halo":
                                k.act(rhalo[j][:, :], psa[:, :w], AF.Identity)
                                continue
                            rw = rwp.next()
                            k.copy(rw[:, 0:HALO], rhalo[j][:, :])
                            k.act(rw[:, HALO:HALO + W], psa[:, :W], AF.Identity)
                            G = fp.next()
                            k.act(G[:, :], psb[:, :W], AF.Gelu_apprx_tanh)
                            rc = fp.next()
                            k.ts(rc[:, :], rw[:, HALO - 3:HALO - 3 + W], vcol("lw", j * LK), vcol("lcb", j),
                                 ALU.mult, ALU.add)
                            for kk in range(1, LK):
                                k.stt(rc[:, :], rw[:, HALO - 3 + kk:HALO - 3 + kk + W], vcol("lw", j * LK + kk),
                                      rc[:, :], ALU.mult, ALU.add)
                            k.copy(rhalo[j][:, :], rw[:, W:W + HALO])
                            rcb = bfp.next()
                            k.act(rcb[:, :], rc[:, :], AF.Identity)
                            pga, pgx = k.bank(), k.bank()
                            k.mm(pga[:, :W], wa[:, j, :], rcb[:, :], True, True)
                            k.mm(pgx[:, :W], wx[:, j, :], rcb[:, :], True, True)
                            ga, gx, a_, a2, ml = fp.next(), fp.next(), fp.next(), fp.next(), fp.next()
                            k.act(ga[:, :], pga[:, :W], AF.Sigmoid, bias=vcol("ba", j))
                            k.act(gx[:, :], pgx[:, :W], AF.Sigmoid, bias=vcol("bx", j))
                            k.act(a_[:, :], ga[:, :], AF.Exp, scale=nsp[:, j:j + 1])
                            k.act(a2[:, :], ga[:, :], AF.Exp, scale=nsp2[:, j:j + 1])
                            k.act(ml[:, :], a2[:, :], AF.Sqrt, bias=1.0, scale=-1.0)
                            if ti == 0:
                                k.ts(ml[:, 0:1], ml[:, 0:1], flags[:, 4:5], flags[:, 3:4], ALU.mult, ALU.add)
                            if pend is not None:
                                lru_part2(*pend)
                            pend = (j, ti, gx, rc, ml, a_, G)
            if branch == 1 and pend is not None:
                lru_part2(*pend)
                pend = None
            if branch == 0:
                ti = grp[-1][1]
                mu2, var, sd = fp.next(), fp.next(), fp.next()
                k.act(mu[:, :], ps_s1[:, :W], AF.Identity, scale=1.0 / CW)
                k.tt(mu2[:, :], mu[:, :], mu[:, :], ALU.mult)
                k.stt(var[:, :], ps_s2[:, :W], 1.0 / CW, mu2[:, :], ALU.mult, ALU.subtract)
                k.act(sd[:, :], var[:, :], AF.Sqrt, bias=EPS)
                k.recip(rs2[:, :], sd[:, :])
                yc = ycv.next()
                for j in range(NJ):
                    t_ = fp.next()
                    k.tt(t_[:, :], ccb[j][:, :], mu[:, :], ALU.subtract)
                    k.tt(t_[:, :], t_[:, :], rs2[:, :], ALU.mult)
                    k.act(yc[:, j, :], t_[:, :], AF.Silu, bias=vcol("lnb", j), scale=vcol("lng", j))
                k.dma("sp", rows(yT, 0, CW, ti * W, W), yc.v())
    k.dma("sp", cst[0:128, :], pst.v())
    k.dma("sp", cst[128:256, :], hst.v())


def stage_B(k, cfg, c, dw, xres, xroff, yT, YL, PG, carr, xout, xlast, outT, is_moe, is_last):
    D, KC, NJ, T, W, CW, TH = cfg["D"], cfg["KC"], cfg["NJ"], cfg["T"], cfg["W"], cfg["CW"], cfg["TH"]
    off, DGW = cfg["off"], cfg["DGW"]
    NE = cfg["NE"] if is_moe else 1
    F = cfg["FE"] if is_moe else cfg["F"]
    NTH = TH // W
    k.ptr = k.persist
    vec = k.alloc("vec", [128, cfg["NV"]], F32)
    k.dma("sp", vec.v(), dw["vecs"].v())

    def vcol(name, i, n=1):
        return vec[:, off[name] + i: off[name] + i + n]

    flags = c["flags"]
    ones = c["ones"].v()
    ca = k.alloc("ca", [128, 8, NJ], F32)
    k.dma("sp", ca.v(), carr.v().re("(sa p) j -> p sa j", p=128))
    carry = k.alloc("carry", [128, NJ], F32)
    ctmp = k.alloc("ctmp", [128, NJ], F32)
    k.memset(carry.v(), 0.0)
    for s in range(3):
        k.tt(ctmp[:, :], ca[:, 2 * s, :], carry[:, :], ALU.mult)
        k.tt(ctmp[:, :], ctmp[:, :], ca[:, 2 * s + 1, :], ALU.add)
        k.tt(ctmp[:, :], ctmp[:, :], carry[:, :], ALU.subtract)
        k.stt(carry[:, :], ctmp[:, :], flags[:, s:s + 1], carry[:, :], ALU.mult, ALU.add)

    if is_moe:
        wr = k.alloc("wr", [128, KC, 8], F32)
        k.dma("sp", wr.v(), dw["w_router"].v().re("(kc p) e -> p kc e", p=128))
        lgT = k.alloc("lgT", [8, TH], F32)
        gwT = k.alloc("gwT", [8, TH], F32)
        smal = k.alloc("smal", [128, 64], F32)
    h2T = [k.alloc(f"h2T{i}", [128, TH], BF16) for i in range(KC)]
    acc = [k.alloc(f"acc{i}", [128, TH], F32) for i in range(KC)]
    base = k.ptr
    ps_ss = k.banks[6]

    for hh in range(2):
        k.barrier()
        k.ptr = base
        yt = k.alloc("yt", [128, 2 * NJ, W], BF16)
        ylp = Rot([k.alloc(f"yl{i}", [128, W], F32) for i in range(2)])
        pgp = Rot([k.alloc(f"pg{i}", [128, W], F32) for i in range(2)])
        wop = Rot([k.alloc(f"wo{i}", [128, 2 * NJ, DGW], BF16) for i in range(2)])
        sqp = Rot([k.alloc(f"sqb{i}", [128, W], BF16) for i in range(2)])
        rt = k.alloc("rtb", [128, W], F32)
        rstd = k.alloc("rstdb", [128, W], F32)
        hfp = Rot([k.alloc(f"hf{i}", [128, W], F32) for i in range(2)])
        for tt in range(NTH):
            col = hh * TH + tt * W
            lc = tt * W
            for d in range(KC):
                k.dma("sp", acc[d][:, lc:lc + W], xres[d * 128:(d + 1) * 128, xroff + col:xroff + col + W])
            k.dma("sp", yt[:, 0:NJ, :], rows(yT, 0, CW, col, W))
            for j in range(NJ):
                yl, pg = ylp.next(), pgp.next()
                k.dma("sp", yl[:, :], YL[j * 128:(j + 1) * 128, col:col + W])
                k.dma("sp", pg[:, :], PG[j * 128:(j + 1) * 128, col:col + W])
                k.stt(yt[:, NJ + j, :], pg[:, :], carry[:, j:j + 1], yl[:, :], ALU.mult, ALU.add)
            for dg in range(D // DGW):
                wo = wop.next()
                k.dma("pool", wo.v(), rows(dw["w_out"], 0, D, dg * DGW, DGW))
                for dc in range(DGW // 128):
                    d = dg * (DGW // 128) + dc
                    ps = k.bank()
                    for e in range(2 * NJ):
                        k.mm(ps[:, :W], wo[:, e, dc * 128:(dc + 1) * 128], yt[:, e, :], e == 0, e == 2 * NJ - 1)
                    k.tt(acc[d][:, lc:lc + W], ps[:, :W], acc[d][:, lc:lc + W], ALU.add)
                    sq = sqp.next()
                    k.act(sq[:, :], acc[d][:, lc:lc + W], AF.Square)
                    k.mm(ps_ss[:, :W], ones, sq[:, :], d == 0, d == KC - 1)
            k.act(rt[:, :], ps_ss[:, :W], AF.Sqrt, bias=EPS, scale=1.0 / D)
            k.recip(rstd[:, :], rt[:, :])
            for kc in range(KC):
                k.stt(h2T[kc][:, lc:lc + W], acc[kc][:, lc:lc + W], vcol("ffnn", kc), rstd[:, :],
                      ALU.mult, ALU.mult)
            if is_moe:
                pl = k.bank()
                for kc in range(KC):
                    hf = hfp.next()
                    k.stt(hf[:, :], acc[kc][:, lc:lc + W], vcol("ffnn", kc), rstd[:, :], ALU.mult, ALU.mult)
                    k.mm(pl[0:8, :W], wr[:, kc, :], hf[:, :], kc == 0, kc == KC - 1)
                k.act(lgT[0:8, lc:lc + W], pl[0:8, :W], AF.Identity)
        if is_moe:
            for blk in range(TH // 128):
                sl = slice(blk * 128, (blk + 1) * 128)
                lg, m8, msk, nl1 = smal[:, 0:8], smal[:, 8:16], smal[:, 16:24], smal[:, 24:25]
                ex, e2, den, rden, gw = smal[:, 32:40], smal[:, 25:26], smal[:, 26:27], smal[:, 27:28], smal[:, 40:48]
                pl = k.bank()
                k.mm(pl[:, 0:8], lgT[0:8, sl], c["ident"][0:8, 0:8], True, True)
                k.act(lg, pl[:, 0:8], AF.Identity)
                k.vmax(m8, lg)
                k.ts(msk, lg, m8[:, 1:2], None, ALU.is_ge)
                k.ts(nl1, m8[:, 0:1], -1.0, None, ALU.mult)
                k.act(ex, lg, AF.Exp, bias=nl1)
                k.act(e2, m8[:, 1:2], AF.Exp, bias=nl1)
                k.ts(den, e2, 1.0, None, ALU.add)
                k.recip(rden, den)
                k.stt(gw, ex, rden, msk, ALU.mult, ALU.mult)
                pt = k.bank()
                k.mm(pt[0:8, 0:128], gw, c["ident"][:, :], True, True)
                k.act(gwT[0:8, sl], pt[0:8, 0:128], AF.Identity)

        k.barrier()
        k.ptr = base
        wgp = Rot([k.alloc(f"wg{i}", [128, KC, 256], BF16) for i in range(2)])
        wup = Rot([k.alloc(f"wu{i}", [128, KC, 256], BF16) for i in range(2)])
        wdp = Rot([k.alloc(f"wd{i}", [128, 4, D], BF16) for i in range(2)])
        actT = [k.alloc(f"actT{i}", [128, TH], BF16) for i in range(4)]
        sgp = Rot([k.alloc(f"sg{i}", [128, W], F32) for i in range(2)])
        tmpp = Rot([k.alloc(f"tm{i}", [128, W], F32) for i in range(2)])
        if is_moe:
            gwBp = Rot([k.alloc(f"gwB{i}", [128, TH], F32) for i in range(2)])
        for ex_i in range(NE):
            if is_moe:
                wg_d, wu_d, wd_d = (V(dw[n], dw[n].ap[ex_i]) for n in ("wg", "wu", "wd"))
                gwB = gwBp.next()
                for tt in range(NTH):
                    pb = k.bank()
                    k.mm(pb[:, :W], c["sel"][0:8, ex_i * 128:(ex_i + 1) * 128], gwT[0:8, tt * W:(tt + 1) * W],
                         True, True)
                    k.act(gwB[:, tt * W:(tt + 1) * W], pb[:, :W], AF.Identity)
            else:
                wg_d, wu_d, wd_d = (dw[n].v() for n in ("wg", "wu", "wd"))
            for g in range(F // 512):
                wd_t = wdp.next()
                k.dma("pool", wd_t.v(), wd_d[g * 512:(g + 1) * 512, :].re("(f p) d -> p f d", p=128))
                for pr in range(2):
                    wg_t, wu_t = wgp.next(), wup.next()
                    f0 = g * 512 + pr * 256
                    k.dma("pool", wg_t.v(), wg_d[:, f0:f0 + 256].re("(kc p) f -> p kc f", p=128))
                    k.dma("pool", wu_t.v(), wu_d[:, f0:f0 + 256].re("(kc p) f -> p kc f", p=128))
                    for fc2 in range(2):
                        fc = pr * 2 + fc2
                        for tt in range(NTH):
                            cs = slice(tt * W, (tt + 1) * W)
                            pg_, pu_ = k.bank(), k.bank()
                            for kc in range(KC):
                                k.mm(pg_[:, :W], wg_t[:, kc, fc2 * 128:(fc2 + 1) * 128], h2T[kc][:, cs], kc == 0,
                                     kc == KC - 1)
                            for kc in range(KC):
                                k.mm(pu_[:, :W], wu_t[:, kc, fc2 * 128:(fc2 + 1) * 128], h2T[kc][:, cs], kc == 0,
                                     kc == KC - 1)
                            sg = sgp.next()
                            k.act(sg[:, :], pg_[:, :W], AF.Silu)
                            if is_moe:
                                tm = tmpp.next()
                                k.tt(tm[:, :], pu_[:, :W], sg[:, :], ALU.mult)
                                k.tt(actT[fc][:, cs], tm[:, :], gwB[:, cs], ALU.mult)
                            else:
                                k.tt(actT[fc][:, cs], pu_[:, :W], sg[:, :], ALU.mult)
                for d in range(KC):
                    for tt in range(NTH):
                        cs = slice(tt * W, (tt + 1) * W)
                        pd = k.bank()
                        for fc in range(4):
                            k.mm(pd[:, :W], wd_t[:, fc, d * 128:(d + 1) * 128], actT[fc][:, cs], fc == 0, fc == 3)
                        k.tt(acc[d][:, cs], pd[:, :W], acc[d][:, cs], ALU.add)
        if not is_last:
            for d in range(KC):
                k.dma("sp", xout[d * 128:(d + 1) * 128, hh * TH:(hh + 1) * TH], acc[d][:, :])
                if hh == 1:
                    k.dma("sp", xlast[d * 128:(d + 1) * 128, :], acc[d][:, TH - HALO:TH])
        else:
            k.barrier()
            k.ptr = base
            sqf = Rot([k.alloc(f"sqf{i}", [128, W], BF16) for i in range(2)])
            otp = Rot([k.alloc(f"ot{i}", [128, W], F32) for i in range(2)])
            rtf = k.alloc("rtf", [128, W], F32)
            rsf = k.alloc("rsf", [128, W], F32)
            for tt in range(NTH):
                cs = slice(tt * W, (tt + 1) * W)
                for d in range(KC):
                    sq = sqf.next()
                    k.act(sq[:, :], acc[d][:, cs], AF.Square)
                    k.mm(ps_ss[:, :W], ones, sq[:, :], d == 0, d == KC - 1)
                k.act(rtf[:, :], ps_ss[:, :W], AF.Sqrt, bias=EPS, scale=1.0 / D)
                k.recip(rsf[:, :], rtf[:, :])
                for d in range(KC):
                    ot = otp.next()
                    k.stt(ot[:, :], acc[d][:, cs], vcol("finn", d), rsf[:, :], ALU.mult, ALU.mult)
                    k.dma("sp", outT[d * 128:(d + 1) * 128, hh * TH + tt * W: hh * TH + (tt + 1) * W], ot[:, :])


ARENA_WORDS = 52800


def build_program(cfg, stages):
    nc = bass.Bass("TRN2", target_bir_lowering=False)
    D, KC, NJ, T, CW = cfg["D"], cfg["KC"], cfg["NJ"], cfg["T"], cfg["CW"]
    fused = len(stages) == 4
    groups = [[0, 1, 2, 3], [4, 5, 6, 7]]
    final_bufs = []
    with ExitStack() as st:
        k = K(nc, st, ARENA_WORDS)

        def ext_in(name, shape, dtype=F32):
            return k.dram(name, shape, dtype, "ExternalInput")

        def inter(name, shape, dtype, producer_stage, consumer_stages):
            if producer_stage in stages:
                if all(s in stages for s in consumer_stages):
                    b = k.dram(name, shape, dtype, "Internal")
                else:
                    b = k.dram(name, shape, dtype, "ExternalOutput")
                    final_bufs.append(b)
                return b
            if any(s in stages for s in consumer_stages):
                return ext_in(name, shape, dtype)
            return None

        din = {"ident": ext_in("ident", [128, 128]), "sel": ext_in("sel", [8, 1024]),
               "flags": ext_in("flags", [128, 8])}
        c = setup_consts(k, cfg, din)
        xTin = ext_in("xTin", [D, HALO + T]) if ("A0" in stages or "B0" in stages) else None
        t = {}
        for l in (0, 1):
            t[f"yT{l}"] = inter(f"yT{l}", [CW, T], BF16, f"A{l}", [f"B{l}"])
            t[f"YL{l}"] = inter(f"YL{l}", [CW, T], F32, f"A{l}", [f"B{l}"])
            t[f"PG{l}"] = inter(f"PG{l}", [CW, T], F32, f"A{l}", [f"B{l}"])
            if fused:
                t[f"cst{l}"] = k.dram(f"cst{l}", [256, NJ], F32, "Internal")
                t[f"carr{l}"] = k.dram(f"carr{l}", [1024, NJ], F32, "Internal")
            else:
                t[f"cst{l}"] = inter(f"cst{l}", [256, NJ], F32, f"A{l}", ["host"])
                t[f"carr{l}"] = ext_in(f"carr{l}", [1024, NJ]) if f"B{l}" in stages else None
        t["xT1"] = inter("xT1", [D, T], F32, "B0", ["A1", "B1"])
        if fused:
            t["xlast"] = k.dram("xlast", [D, HALO], F32, "Internal")
            t["xh1"] = k.dram("xhall", [4 * D, HALO], F32, "Internal")
        else:
            t["xlast"] = inter("xlast", [D, HALO], F32, "B0", ["host"])
            t["xh1"] = ext_in("xh1", [D, HALO]) if "A1" in stages else None
        if "B1" in stages:
            outT = k.dram("outT", [D, T], F32, "ExternalOutput")
            final_bufs.append(outT)

        def layer_w(l, names):
            return {n: ext_in(f"{n}{l}", shp, F32) for n, shp in names}

        for sname in stages:
            l = int(sname[1])
            k.barrier()
            if sname[0] == "A":
                dw = layer_w(l, [("vecs", [128, cfg["NV"]]), ("w_in", [D, cfg["EIN"]]),
                                 ("wa", [NJ, 128, 128]), ("wx", [NJ, 128, 128])])
                if l == 0:
                    stage_A(k, cfg, c, dw, xTin, HALO, xTin, 0, t["yT0"], t["YL0"], t["PG0"], t["cst0"])
                else:
                    stage_A(k, cfg, c, dw, t["xT1"], 0, t["xh1"], 0, t["yT1"], t["YL1"], t["PG1"], t["cst1"],
                            halo_gathered=fused)
                if fused:
                    k.collective(t[f"carr{l}"], t[f"cst{l}"], groups)
            else:
                names = [("vecsB", [128, cfg["NV"]]), ("w_out", [D, D])]
                if l == 0:
                    names += [("wg", [D, cfg["F"]]), ("wu", [D, cfg["F"]]), ("wd", [cfg["F"], D])]
                else:
                    names += [("w_router", [D, 8]), ("wg", [cfg["NE"], D, cfg["FE"]]),
                              ("wu", [cfg["NE"], D, cfg["FE"]]), ("wd", [cfg["NE"], cfg["FE"], D])]
                dw = layer_w(l, names)
                dw["vecs"] = dw["vecsB"]
                if l == 0:
                    stage_B(k, cfg, c, dw, xTin, HALO, t["yT0"], t["YL0"], t["PG0"], t["carr0"],
                            t["xT1"], t["xlast"], None, False, False)
                    if fused:
                        k.collective(t["xh1"], t["xlast"], groups)
                else:
                    stage_B(k, cfg, c, dw, t["xT1"], 0, t["yT1"], t["YL1"], t["PG1"], t["carr1"],
                            None, None, outT, True, True)
        k.emit(final_bufs)
        build_program.last_peak = k.peak
    return nc


def vec2d(v, n):
    return np.ascontiguousarray(np.asarray(v, np.float32).reshape(n, 128).T)


def pack_vecs(cfg, inp, l):
    KC, NJ = cfg["KC"], cfg["NJ"]
    cw = np.asarray(inp["conv_w"][l], np.float32)
    lw = np.asarray(inp["lru_conv_w"][l], np.float32)
    parts = [vec2d(inp["mix_norm"][l], KC), vec2d(inp["ffn_norm"][l], KC), vec2d(inp["final_norm"], KC),
             vec2d(inp["conv_b"][l], NJ), vec2d(inp["conv_ln_g"][l], NJ), vec2d(inp["conv_ln_b"][l], NJ),
             vec2d(inp["lru_conv_b"][l], NJ), vec2d(inp["lru_ba"][l], NJ), vec2d(inp["lru_bx"][l], NJ),
             vec2d(inp["lru_lambda"][l], NJ),
             cw.reshape(CK, NJ, 128).transpose(2, 1, 0).reshape(128, NJ * CK),
             lw.reshape(LK, NJ, 128).transpose(2, 1, 0).reshape(128, NJ * LK)]
    return np.ascontiguousarray(np.concatenate(parts, axis=1).astype(np.float32))


def host_consts():
    ident = np.eye(128, dtype=np.float32)
    sel = np.zeros((8, 8, 128), np.float32)
    for e in range(8):
        sel[e, e, :] = 1.0
    return ident, sel.reshape(8, 1024)


def core_flags(s):
    f = np.zeros((128, 8), np.float32)
    for j in range(3):
        f[:, j] = 1.0 if j < s else 0.0
    f[:, 3] = 1.0 if s == 0 else 0.0
    f[:, 4] = 0.0 if s == 0 else 1.0
    for j in range(3):
        f[:, 5 + j] = 1.0 if j == s - 1 else 0.0
    return f


_PROG_CACHE = {}


def get_prog(cfg, stages):
    key = (cfg["D"], cfg["T"], cfg["F"], cfg["FE"], cfg["W"], tuple(stages))
    if key not in _PROG_CACHE:
        _PROG_CACHE[key] = build_program(cfg, list(stages))
    return _PROG_CACHE[key]


def run_unfused(cfg, inp, n_cores=8):
    D, T, NJ = cfg["D"], cfg["T"], cfg["NJ"]
    x = np.asarray(inp["x"], np.float32)
    B, S, _ = x.shape
    nseg = S // T
    assert B * nseg == n_cores and nseg == 4
    ident, sel = host_consts()
    common = []
    for cidx in range(n_cores):
        b, s = divmod(cidx, nseg)
        xT = np.zeros((D, HALO + T), np.float32)
        lo = s * T - HALO
        if s == 0:
            xT[:, HALO:] = x[b, 0:T, :].T
        else:
            xT[:, :] = x[b, lo:lo + HALO + T, :].T
        common.append({"ident": ident, "sel": sel, "flags": core_flags(s), "xTin": xT})
    vecs = [pack_vecs(cfg, inp, l) for l in (0, 1)]
    f32 = lambda a: np.ascontiguousarray(np.asarray(a, np.float32))

    def wA(l):
        return {f"vecs{l}": vecs[l], f"w_in{l}": f32(inp["w_in"][l]), f"wa{l}": f32(inp["lru_wa"][l]),
                f"wx{l}": f32(inp["lru_wx"][l])}

    def wB(l):
        d = {f"vecsB{l}": vecs[l], f"w_out{l}": f32(inp["w_out"][l])}
        if l == 0:
            d.update({"wg0": f32(inp["dense_wg"][0]), "wu0": f32(inp["dense_wu"][0]), "wd0": f32(inp["dense_wd"][0])})
        else:
            d.update({"w_router1": f32(inp["w_router"][0]), "wg1": f32(inp["moe_wg"][0]),
                      "wu1": f32(inp["moe_wu"][0]), "wd1": f32(inp["moe_wd"][0])})
        return d

    def launch(stage, maps):
        nc = get_prog(cfg, [stage])
        res = run_bass_kernel_spmd(nc, maps, core_ids=list(range(n_cores)))
        return res.results

    def gather_carry(res, l):
        out = []
        for cidx in range(n_cores):
            b = cidx // nseg
            out.append(np.ascontiguousarray(np.concatenate([res[b * nseg + j][f"cst{l}"] for j in range(nseg)], 0)))
        return out

    keepA = ("ident", "sel", "flags")
    rA0 = launch("A0", [dict(common[i], **wA(0)) for i in range(n_cores)])
    carr0 = gather_carry(rA0, 0)
    mB0 = [dict(common[i], **wB(0), yT0=rA0[i]["yT0"], YL0=rA0[i]["YL0"], PG0=rA0[i]["PG0"], carr0=carr0[i])
           for i in range(n_cores)]
    rB0 = launch("B0", mB0)
    mA1 = []
    for i in range(n_cores):
        s = i % nseg
        xh = rB0[i - 1]["xlast"] if s > 0 else np.zeros((D, HALO), np.float32)
        m = {kk: common[i][kk] for kk in keepA}
        m.update(wA(1))
        m.update(xT1=rB0[i]["xT1"], xh1=np.ascontiguousarray(xh))
        mA1.append(m)
    rA1 = launch("A1", mA1)
    carr1 = gather_carry(rA1, 1)
    mB1 = []
    for i in range(n_cores):
        m = {kk: common[i][kk] for kk in keepA}
        m.update(wB(1))
        m.update(xT1=rB0[i]["xT1"], yT1=rA1[i]["yT1"], YL1=rA1[i]["YL1"], PG1=rA1[i]["PG1"], carr1=carr1[i])
        mB1.append(m)
    rB1 = launch("B1", mB1)
    out = np.empty((B, S, D), np.float32)
    for i in range(n_cores):
        b, s = divmod(i, nseg)
        out[b, s * T:(s + 1) * T, :] = rB1[i]["outT"].T
    return out


def run_fused(cfg, inp, n_cores=8):
    D, T = cfg["D"], cfg["T"]
    x = np.asarray(inp["x"], np.float32)
    B, S, _ = x.shape
    nseg = S // T
    assert B * nseg == n_cores and nseg == 4
    ident, sel = host_consts()
    f32 = lambda a: np.ascontiguousarray(np.asarray(a, np.float32))
    vecs = [pack_vecs(cfg, inp, l) for l in (0, 1)]
    shared = {"ident": ident, "sel": sel}
    for l in (0, 1):
        shared.update({f"vecs{l}": vecs[l], f"vecsB{l}": vecs[l], f"w_in{l}": f32(inp["w_in"][l]),
                       f"wa{l}": f32(inp["lru_wa"][l]), f"wx{l}": f32(inp["lru_wx"][l]),
                       f"w_out{l}": f32(inp["w_out"][l])})
    shared.update({"wg0": f32(inp["dense_wg"][0]), "wu0": f32(inp["dense_wu"][0]), "wd0": f32(inp["dense_wd"][0]),
                   "w_router1": f32(inp["w_router"][0]), "wg1": f32(inp["moe_wg"][0]),
                   "wu1": f32(inp["moe_wu"][0]), "wd1": f32(inp["moe_wd"][0])})
    maps = []
    for cidx in range(n_cores):
        b, s = divmod(cidx, nseg)
        xT = np.zeros((D, HALO + T), np.float32)
        if s == 0:
            xT[:, HALO:] = x[b, 0:T, :].T
        else:
            xT[:, :] = x[b, s * T - HALO:(s + 1) * T, :].T
        maps.append(dict(shared, flags=core_flags(s), xTin=xT))
    nc = get_prog(cfg, ["A0", "B0", "A1", "B1"])
    res = run_bass_kernel_spmd(nc, maps, core_ids=list(range(n_cores))).results
    out = np.empty((B, S, D), np.float32)
    for i in range(n_cores):
        b, s = divmod(i, nseg)
        out[b, s * T:(s + 1) * T, :] = res[i]["outT"].T
    return out


FULL_CFG = make_cfg()


def kernel(**inputs):
    return run_fused(FULL_CFG, inputs)
```

```python
import numpy as np
from contextlib import ExitStack
import ml_dtypes
import concourse.bass as bass
import concourse.mybir as mybir
from concourse.bass_utils import run_bass_kernel_spmd

F32 = mybir.dt.float32
BF16 = mybir.dt.bfloat16
AF = mybir.ActivationFunctionType
ALU = mybir.AluOpType
EPS = 1e-6
HALO = 32
CK = 31
LK = 4


def make_cfg(D=2048, T=2048, F=6144, FE=6144, NE=8, W=512):
    c = dict(D=D, KC=D // 128, CW=D // 2, NJ=D // 256, EIN=2 * D, T=T, W=W, F=F, FE=FE, NE=NE,
             TH=T // 2)
    c["PP"] = min(2, c["NJ"])
    c["XG"] = min(4, c["KC"])
    c["DGW"] = min(512, D)
    KC, NJ = c["KC"], c["NJ"]
    off = {}
    p = 0
    for name, n in (("mixn", KC), ("ffnn", KC), ("finn", KC), ("cb", NJ), ("lng", NJ), ("lnb", NJ),
                    ("lcb", NJ), ("ba", NJ), ("bx", NJ), ("lam", NJ), ("cw", NJ * CK), ("lw", NJ * LK)):
        off[name] = p
        p += n
    c["off"] = off
    c["NV"] = p
    return c


class Buf:
    _n = 0

    def __init__(self, ap, kind, name):
        self.ap, self.kind, self.name = ap, kind, name
        self.last_w = None
        self.reads = {}
        self.dcnt = 0
        Buf._n += 1
        self.id = Buf._n

    def __getitem__(self, idx):
        return V(self, self.ap[idx])

    def v(self):
        return V(self, self.ap)


class V:
    def __init__(self, buf, ap):
        self.buf, self.ap = buf, ap

    def __getitem__(self, idx):
        return V(self.buf, self.ap[idx])

    def re(self, s, **kw):
        return V(self.buf, self.ap.rearrange(s, **kw))


class Rot:
    def __init__(self, bufs):
        self.bufs, self.i = bufs, 0

    def next(self):
        b = self.bufs[self.i % len(self.bufs)]
        self.i += 1
        return b


COMPUTE = ("pe", "act", "dve", "pool")
STREAMS = ("pe", "act", "dve", "pool", "sp")


class K:
    def __init__(self, nc, st, arena_words):
        self.nc = nc
        self.arena = st.enter_context(nc.sbuf_tensor("arena", [128, arena_words], F32))[:]
        self.arena_words = arena_words
        self.ptr = 0
        self.ops = []
        self.cnt = {e: 0 for e in COMPUTE}
        self.waited = {e: {} for e in STREAMS}
        self.slot_cnt = []
        self.free_slots = []
        self.live_dbufs = []
        self.cbufs = {}
        self.banks = [Buf(st.enter_context(nc.psum_tensor(f"ps{i}", [128, 512], F32))[:], "psum", f"ps{i}")
                      for i in range(8)]
        self.rot = 0
        self.peak = 0

    def alloc(self, name, shape, dtype):
        n = 1
        for s in shape[1:]:
            n *= s
        words = (n + 1) // 2 if dtype == BF16 else n
        words = (words + 7) // 8 * 8
        off = self.ptr
        self.ptr += words
        self.peak = max(self.peak, self.ptr)
        assert self.ptr <= self.arena_words, f"SBUF arena overflow at {name}: {self.ptr} > {self.arena_words}"
        ap = self.arena[:, off:off + words]
        if dtype == BF16:
            ap = ap.bitcast(BF16)
        ap = ap[:, :n]
        if shape[0] < 128:
            ap = ap[0:shape[0]]
        if len(shape) == 3:
            ap = ap.rearrange("p (a b) -> p a b", a=shape[1])
        elif len(shape) == 4:
            ap = ap.rearrange("p (a b c) -> p a b c", a=shape[1], b=shape[2])
        return Buf(ap, "sbuf", name)

    def bank(self):
        b = self.banks[self.rot % 6]
        self.rot += 1
        return b

    def dram(self, name, shape, dtype, kind):
        return Buf(self.nc.dram_tensor(name, list(shape), dtype, kind=kind).ap(), "dram", name)

    def _collect(self, eng, reads, writes):
        need = {}

        def add(tok):
            if tok is not None:
                need[tok[0]] = max(need.get(tok[0], 0), tok[1])

        for b in reads:
            add(b.last_w)
        for b in writes:
            if b.kind != "dram":
                add(b.last_w)
            for kk, v in b.reads.items():
                add((kk, v))
        out = []
        for kk, v in need.items():
            if kk == eng and eng == "pe":
                continue
            if self.waited[eng].get(kk, 0) >= v:
                continue
            self.waited[eng][kk] = v
            out.append((kk, v))
        return out

    def op(self, eng, fn, reads, writes):
        reads = [r.buf for r in reads if isinstance(r, V)]
        writes = [w.buf for w in writes]
        waits = self._collect(eng, reads, writes)
        n = self.cnt[eng] + 1
        self.cnt[eng] = n
        self.ops.append((eng, fn, waits, (eng, 1)))
        for b in reads:
            b.reads[eng] = max(b.reads.get(eng, 0), n)
        for b in writes:
            b.last_w = (eng, n)
            b.reads = {}

    def dma(self, q, out, in_):
        waits = self._collect(q, [in_.buf], [out.buf])
        dst = out.buf
        if getattr(dst, "dslot", None) is None:
            if self.free_slots:
                dst.dslot = self.free_slots.pop()
            else:
                dst.dslot = len(self.slot_cnt)
                self.slot_cnt.append(0)
            self.live_dbufs.append(dst)
        slot = dst.dslot
        key = ("d", slot)
        self.slot_cnt[slot] += 16
        cntv = self.slot_cnt[slot]
        o_ap, i_ap = out.ap, in_.ap
        self.ops.append((q, lambda e: e.dma_start(out=o_ap, in_=i_ap), waits, (key, 16)))
        in_.buf.reads[key] = max(in_.buf.reads.get(key, 0), cntv)
        dst.last_w = (key, cntv)
        dst.reads = {}

    def collective(self, out_buf, in_buf, groups):
        waits = self._collect("pool", [in_buf], [out_buf])
        key = ("c", out_buf.id)
        self.cbufs[out_buf.id] = out_buf
        o_ap, i_ap = out_buf.ap, in_buf.ap
        self.ops.append(("pool", lambda e: e.collective_compute("AllGather", ALU.bypass, replica_groups=groups,
                                                                  ins=[i_ap], outs=[o_ap]), waits, (key, None)))
        in_buf.reads[key] = 1
        out_buf.last_w = (key, 1)
        out_buf.reads = {}

    def barrier(self):
        allw = [(e, self.cnt[e]) for e in COMPUTE if self.cnt[e] > 0]
        allw += [(("d", i), v) for i, v in enumerate(self.slot_cnt) if v > 0]
        allw += [(("c", b.id), 1) for b in self.cbufs.values()]
        for b in self.live_dbufs:
            self.free_slots.append(b.dslot)
            b.dslot = None
        self.live_dbufs = []
        for e in STREAMS:
            waits = []
            for kk, v in allw:
                if self.waited[e].get(kk, 0) < v:
                    self.waited[e][kk] = v
                    waits.append((kk, v))
            self.ops.append((e, None, waits, None))

    @staticmethod
    def _a(x):
        return x.ap if isinstance(x, V) else x

    def mm(self, out, lhsT, rhs, start, stop):
        o, l, r = out.ap, lhsT.ap, rhs.ap
        self.op("pe", lambda e: e.matmul(o, l, r, start=start, stop=stop), [lhsT, rhs], [out])

    def act(self, out, in_, func, bias=None, scale=None):
        o, i = out.ap, in_.ap
        kw = {}
        if bias is not None:
            kw["bias"] = self._a(bias)
        if scale is not None:
            kw["scale"] = self._a(scale)
        self.op("act", lambda e: e.activation(out=o, in_=i, func=func, **kw), [in_, bias, scale], [out])

    def tt(self, out, in0, in1, op, eng="dve"):
        o, a, b = out.ap, in0.ap, in1.ap
        self.op(eng, lambda e: e.tensor_tensor(out=o, in0=a, in1=b, op=op), [in0, in1], [out])

    def stt(self, out, in0, scalar, in1, op0, op1):
        o, a, s, b = out.ap, in0.ap, self._a(scalar), in1.ap
        self.op("dve", lambda e: e.scalar_tensor_tensor(out=o, in0=a, scalar=s, in1=b, op0=op0, op1=op1),
                [in0, scalar, in1], [out])

    def ts(self, out, in0, s1, s2, op0, op1=None, eng="dve"):
        o, a, x1, x2 = out.ap, in0.ap, self._a(s1), self._a(s2)
        if op1 is None:
            fn = lambda e: e.tensor_scalar(out=o, in0=a, scalar1=x1, scalar2=None, op0=op0)
        else:
            fn = lambda e: e.tensor_scalar(out=o, in0=a, scalar1=x1, scalar2=x2, op0=op0, op1=op1)
        self.op(eng, fn, [in0, s1, s2], [out])

    def scan(self, out, d0, d1, initial, op0, op1):
        o, a, b, i = out.ap, d0.ap, d1.ap, self._a(initial)
        self.op("dve", lambda e: e.tensor_tensor_scan(out=o, data0=a, data1=b, initial=i, op0=op0, op1=op1),
                [d0, d1, initial], [out])

    def copy(self, out, in_, eng="dve"):
        o, i = out.ap, in_.ap
        self.op(eng, lambda e: e.tensor_copy(out=o, in_=i), [in_], [out])

    def recip(self, out, in_):
        o, i = out.ap, in_.ap
        self.op("dve", lambda e: e.reciprocal(out=o, in_=i), [in_], [out])

    def memset(self, v, val, eng="dve"):
        a = v.ap
        self.op(eng, lambda e: e.memset(a, val), [], [v])

    def vmax(self, out, in_):
        o, i = out.ap, in_.ap
        self.op("dve", lambda e: e.max(out=o, in_=i), [in_], [out])

    def emit(self, final_bufs):
        nc = self.nc
        waits = [b.last_w for b in final_bufs if b.last_w is not None]
        self.ops.append(("sp", None, waits, None))
        with ExitStack() as st:
            sems = {}
            for e in COMPUTE:
                sems[e] = st.enter_context(nc.semaphore("s_" + e))
            print("free sems", nc.free_len(), "dma slots", len(self.slot_cnt))
            for i in range(len(self.slot_cnt)):
                sems[("d", i)] = st.enter_context(nc.semaphore(f"d{i}"))
            for b in self.cbufs.values():
                sems[("c", b.id)] = st.enter_context(nc.semaphore(f"c{b.id}"))
            block = st.enter_context(nc.Block())
            ops = self.ops

            def mk(name):
                def body(eng):
                    for (e, fn, waits, inc) in ops:
                        if e != name:
                            continue
                        for (kk, v) in waits:
                            eng.wait_ge(sems[kk], v)
                        if fn is None:
                            continue
                        ins = fn(eng)
                        if inc[1] is None:
                            ins.then_inc(sems[inc[0]])
                        else:
                            ins.then_inc(sems[inc[0]], inc[1])
                return body

            block.tensor(mk("pe"))
            block.scalar(mk("act"))
            block.vector(mk("dve"))
            block.gpsimd(mk("pool"))
            block.sync(mk("sp"))


def setup_consts(k, cfg, din):
    c = {}
    c["ones"] = k.alloc("ones", [128, 128], BF16)
    k.memset(c["ones"].v(), 1.0)
    c["zeros"] = k.alloc("zeros", [128, 512], F32)
    k.memset(c["zeros"].v(), 0.0)
    c["ident"] = k.alloc("ident", [128, 128], F32)
    k.dma("sp", c["ident"].v(), din["ident"].v())
    c["sel"] = k.alloc("sel", [8, 8 * 128], F32)
    k.dma("sp", c["sel"].v(), din["sel"].v())
    c["flags"] = k.alloc("flags", [128, 8], F32)
    k.dma("sp", c["flags"].v(), din["flags"].v())
    k.persist = k.ptr
    return c


def rows(buf, r0, nr, c0, nc_):
    return V(buf, buf.ap[r0:r0 + nr, c0:c0 + nc_].rearrange("(k p) t -> p k t", p=128))


def stage_A(k, cfg, c, dw, xmain, xoff, xhalo, xhoff, yT, YL, PG, cst, halo_gathered=False):
    D, KC, NJ, T, W, CW = cfg["D"], cfg["KC"], cfg["NJ"], cfg["T"], cfg["W"], cfg["CW"]
    PP, XG, off = cfg["PP"], cfg["XG"], cfg["off"]
    k.ptr = k.persist
    vec = k.alloc("vec", [128, cfg["NV"]], F32)
    k.dma("sp", vec.v(), dw["vecs"].v())
    wa = k.alloc("wa", [128, NJ, 128], BF16)
    wx = k.alloc("wx", [128, NJ, 128], BF16)
    k.dma("pool", wa.v(), dw["wa"].v().re("h i j -> i h j"))
    k.dma("pool", wx.v(), dw["wx"].v().re("h i j -> i h j"))

    def vcol(name, i, n=1):
        return vec[:, off[name] + i: off[name] + i + n]

    sm = k.alloc("sm", [128, 10 * NJ], F32)
    s_ = [sm[:, i * NJ:(i + 1) * NJ] for i in range(10)]
    e_, ln_, t1, msk, t2, spv, nsp, nsp2 = s_[0], s_[1], s_[2], s_[3], s_[4], s_[5], s_[6], s_[7]
    lam = vcol("lam", 0, NJ)
    k.act(e_, lam, AF.Exp, scale=-1.0)
    k.act(ln_, e_, AF.Ln, bias=1.0)
    k.ts(t1, e_, -1.0 / 3.0, 0.5, ALU.mult, ALU.add)
    k.tt(t1, t1, e_, ALU.mult)
    k.ts(t1, t1, -1.0, 1.0, ALU.mult, ALU.add)
    k.tt(t1, t1, e_, ALU.mult)
    k.ts(msk, e_, 0.05, None, ALU.is_lt)
    k.tt(t2, t1, ln_, ALU.subtract)
    k.tt(t2, t2, msk, ALU.mult)
    k.tt(spv, t2, ln_, ALU.add)
    k.ts(nsp, spv, -8.0, None, ALU.mult)
    k.ts(nsp2, spv, -16.0, None, ALU.mult)

    hst = k.alloc("hst", [128, NJ], F32)
    pst = k.alloc("pst", [128, NJ], F32)
    k.memset(hst.v(), 0.0)
    k.memset(pst.v(), 1.0)
    chalo = [k.alloc(f"chalo{j}", [128, HALO], BF16) for j in range(NJ)]
    rhalo = [k.alloc(f"rhalo{j}", [128, HALO], F32) for j in range(NJ)]

    xt = [k.alloc(f"xt{g}", [128, XG, W], F32) for g in range(KC // XG)]
    if halo_gathered:
        xhp = [k.alloc(f"xhp{i}", [128, XG, HALO], F32) for i in range(3)]
    hT = [k.alloc(f"hT{i}", [128, W], BF16) for i in range(KC)]
    xth = [k.alloc(f"xth{g}", [128, XG, HALO], F32) for g in range(KC // XG)]
    hTh = [k.alloc(f"hTh{i}", [128, HALO], BF16) for i in range(KC)]
    sqp = Rot([k.alloc(f"sq{i}", [128, W], BF16) for i in range(2)])
    bfp = Rot([k.alloc(f"bfp{i}", [128, W], BF16) for i in range(4)])
    wtA = Rot([k.alloc(f"wtA{i}", [128, KC, PP * 128], BF16) for i in range(2)])
    wtB = Rot([k.alloc(f"wtB{i}", [128, KC, PP * 128], BF16) for i in range(2)])
    cwp = Rot([k.alloc(f"cw{i}", [128, HALO + W], BF16) for i in range(2)])
    dgp = Rot([k.alloc(f"dg{i}", [128, CK, 128], BF16) for i in range(2)])
    identb = k.alloc("identb", [128, 128], BF16)
    k.copy(identb.v(), c["ident"].v())
    rwp = Rot([k.alloc(f"rw{i}", [128, HALO + W], F32) for i in range(2)])
    ccb = [k.alloc(f"cc{j}", [128, W], F32) for j in range(NJ)]
    fp = Rot([k.alloc(f"fp{i}", [128, W], F32) for i in range(20)])
    rstd = k.alloc("rstd", [128, W], F32)
    mu = k.alloc("mu", [128, W], F32)
    rs2 = k.alloc("rs2", [128, W], F32)
    ycv = Rot([k.alloc(f"ycv{i}", [128, NJ, W], BF16) for i in range(2)])
    ps_ss, ps_s1, ps_s2 = k.banks[6], k.banks[6], k.banks[7]
    ones = c["ones"].v()
    flags = c["flags"]
    w_in = dw["w_in"]

    def lru_part2(j, ti, gx, rc, ml, a_, G):
        bb = fp.next()
        k.tt(bb[:, :], gx[:, :], rc[:, :], ALU.mult)
        k.tt(bb[:, :], bb[:, :], ml[:, :], ALU.mult)
        hl, Pc = fp.next(), fp.next()
        k.scan(hl[:, :], a_[:, :], bb[:, :], hst[:, j:j + 1], ALU.mult, ALU.add)
        k.copy(hst[:, j:j + 1], hl[:, W - 1:W])
        k.scan(Pc[:, :], a_[:, :], c["zeros"][:, :W], pst[:, j:j + 1], ALU.mult, ALU.add)
        k.copy(pst[:, j:j + 1], Pc[:, W - 1:W])
        k.tt(hl[:, :], hl[:, :], G[:, :], ALU.mult)
        k.tt(Pc[:, :], Pc[:, :], G[:, :], ALU.mult)
        k.dma("sp", YL[j * 128:(j + 1) * 128, ti * W:(ti + 1) * W], hl[:, :])
        k.dma("sp", PG[j * 128:(j + 1) * 128, ti * W:(ti + 1) * W], Pc[:, :])

    def conv_part2(j, dg, cw):
        pcv = k.bank()
        for kk in range(CK):
            k.mm(pcv[:, :W], dg[:, kk, :], cw[:, 2 + kk:2 + kk + W], kk == 0, kk == CK - 1)
        k.copy(chalo[j][:, :], cw[:, W:W + HALO])
        cc = ccb[j]
        k.act(cc[:, :], pcv[:, :W], AF.Identity, bias=vcol("cb", j))
        b1, b2 = bfp.next(), bfp.next()
        k.act(b1[:, :], cc[:, :], AF.Identity)
        k.act(b2[:, :], cc[:, :], AF.Square)
        k.mm(ps_s1[:, :W], ones, b1[:, :], j == 0, j == NJ - 1)
        k.mm(ps_s2[:, :W], ones, b2[:, :], j == 0, j == NJ - 1)

    pend_c = None
    pend = None
    grps = [[("halo", 0, HALO), ("main", 0, W)]] + [[("main", i, W)] for i in range(1, T // W)]
    for grp in grps:
      for kind, ti, w in grp:
        xt_, hT_ = (xth, hTh) if kind == "halo" else (xt, hT)
        for g in range(KC // XG):
            if kind == "halo" and halo_gathered:
                for jj in range(3):
                    k.dma("sp", xhp[jj].v(), rows(xhalo, jj * D + g * XG * 128, XG * 128, 0, w))
                k.ts(xt_[g][:, :, 0:w], xhp[0].v(), flags[:, 5:6], None, ALU.mult)
                k.stt(xt_[g][:, :, 0:w], xhp[1].v(), flags[:, 6:7], xt_[g][:, :, 0:w], ALU.mult, ALU.add)
                k.stt(xt_[g][:, :, 0:w], xhp[2].v(), flags[:, 7:8], xt_[g][:, :, 0:w], ALU.mult, ALU.add)
                continue
            if kind == "halo":
                src = rows(xhalo, g * XG * 128, XG * 128, xhoff, w)
            else:
                src = rows(xmain, g * XG * 128, XG * 128, xoff + ti * W, w)
            k.dma("sp", xt_[g][:, :, 0:w], src)
        for kc in range(KC):
            sq = sqp.next()
            k.act(sq[:, :w], xt_[kc // XG][:, kc % XG, 0:w], AF.Square)
            k.mm(ps_ss[:, :w], ones, sq[:, :w], kc == 0, kc == KC - 1)
        rt = fp.next()
        k.act(rt[:, :w], ps_ss[:, :w], AF.Sqrt, bias=EPS, scale=1.0 / D)
        k.recip(rstd[:, :w], rt[:, :w])
        for kc in range(KC):
            k.stt(hT_[kc][:, :w], xt_[kc // XG][:, kc % XG, 0:w], vcol("mixn", kc), rstd[:, :w],
                  ALU.mult, ALU.mult)

      for branch in (0, 1):
            for q in range(NJ // PP):
                A, B = wtA.next(), wtB.next()
                ca0 = branch * 2 * CW + q * PP * 128
                cb0 = ca0 + CW
                k.dma("pool", A.v(), rows(w_in, 0, D, ca0, PP * 128))
                k.dma("pool", B.v(), rows(w_in, 0, D, cb0, PP * 128))
                for pr in range(PP):
                    j = q * PP + pr
                    for kind, ti, w in grp:
                        hT_ = hTh if kind == "halo" else hT
                        psa, psb = k.bank(), k.bank()
                        for kc in range(KC):
                            k.mm(psa[:, :w], A[:, kc, pr * 128:(pr + 1) * 128], hT_[kc][:, :w], kc == 0, kc == KC - 1)
                        need_b = not (branch == 1 and kind == "halo")
                        if need_b:
                            for kc in range(KC):
                                k.mm(psb[:, :w], B[:, kc, pr * 128:(pr + 1) * 128], hT_[kc][:, :w], kc == 0,
                                     kc == KC - 1)
                        if branch == 0:
                            sgt = fp.next()
                            k.act(sgt[:, :w], psb[:, :w], AF.Sigmoid)
                            if kind == "halo":
                                k.tt(chalo[j][:, :], psa[:, :w], sgt[:, :w], ALU.mult)
                                continue
                            cw = cwp.next()
                            k.copy(cw[:, 0:HALO], chalo[j][:, :])
                            k.tt(cw[:, HALO:HALO + W], psa[:, :W], sgt[:, :W], ALU.mult)
                            dg = dgp.next()
                            for kk in range(CK):
                                k.ts(dg[:, kk, :], identb[:, :], vcol("cw", j * CK + kk), None, ALU.mult)
                            if pend_c is not None:
                                conv_part2(*pend_c)
                            pend_c = (j, dg, cw)
                        else:
                            if kind == "halo":
                                k.act(rhalo[j][:, :], psa[:, :w], AF.Identity)
                                continue
                            rw = rwp.next()
                            k.copy(rw[:, 0:HALO], rhalo[j][:, :])
                            k.act(rw[:, HALO:HALO + W], psa[:, :W], AF.Identity)
                            G = fp.next()
                            k.act(G[:, :], psb[:, :W], AF.Gelu_apprx_tanh)
                            rc = fp.next()
                            k.ts(rc[:, :], rw[:, HALO - 3:HALO - 3 + W], vcol("lw", j * LK), vcol("lcb", j),
                                 ALU.mult, ALU.add)
                            for kk in range(1, LK):
                                k.stt(rc[:, :], rw[:, HALO - 3 + kk:HALO - 3 + kk + W], vcol("lw", j * LK + kk),
                                      rc[:, :], ALU.mult, ALU.add)
                            k.copy(rhalo[j][:, :], rw[:, W:W + HALO])
                            rcb = bfp.next()
                            k.act(rcb[:, :], rc[:, :], AF.Identity)
                            pga, pgx = k.bank(), k.bank()
                            k.mm(pga[:, :W], wa[:, j, :], rcb[:, :], True, True)
                            k.mm(pgx[:, :W], wx[:, j, :], rcb[:, :], True, True)
                            ga, gx, a_, a2, ml = fp.next(), fp.next(), fp.next(), fp.next(), fp.next()
                            k.act(ga[:, :], pga[:, :W], AF.Sigmoid, bias=vcol("ba", j))
                            k.act(gx[:, :], pgx[:, :W], AF.Sigmoid, bias=vcol("bx", j))
                            k.act(a_[:, :], ga[:, :], AF.Exp, scale=nsp[:, j:j + 1])
                            k.act(a2[:, :], ga[:, :], AF.Exp, scale=nsp2[:, j:j + 1])
                            k.act(ml[:, :], a2[:, :], AF.Sqrt, bias=1.0, scale=-1.0)
                            if ti == 0:
                                k.ts(ml[:, 0:1], ml[:, 0:1], flags[:, 4:5], flags[:, 3:4], ALU.mult, ALU.add)
                            if pend is not None:
                                lru_part2(*pend)
                            pend = (j, ti, gx, rc, ml, a_, G)
            if branch == 1 and pend is not None:
                lru_part2(*pend)
                pend = None
            if branch == 0:
                if pend_c is not None:
                    conv_part2(*pend_c)
                    pend_c = None
                ti = grp[-1][1]
                mu2, var, sd = fp.next(), fp.next(), fp.next()
                k.act(mu[:, :], ps_s1[:, :W], AF.Identity, scale=1.0 / CW)
                k.tt(mu2[:, :], mu[:, :], mu[:, :], ALU.mult)
                k.stt(var[:, :], ps_s2[:, :W], 1.0 / CW, mu2[:, :], ALU.mult, ALU.subtract)
                k.act(sd[:, :], var[:, :], AF.Sqrt, bias=EPS)
                k.recip(rs2[:, :], sd[:, :])
                yc = ycv.next()
                for j in range(NJ):
                    t_ = fp.next()
                    k.tt(t_[:, :], ccb[j][:, :], mu[:, :], ALU.subtract)
                    k.tt(t_[:, :], t_[:, :], rs2[:, :], ALU.mult)
                    k.act(yc[:, j, :], t_[:, :], AF.Silu, bias=vcol("lnb", j), scale=vcol("lng", j))
                k.dma("sp", rows(yT, 0, CW, ti * W, W), yc.v())
    k.dma("sp", cst[0:128, :], pst.v())
    k.dma("sp", cst[128:256, :], hst.v())


def stage_B(k, cfg, c, dw, xres, xroff, yT, YL, PG, carr, xout, xlast, outT, is_moe, is_last):
    D, KC, NJ, T, W, CW, TH = cfg["D"], cfg["KC"], cfg["NJ"], cfg["T"], cfg["W"], cfg["CW"], cfg["TH"]
    off, DGW = cfg["off"], cfg["DGW"]
    NE = cfg["NE"] if is_moe else 1
    F = cfg["FE"] if is_moe else cfg["F"]
    NTH = TH // W
    k.ptr = k.persist
    vec = k.alloc("vec", [128, cfg["NV"]], F32)
    k.dma("sp", vec.v(), dw["vecs"].v())

    def vcol(name, i, n=1):
        return vec[:, off[name] + i: off[name] + i + n]

    flags = c["flags"]
    ones = c["ones"].v()
    ca = k.alloc("ca", [128, 8, NJ], F32)
    k.dma("sp", ca.v(), carr.v().re("(sa p) j -> p sa j", p=128))
    carry = k.alloc("carry", [128, NJ], F32)
    ctmp = k.alloc("ctmp", [128, NJ], F32)
    k.memset(carry.v(), 0.0)
    for s in range(3):
        k.tt(ctmp[:, :], ca[:, 2 * s, :], carry[:, :], ALU.mult)
        k.tt(ctmp[:, :], ctmp[:, :], ca[:, 2 * s + 1, :], ALU.add)
        k.tt(ctmp[:, :], ctmp[:, :], carry[:, :], ALU.subtract)
        k.stt(carry[:, :], ctmp[:, :], flags[:, s:s + 1], carry[:, :], ALU.mult, ALU.add)

    if is_moe:
        wr = k.alloc("wr", [128, KC, 8], F32)
        k.dma("sp", wr.v(), dw["w_router"].v().re("(kc p) e -> p kc e", p=128))
        lgT = k.alloc("lgT", [8, TH], F32)
        gwT = k.alloc("gwT", [8, TH], F32)
        smalp = Rot([k.alloc(f"smal{i}", [128, 64], F32) for i in range(4)])
    h2T = [k.alloc(f"h2T{i}", [128, TH], BF16) for i in range(KC)]
    acc = [k.alloc(f"acc{i}", [128, TH], F32) for i in range(KC)]
    base = k.ptr
    ps_ss = k.banks[6]

    for hh in range(2):
        k.barrier()
        k.ptr = base
        yt = k.alloc("yt", [128, 2 * NJ, W], BF16)
        ylp = Rot([k.alloc(f"yl{i}", [128, W], F32) for i in range(2)])
        pgp = Rot([k.alloc(f"pg{i}", [128, W], F32) for i in range(2)])
        wop = Rot([k.alloc(f"wo{i}", [128, 2 * NJ, DGW], BF16) for i in range(2)])
        sqp = Rot([k.alloc(f"sqb{i}", [128, W], BF16) for i in range(2)])
        rt = k.alloc("rtb", [128, W], F32)
        rstd = k.alloc("rstdb", [128, W], F32)
        hfp = Rot([k.alloc(f"hf{i}", [128, W], F32) for i in range(2)])
        for tt in range(NTH):
            col = hh * TH + tt * W
            lc = tt * W
            for d in range(KC):
                k.dma("sp", acc[d][:, lc:lc + W], xres[d * 128:(d + 1) * 128, xroff + col:xroff + col + W])
            k.dma("sp", yt[:, 0:NJ, :], rows(yT, 0, CW, col, W))
            for j in range(NJ):
                yl, pg = ylp.next(), pgp.next()
                k.dma("sp", yl[:, :], YL[j * 128:(j + 1) * 128, col:col + W])
                k.dma("sp", pg[:, :], PG[j * 128:(j + 1) * 128, col:col + W])
                k.stt(yt[:, NJ + j, :], pg[:, :], carry[:, j:j + 1], yl[:, :], ALU.mult, ALU.add)
            for dg in range(D // DGW):
                wo = wop.next()
                k.dma("pool", wo.v(), rows(dw["w_out"], 0, D, dg * DGW, DGW))
                for dc in range(DGW // 128):
                    d = dg * (DGW // 128) + dc
                    ps = k.bank()
                    for e in range(2 * NJ):
                        k.mm(ps[:, :W], wo[:, e, dc * 128:(dc + 1) * 128], yt[:, e, :], e == 0, e == 2 * NJ - 1)
                    k.tt(acc[d][:, lc:lc + W], ps[:, :W], acc[d][:, lc:lc + W], ALU.add)
                    sq = sqp.next()
                    k.act(sq[:, :], acc[d][:, lc:lc + W], AF.Square)
                    k.mm(ps_ss[:, :W], ones, sq[:, :], d == 0, d == KC - 1)
            k.act(rt[:, :], ps_ss[:, :W], AF.Sqrt, bias=EPS, scale=1.0 / D)
            k.recip(rstd[:, :], rt[:, :])
            for kc in range(KC):
                k.stt(h2T[kc][:, lc:lc + W], acc[kc][:, lc:lc + W], vcol("ffnn", kc), rstd[:, :],
                      ALU.mult, ALU.mult)
            if is_moe:
                pl = k.bank()
                for kc in range(KC):
                    hf = hfp.next()
                    k.stt(hf[:, :], acc[kc][:, lc:lc + W], vcol("ffnn", kc), rstd[:, :], ALU.mult, ALU.mult)
                    k.mm(pl[0:8, :W], wr[:, kc, :], hf[:, :], kc == 0, kc == KC - 1)
                k.act(lgT[0:8, lc:lc + W], pl[0:8, :W], AF.Identity)
        if is_moe:
            for blk in range(TH // 128):
                sl = slice(blk * 128, (blk + 1) * 128)
                smal = smalp.next()
                lg, m8, msk, nl1 = smal[:, 0:8], smal[:, 8:16], smal[:, 16:24], smal[:, 24:25]
                ex, e2, den, rden, gw = smal[:, 32:40], smal[:, 25:26], smal[:, 26:27], smal[:, 27:28], smal[:, 40:48]
                pl = k.bank()
                k.mm(pl[:, 0:8], lgT[0:8, sl], c["ident"][0:8, 0:8], True, True)
                k.act(lg, pl[:, 0:8], AF.Identity)
                k.vmax(m8, lg)
                k.ts(msk, lg, m8[:, 1:2], None, ALU.is_ge)
                k.ts(nl1, m8[:, 0:1], -1.0, None, ALU.mult)
                k.act(ex, lg, AF.Exp, bias=nl1)
                k.act(e2, m8[:, 1:2], AF.Exp, bias=nl1)
                k.ts(den, e2, 1.0, None, ALU.add)
                k.recip(rden, den)
                k.stt(gw, ex, rden, msk, ALU.mult, ALU.mult)
                pt = k.bank()
                k.mm(pt[0:8, 0:128], gw, c["ident"][:, :], True, True)
                k.act(gwT[0:8, sl], pt[0:8, 0:128], AF.Identity)

        k.barrier()
        k.ptr = base
        wgp = Rot([k.alloc(f"wg{i}", [128, KC, 256], BF16) for i in range(2)])
        wup = Rot([k.alloc(f"wu{i}", [128, KC, 256], BF16) for i in range(2)])
        wdp = Rot([k.alloc(f"wd{i}", [128, 4, D], BF16) for i in range(2)])
        actT = [k.alloc(f"actT{i}", [128, TH], BF16) for i in range(4)]
        sgp = Rot([k.alloc(f"sg{i}", [128, W], F32) for i in range(2)])
        tmpp = Rot([k.alloc(f"tm{i}", [128, W], F32) for i in range(2)])
        if is_moe:
            gwBp = Rot([k.alloc(f"gwB{i}", [128, TH], F32) for i in range(2)])
        for ex_i in range(NE):
            if is_moe:
                wg_d, wu_d, wd_d = (V(dw[n], dw[n].ap[ex_i]) for n in ("wg", "wu", "wd"))
                gwB = gwBp.next()
                for tt in range(NTH):
                    pb = k.bank()
                    k.mm(pb[:, :W], c["sel"][0:8, ex_i * 128:(ex_i + 1) * 128], gwT[0:8, tt * W:(tt + 1) * W],
                         True, True)
                    k.act(gwB[:, tt * W:(tt + 1) * W], pb[:, :W], AF.Identity)
            else:
                wg_d, wu_d, wd_d = (dw[n].v() for n in ("wg", "wu", "wd"))
            for g in range(F // 512):
                wd_t = wdp.next()
                k.dma("pool", wd_t.v(), wd_d[g * 512:(g + 1) * 512, :].re("(f p) d -> p f d", p=128))
                for pr in range(2):
                    wg_t, wu_t = wgp.next(), wup.next()
                    f0 = g * 512 + pr * 256
                    k.dma("pool", wg_t.v(), wg_d[:, f0:f0 + 256].re("(kc p) f -> p kc f", p=128))
                    k.dma("pool", wu_t.v(), wu_d[:, f0:f0 + 256].re("(kc p) f -> p kc f", p=128))
                    for fc2 in range(2):
                        fc = pr * 2 + fc2
                        for tt in range(NTH):
                            cs = slice(tt * W, (tt + 1) * W)
                            pg_, pu_ = k.bank(), k.bank()
                            for kc in range(KC):
                                k.mm(pg_[:, :W], wg_t[:, kc, fc2 * 128:(fc2 + 1) * 128], h2T[kc][:, cs], kc == 0,
                                     kc == KC - 1)
                            for kc in range(KC):
                                k.mm(pu_[:, :W], wu_t[:, kc, fc2 * 128:(fc2 + 1) * 128], h2T[kc][:, cs], kc == 0,
                                     kc == KC - 1)
                            sg = sgp.next()
                            k.act(sg[:, :], pg_[:, :W], AF.Silu)
                            if is_moe:
                                tm = tmpp.next()
                                k.tt(tm[:, :], pu_[:, :W], sg[:, :], ALU.mult)
                                k.tt(actT[fc][:, cs], tm[:, :], gwB[:, cs], ALU.mult)
                            else:
                                k.tt(actT[fc][:, cs], pu_[:, :W], sg[:, :], ALU.mult)
                for d in range(KC):
                    for tt in range(NTH):
                        cs = slice(tt * W, (tt + 1) * W)
                        pd = k.bank()
                        for fc in range(4):
                            k.mm(pd[:, :W], wd_t[:, fc, d * 128:(d + 1) * 128], actT[fc][:, cs], fc == 0, fc == 3)
                        k.tt(acc[d][:, cs], pd[:, :W], acc[d][:, cs], ALU.add)
        if not is_last:
            for d in range(KC):
                k.dma("sp", xout[d * 128:(d + 1) * 128, hh * TH:(hh + 1) * TH], acc[d][:, :])
                if hh == 1:
                    k.dma("sp", xlast[d * 128:(d + 1) * 128, :], acc[d][:, TH - HALO:TH])
        else:
            k.barrier()
            k.ptr = base
            sqf = Rot([k.alloc(f"sqf{i}", [128, W], BF16) for i in range(2)])
            otp = Rot([k.alloc(f"ot{i}", [128, W], F32) for i in range(2)])
            rtf = k.alloc("rtf", [128, W], F32)
            rsf = k.alloc("rsf", [128, W], F32)
            for tt in range(NTH):
                cs = slice(tt * W, (tt + 1) * W)
                for d in range(KC):
                    sq = sqf.next()
                    k.act(sq[:, :], acc[d][:, cs], AF.Square)
                    k.mm(ps_ss[:, :W], ones, sq[:, :], d == 0, d == KC - 1)
                k.act(rtf[:, :], ps_ss[:, :W], AF.Sqrt, bias=EPS, scale=1.0 / D)
                k.recip(rsf[:, :], rtf[:, :])
                for d in range(KC):
                    ot = otp.next()
                    k.stt(ot[:, :], acc[d][:, cs], vcol("finn", d), rsf[:, :], ALU.mult, ALU.mult)
                    k.dma("sp", outT[d * 128:(d + 1) * 128, hh * TH + tt * W: hh * TH + (tt + 1) * W], ot[:, :])


ARENA_WORDS = 52800


def build_program(cfg, stages):
    nc = bass.Bass("TRN2", target_bir_lowering=False)
    D, KC, NJ, T, CW = cfg["D"], cfg["KC"], cfg["NJ"], cfg["T"], cfg["CW"]
    fused = len(stages) == 4
    groups = [[0, 1, 2, 3], [4, 5, 6, 7]]
    final_bufs = []
    with ExitStack() as st:
        k = K(nc, st, ARENA_WORDS)

        def ext_in(name, shape, dtype=F32):
            return k.dram(name, shape, dtype, "ExternalInput")

        def inter(name, shape, dtype, producer_stage, consumer_stages):
            if producer_stage in stages:
                if all(s in stages for s in consumer_stages):
                    b = k.dram(name, shape, dtype, "Internal")
                else:
                    b = k.dram(name, shape, dtype, "ExternalOutput")
                    final_bufs.append(b)
                return b
            if any(s in stages for s in consumer_stages):
                return ext_in(name, shape, dtype)
            return None

        din = {"ident": ext_in("ident", [128, 128]), "sel": ext_in("sel", [8, 1024]),
               "flags": ext_in("flags", [128, 8])}
        c = setup_consts(k, cfg, din)
        xTin = ext_in("xTin", [D, HALO + T]) if ("A0" in stages or "B0" in stages) else None
        t = {}
        for l in (0, 1):
            t[f"yT{l}"] = inter(f"yT{l}", [CW, T], BF16, f"A{l}", [f"B{l}"])
            t[f"YL{l}"] = inter(f"YL{l}", [CW, T], F32, f"A{l}", [f"B{l}"])
            t[f"PG{l}"] = inter(f"PG{l}", [CW, T], F32, f"A{l}", [f"B{l}"])
            if fused:
                t[f"cst{l}"] = k.dram(f"cst{l}", [256, NJ], F32, "Internal")
                t[f"carr{l}"] = k.dram(f"carr{l}", [1024, NJ], F32, "Internal")
            else:
                t[f"cst{l}"] = inter(f"cst{l}", [256, NJ], F32, f"A{l}", ["host"])
                t[f"carr{l}"] = ext_in(f"carr{l}", [1024, NJ]) if f"B{l}" in stages else None
        t["xT1"] = inter("xT1", [D, T], F32, "B0", ["A1", "B1"])
        if fused:
            t["xlast"] = k.dram("xlast", [D, HALO], F32, "Internal")
            t["xh1"] = k.dram("xhall", [4 * D, HALO], F32, "Internal")
        else:
            t["xlast"] = inter("xlast", [D, HALO], F32, "B0", ["host"])
            t["xh1"] = ext_in("xh1", [D, HALO]) if "A1" in stages else None
        if "B1" in stages:
            outT = k.dram("outT", [D, T], F32, "ExternalOutput")
            final_bufs.append(outT)

        def layer_w(l, names):
            return {n: ext_in(f"{n}{l}", shp, F32) for n, shp in names}

        for sname in stages:
            l = int(sname[1])
            k.barrier()
            if sname[0] == "A":
                dw = layer_w(l, [("vecs", [128, cfg["NV"]]), ("w_in", [D, cfg["EIN"]]),
                                 ("wa", [NJ, 128, 128]), ("wx", [NJ, 128, 128])])
                if l == 0:
                    stage_A(k, cfg, c, dw, xTin, HALO, xTin, 0, t["yT0"], t["YL0"], t["PG0"], t["cst0"])
                else:
                    stage_A(k, cfg, c, dw, t["xT1"], 0, t["xh1"], 0, t["yT1"], t["YL1"], t["PG1"], t["cst1"],
                            halo_gathered=fused)
                if fused:
                    k.collective(t[f"carr{l}"], t[f"cst{l}"], groups)
            else:
                names = [("vecsB", [128, cfg["NV"]]), ("w_out", [D, D])]
                if l == 0:
                    names += [("wg", [D, cfg["F"]]), ("wu", [D, cfg["F"]]), ("wd", [cfg["F"], D])]
                else:
                    names += [("w_router", [D, 8]), ("wg", [cfg["NE"], D, cfg["FE"]]),
                              ("wu", [cfg["NE"], D, cfg["FE"]]), ("wd", [cfg["NE"], cfg["FE"], D])]
                dw = layer_w(l, names)
                dw["vecs"] = dw["vecsB"]
                if l == 0:
                    stage_B(k, cfg, c, dw, xTin, HALO, t["yT0"], t["YL0"], t["PG0"], t["carr0"],
                            t["xT1"], t["xlast"], None, False, False)
                    if fused:
                        k.collective(t["xh1"], t["xlast"], groups)
                else:
                    stage_B(k, cfg, c, dw, t["xT1"], 0, t["yT1"], t["YL1"], t["PG1"], t["carr1"],
                            None, None, outT, True, True)
        k.emit(final_bufs)
        build_program.last_peak = k.peak
    return nc


def vec2d(v, n):
    return np.ascontiguousarray(np.asarray(v, np.float32).reshape(n, 128).T)


def pack_vecs(cfg, inp, l):
    KC, NJ = cfg["KC"], cfg["NJ"]
    cw = np.asarray(inp["conv_w"][l], np.float32)
    lw = np.asarray(inp["lru_conv_w"][l], np.float32)
    parts = [vec2d(inp["mix_norm"][l], KC), vec2d(inp["ffn_norm"][l], KC), vec2d(inp["final_norm"], KC),
             vec2d(inp["conv_b"][l], NJ), vec2d(inp["conv_ln_g"][l], NJ), vec2d(inp["conv_ln_b"][l], NJ),
             vec2d(inp["lru_conv_b"][l], NJ), vec2d(inp["lru_ba"][l], NJ), vec2d(inp["lru_bx"][l], NJ),
             vec2d(inp["lru_lambda"][l], NJ),
             cw.reshape(CK, NJ, 128).transpose(2, 1, 0).reshape(128, NJ * CK),
             lw.reshape(LK, NJ, 128).transpose(2, 1, 0).reshape(128, NJ * LK)]
    return np.ascontiguousarray(np.concatenate(parts, axis=1).astype(np.float32))


def host_consts():
    ident = np.eye(128, dtype=np.float32)
    sel = np.zeros((8, 8, 128), np.float32)
    for e in range(8):
        sel[e, e, :] = 1.0
    return ident, sel.reshape(8, 1024)


def core_flags(s):
    f = np.zeros((128, 8), np.float32)
    for j in range(3):
        f[:, j] = 1.0 if j < s else 0.0
    f[:, 3] = 1.0 if s == 0 else 0.0
    f[:, 4] = 0.0 if s == 0 else 1.0
    for j in range(3):
        f[:, 5 + j] = 1.0 if j == s - 1 else 0.0
    return f


_PROG_CACHE = {}


def get_prog(cfg, stages):
    key = (cfg["D"], cfg["T"], cfg["F"], cfg["FE"], cfg["W"], tuple(stages))
    if key not in _PROG_CACHE:
        _PROG_CACHE[key] = build_program(cfg, list(stages))
    return _PROG_CACHE[key]


def run_unfused(cfg, inp, n_cores=8):
    D, T, NJ = cfg["D"], cfg["T"], cfg["NJ"]
    x = np.asarray(inp["x"], np.float32)
    B, S, _ = x.shape
    nseg = S // T
    assert B * nseg == n_cores and nseg == 4
    ident, sel = host_consts()
    common = []
    for cidx in range(n_cores):
        b, s = divmod(cidx, nseg)
        xT = np.zeros((D, HALO + T), np.float32)
        lo = s * T - HALO
        if s == 0:
            xT[:, HALO:] = x[b, 0:T, :].T
        else:
            xT[:, :] = x[b, lo:lo + HALO + T, :].T
        common.append({"ident": ident, "sel": sel, "flags": core_flags(s), "xTin": xT})
    vecs = [pack_vecs(cfg, inp, l) for l in (0, 1)]
    f32 = lambda a: np.ascontiguousarray(np.asarray(a, np.float32))

    def wA(l):
        return {f"vecs{l}": vecs[l], f"w_in{l}": f32(inp["w_in"][l]), f"wa{l}": f32(inp["lru_wa"][l]),
                f"wx{l}": f32(inp["lru_wx"][l])}

    def wB(l):
        d = {f"vecsB{l}": vecs[l], f"w_out{l}": f32(inp["w_out"][l])}
        if l == 0:
            d.update({"wg0": f32(inp["dense_wg"][0]), "wu0": f32(inp["dense_wu"][0]), "wd0": f32(inp["dense_wd"][0])})
        else:
            d.update({"w_router1": f32(inp["w_router"][0]), "wg1": f32(inp["moe_wg"][0]),
                      "wu1": f32(inp["moe_wu"][0]), "wd1": f32(inp["moe_wd"][0])})
        return d

    def launch(stage, maps):
        nc = get_prog(cfg, [stage])
        res = run_bass_kernel_spmd(nc, maps, core_ids=list(range(n_cores)))
        return res.results

    def gather_carry(res, l):
        out = []
        for cidx in range(n_cores):
            b = cidx // nseg
            out.append(np.ascontiguousarray(np.concatenate([res[b * nseg + j][f"cst{l}"] for j in range(nseg)], 0)))
        return out

    keepA = ("ident", "sel", "flags")
    rA0 = launch("A0", [dict(common[i], **wA(0)) for i in range(n_cores)])
    carr0 = gather_carry(rA0, 0)
    mB0 = [dict(common[i], **wB(0), yT0=rA0[i]["yT0"], YL0=rA0[i]["YL0"], PG0=rA0[i]["PG0"], carr0=carr0[i])
           for i in range(n_cores)]
    rB0 = launch("B0", mB0)
    mA1 = []
    for i in range(n_cores):
        s = i % nseg
        xh = rB0[i - 1]["xlast"] if s > 0 else np.zeros((D, HALO), np.float32)
        m = {kk: common[i][kk] for kk in keepA}
        m.update(wA(1))
        m.update(xT1=rB0[i]["xT1"], xh1=np.ascontiguousarray(xh))
        mA1.append(m)
    rA1 = launch("A1", mA1)
    carr1 = gather_carry(rA1, 1)
    mB1 = []
    for i in range(n_cores):
        m = {kk: common[i][kk] for kk in keepA}
        m.update(wB(1))
        m.update(xT1=rB0[i]["xT1"], yT1=rA1[i]["yT1"], YL1=rA1[i]["YL1"], PG1=rA1[i]["PG1"], carr1=carr1[i])
        mB1.append(m)
    rB1 = launch("B1", mB1)
    out = np.empty((B, S, D), np.float32)
    for i in range(n_cores):
        b, s = divmod(i, nseg)
        out[b, s * T:(s + 1) * T, :] = rB1[i]["outT"].T
    return out


def run_fused(cfg, inp, n_cores=8):
    D, T = cfg["D"], cfg["T"]
    x = np.asarray(inp["x"], np.float32)
    B, S, _ = x.shape
    nseg = S // T
    assert B * nseg == n_cores and nseg == 4
    ident, sel = host_consts()
    f32 = lambda a: np.ascontiguousarray(np.asarray(a, np.float32))
    vecs = [pack_vecs(cfg, inp, l) for l in (0, 1)]
    shared = {"ident": ident, "sel": sel}
    for l in (0, 1):
        shared.update({f"vecs{l}": vecs[l], f"vecsB{l}": vecs[l], f"w_in{l}": f32(inp["w_in"][l]),
                       f"wa{l}": f32(inp["lru_wa"][l]), f"wx{l}": f32(inp["lru_wx"][l]),
                       f"w_out{l}": f32(inp["w_out"][l])})
    shared.update({"wg0": f32(inp["dense_wg"][0]), "wu0": f32(inp["dense_wu"][0]), "wd0": f32(inp["dense_wd"][0]),
                   "w_router1": f32(inp["w_router"][0]), "wg1": f32(inp["moe_wg"][0]),
                   "wu1": f32(inp["moe_wu"][0]), "wd1": f32(inp["moe_wd"][0])})
    maps = []
    for cidx in range(n_cores):
        b, s = divmod(cidx, nseg)
        xT = np.zeros((D, HALO + T), np.float32)
        if s == 0:
            xT[:, HALO:] = x[b, 0:T, :].T
        else:
            xT[:, :] = x[b, s * T - HALO:(s + 1) * T, :].T
        maps.append(dict(shared, flags=core_flags(s), xTin=xT))
    nc = get_prog(cfg, ["A0", "B0", "A1", "B1"])
    res = run_bass_kernel_spmd(nc, maps, core_ids=list(range(n_cores))).results
    out = np.empty((B, S, D), np.float32)
    for i in range(n_cores):
        b, s = divmod(i, nseg)
        out[b, s * T:(s + 1) * T, :] = res[i]["outT"].T
    return out


FULL_CFG = make_cfg()


def kernel(**inputs):
    return run_fused(FULL_CFG, inputs)
```

```python
import numpy as np
from contextlib import ExitStack
import ml_dtypes
import concourse.bass as bass
import concourse.mybir as mybir
from concourse.bass_utils import run_bass_kernel_spmd

F32 = mybir.dt.float32
BF16 = mybir.dt.bfloat16
AF = mybir.ActivationFunctionType
ALU = mybir.AluOpType
EPS = 1e-6
HALO = 32
CK = 31
LK = 4


def make_cfg(D=2048, T=2048, F=6144, FE=6144, NE=8, W=512):
    c = dict(D=D, KC=D // 128, CW=D // 2, NJ=D // 256, EIN=2 * D, T=T, W=W, F=F, FE=FE, NE=NE,
             TH=T // 2)
    c["PP"] = min(2, c["NJ"])
    c["XG"] = min(4, c["KC"])
    c["DGW"] = min(512, D)
    KC, NJ = c["KC"], c["NJ"]
    off = {}
    p = 0
    for name, n in (("mixn", KC), ("ffnn", KC), ("finn", KC), ("cb", NJ), ("lng", NJ), ("lnb", NJ),
                    ("lcb", NJ), ("ba", NJ), ("bx", NJ), ("lam", NJ), ("cw", NJ * CK), ("lw", NJ * LK)):
        off[name] = p
        p += n
    c["off"] = off
    c["NV"] = p
    return c


class Buf:
    _n = 0

    def __init__(self, ap, kind, name):
        self.ap, self.kind, self.name = ap, kind, name
        self.last_w = None
        self.reads = {}
        self.dcnt = 0
        Buf._n += 1
        self.id = Buf._n

    def __getitem__(self, idx):
        return V(self, self.ap[idx])

    def v(self):
        return V(self, self.ap)


class V:
    def __init__(self, buf, ap):
        self.buf, self.ap = buf, ap

    def __getitem__(self, idx):
        return V(self.buf, self.ap[idx])

    def re(self, s, **kw):
        return V(self.buf, self.ap.rearrange(s, **kw))


class Rot:
    def __init__(self, bufs):
        self.bufs, self.i = bufs, 0

    def next(self):
        b = self.bufs[self.i % len(self.bufs)]
        self.i += 1
        return b


COMPUTE = ("pe", "act", "dve", "pool")
STREAMS = ("pe", "act", "dve", "pool", "sp")


class K:
    def __init__(self, nc, st, arena_words):
        self.nc = nc
        self.arena = st.enter_context(nc.sbuf_tensor("arena", [128, arena_words], F32))[:]
        self.arena_words = arena_words
        self.ptr = 0
        self.ops = []
        self.cnt = {e: 0 for e in COMPUTE}
        self.waited = {e: {} for e in STREAMS}
        self.slot_cnt = []
        self.free_slots = []
        self.live_dbufs = []
        self.cbufs = {}
        self.banks = [Buf(st.enter_context(nc.psum_tensor(f"ps{i}", [128, 512], F32))[:], "psum", f"ps{i}")
                      for i in range(8)]
        self.rot = 0
        self.peak = 0

    def alloc(self, name, shape, dtype):
        n = 1
        for s in shape[1:]:
            n *= s
        words = (n + 1) // 2 if dtype == BF16 else n
        words = (words + 7) // 8 * 8
        off = self.ptr
        self.ptr += words
        self.peak = max(self.peak, self.ptr)
        assert self.ptr <= self.arena_words, f"SBUF arena overflow at {name}: {self.ptr} > {self.arena_words}"
        ap = self.arena[:, off:off + words]
        if dtype == BF16:
            ap = ap.bitcast(BF16)
        ap = ap[:, :n]
        if shape[0] < 128:
            ap = ap[0:shape[0]]
        if len(shape) == 3:
            ap = ap.rearrange("p (a b) -> p a b", a=shape[1])
        elif len(shape) == 4:
            ap = ap.rearrange("p (a b c) -> p a b c", a=shape[1], b=shape[2])
        return Buf(ap, "sbuf", name)

    def bank(self):
        b = self.banks[self.rot % 6]
        self.rot += 1
        return b

    def dram(self, name, shape, dtype, kind):
        return Buf(self.nc.dram_tensor(name, list(shape), dtype, kind=kind).ap(), "dram", name)

    def _collect(self, eng, reads, writes):
        need = {}

        def add(tok):
            if tok is not None:
                need[tok[0]] = max(need.get(tok[0], 0), tok[1])

        for b in reads:
            add(b.last_w)
        for b in writes:
            if b.kind != "dram":
                add(b.last_w)
            for kk, v in b.reads.items():
                add((kk, v))
        out = []
        for kk, v in need.items():
            if kk == eng and eng == "pe":
                continue
            if self.waited[eng].get(kk, 0) >= v:
                continue
            self.waited[eng][kk] = v
            out.append((kk, v))
        return out

    def op(self, eng, fn, reads, writes):
        reads = [r.buf for r in reads if isinstance(r, V)]
        writes = [w.buf for w in writes]
        waits = self._collect(eng, reads, writes)
        n = self.cnt[eng] + 1
        self.cnt[eng] = n
        self.ops.append((eng, fn, waits, (eng, 1)))
        for b in reads:
            b.reads[eng] = max(b.reads.get(eng, 0), n)
        for b in writes:
            b.last_w = (eng, n)
            b.reads = {}

    def dma(self, q, out, in_):
        waits = self._collect(q, [in_.buf], [out.buf])
        dst = out.buf
        if getattr(dst, "dslot", None) is None:
            if self.free_slots:
                dst.dslot = self.free_slots.pop()
            else:
                dst.dslot = len(self.slot_cnt)
                self.slot_cnt.append(0)
            self.live_dbufs.append(dst)
        slot = dst.dslot
        key = ("d", slot)
        self.slot_cnt[slot] += 16
        cntv = self.slot_cnt[slot]
        o_ap, i_ap = out.ap, in_.ap
        self.ops.append((q, lambda e: e.dma_start(out=o_ap, in_=i_ap), waits, (key, 16)))
        in_.buf.reads[key] = max(in_.buf.reads.get(key, 0), cntv)
        dst.last_w = (key, cntv)
        dst.reads = {}

    def collective(self, out_buf, in_buf, groups):
        waits = self._collect("pool", [in_buf], [out_buf])
        key = ("c", out_buf.id)
        self.cbufs[out_buf.id] = out_buf
        o_ap, i_ap = out_buf.ap, in_buf.ap
        self.ops.append(("pool", lambda e: e.collective_compute("AllGather", ALU.bypass, replica_groups=groups,
                                                                  ins=[i_ap], outs=[o_ap]), waits, (key, None)))
        in_buf.reads[key] = 1
        out_buf.last_w = (key, 1)
        out_buf.reads = {}

    def barrier(self):
        allw = [(e, self.cnt[e]) for e in COMPUTE if self.cnt[e] > 0]
        allw += [(("d", i), v) for i, v in enumerate(self.slot_cnt) if v > 0]
        allw += [(("c", b.id), 1) for b in self.cbufs.values()]
        for b in self.live_dbufs:
            self.free_slots.append(b.dslot)
            b.dslot = None
        self.live_dbufs = []
        for e in STREAMS:
            waits = []
            for kk, v in allw:
                if self.waited[e].get(kk, 0) < v:
                    self.waited[e][kk] = v
                    waits.append((kk, v))
            self.ops.append((e, None, waits, None))

    @staticmethod
    def _a(x):
        return x.ap if isinstance(x, V) else x

    def mm(self, out, lhsT, rhs, start, stop):
        o, l, r = out.ap, lhsT.ap, rhs.ap
        self.op("pe", lambda e: e.matmul(o, l, r, start=start, stop=stop), [lhsT, rhs], [out])

    def act(self, out, in_, func, bias=None, scale=None):
        o, i = out.ap, in_.ap
        kw = {}
        if bias is not None:
            kw["bias"] = self._a(bias)
        if scale is not None:
            kw["scale"] = self._a(scale)
        self.op("act", lambda e: e.activation(out=o, in_=i, func=func, **kw), [in_, bias, scale], [out])

    def tt(self, out, in0, in1, op, eng="dve"):
        o, a, b = out.ap, in0.ap, in1.ap
        self.op(eng, lambda e: e.tensor_tensor(out=o, in0=a, in1=b, op=op), [in0, in1], [out])

    def stt(self, out, in0, scalar, in1, op0, op1):
        o, a, s, b = out.ap, in0.ap, self._a(scalar), in1.ap
        self.op("dve", lambda e: e.scalar_tensor_tensor(out=o, in0=a, scalar=s, in1=b, op0=op0, op1=op1),
                [in0, scalar, in1], [out])

    def ts(self, out, in0, s1, s2, op0, op1=None, eng="dve"):
        o, a, x1, x2 = out.ap, in0.ap, self._a(s1), self._a(s2)
        if op1 is None:
            fn = lambda e: e.tensor_scalar(out=o, in0=a, scalar1=x1, scalar2=None, op0=op0)
        else:
            fn = lambda e: e.tensor_scalar(out=o, in0=a, scalar1=x1, scalar2=x2, op0=op0, op1=op1)
        self.op(eng, fn, [in0, s1, s2], [out])

    def scan(self, out, d0, d1, initial, op0, op1):
        o, a, b, i = out.ap, d0.ap, d1.ap, self._a(initial)
        self.op("dve", lambda e: e.tensor_tensor_scan(out=o, data0=a, data1=b, initial=i, op0=op0, op1=op1),
                [d0, d1, initial], [out])

    def copy(self, out, in_, eng="dve"):
        o, i = out.ap, in_.ap
        self.op(eng, lambda e: e.tensor_copy(out=o, in_=i), [in_], [out])

    def recip(self, out, in_):
        o, i = out.ap, in_.ap
        self.op("dve", lambda e: e.reciprocal(out=o, in_=i), [in_], [out])

    def memset(self, v, val, eng="dve"):
        a = v.ap
        self.op(eng, lambda e: e.memset(a, val), [], [v])

    def vmax(self, out, in_):
        o, i = out.ap, in_.ap
        self.op("dve", lambda e: e.max(out=o, in_=i), [in_], [out])

    def emit(self, final_bufs):
        nc = self.nc
        waits = [b.last_w for b in final_bufs if b.last_w is not None]
        self.ops.append(("sp", None, waits, None))
        with ExitStack() as st:
            sems = {}
            for e in COMPUTE:
                sems[e] = st.enter_context(nc.semaphore("s_" + e))
            print("free sems", nc.free_len(), "dma slots", len(self.slot_cnt))
            for i in range(len(self.slot_cnt)):
                sems[("d", i)] = st.enter_context(nc.semaphore(f"d{i}"))
            for b in self.cbufs.values():
                sems[("c", b.id)] = st.enter_context(nc.semaphore(f"c{b.id}"))
            block = st.enter_context(nc.Block())
            ops = self.ops

            def mk(name):
                def body(eng):
                    for (e, fn, waits, inc) in ops:
                        if e != name:
                            continue
                        for (kk, v) in waits:
                            eng.wait_ge(sems[kk], v)
                        if fn is None:
                            continue
                        ins = fn(eng)
                        if inc[1] is None:
                            ins.then_inc(sems[inc[0]])
                        else:
                            ins.then_inc(sems[inc[0]], inc[1])
                return body

            block.tensor(mk("pe"))
            block.scalar(mk("act"))
            block.vector(mk("dve"))
            block.gpsimd(mk("pool"))
            block.sync(mk("sp"))


def setup_consts(k, cfg, din):
    c = {}
    c["ones"] = k.alloc("ones", [128, 128], BF16)
    k.memset(c["ones"].v(), 1.0)
    c["zeros"] = k.alloc("zeros", [128, 512], F32)
    k.memset(c["zeros"].v(), 0.0)
    c["ident"] = k.alloc("ident", [128, 128], F32)
    k.dma("sp", c["ident"].v(), din["ident"].v())
    c["sel"] = k.alloc("sel", [8, 8 * 128], F32)
    k.dma("sp", c["sel"].v(), din["sel"].v())
    c["flags"] = k.alloc("flags", [128, 8], F32)
    k.dma("sp", c["flags"].v(), din["flags"].v())
    k.persist = k.ptr
    return c


def rows(buf, r0, nr, c0, nc_):
    return V(buf, buf.ap[r0:r0 + nr, c0:c0 + nc_].rearrange("(k p) t -> p k t", p=128))


def stage_A(k, cfg, c, dw, xmain, xoff, xhalo, xhoff, yT, YL, PG, cst, halo_gathered=False):
    D, KC, NJ, T, W, CW = cfg["D"], cfg["KC"], cfg["NJ"], cfg["T"], cfg["W"], cfg["CW"]
    PP, XG, off = cfg["PP"], cfg["XG"], cfg["off"]
    k.ptr = k.persist
    vec = k.alloc("vec", [128, cfg["NV"]], F32)
    k.dma("sp", vec.v(), dw["vecs"].v())
    wa = k.alloc("wa", [128, NJ, 128], BF16)
    wx = k.alloc("wx", [128, NJ, 128], BF16)
    k.dma("pool", wa.v(), dw["wa"].v().re("h i j -> i h j"))
    k.dma("pool", wx.v(), dw["wx"].v().re("h i j -> i h j"))

    def vcol(name, i, n=1):
        return vec[:, off[name] + i: off[name] + i + n]

    sm = k.alloc("sm", [128, 10 * NJ], F32)
    s_ = [sm[:, i * NJ:(i + 1) * NJ] for i in range(10)]
    e_, ln_, t1, msk, t2, spv, nsp, nsp2 = s_[0], s_[1], s_[2], s_[3], s_[4], s_[5], s_[6], s_[7]
    lam = vcol("lam", 0, NJ)
    k.act(e_, lam, AF.Exp, scale=-1.0)
    k.act(ln_, e_, AF.Ln, bias=1.0)
    k.ts(t1, e_, -1.0 / 3.0, 0.5, ALU.mult, ALU.add)
    k.tt(t1, t1, e_, ALU.mult)
    k.ts(t1, t1, -1.0, 1.0, ALU.mult, ALU.add)
    k.tt(t1, t1, e_, ALU.mult)
    k.ts(msk, e_, 0.05, None, ALU.is_lt)
    k.tt(t2, t1, ln_, ALU.subtract)
    k.tt(t2, t2, msk, ALU.mult)
    k.tt(spv, t2, ln_, ALU.add)
    k.ts(nsp, spv, -8.0, None, ALU.mult)
    k.ts(nsp2, spv, -16.0, None, ALU.mult)

    hst = k.alloc("hst", [128, NJ], F32)
    pst = k.alloc("pst", [128, NJ], F32)
    k.memset(hst.v(), 0.0)
    k.memset(pst.v(), 1.0)
    chalo = [k.alloc(f"chalo{j}", [128, HALO], BF16) for j in range(NJ)]
    rhalo = [k.alloc(f"rhalo{j}", [128, HALO], F32) for j in range(NJ)]

    xt = [k.alloc(f"xt{g}", [128, XG, W], F32) for g in range(KC // XG)]
    if halo_gathered:
        xhp = [k.alloc(f"xhp{i}", [128, XG, HALO], F32) for i in range(3)]
    hT = [k.alloc(f"hT{i}", [128, W], BF16) for i in range(KC)]
    xth = [k.alloc(f"xth{g}", [128, XG, HALO], F32) for g in range(KC // XG)]
    hTh = [k.alloc(f"hTh{i}", [128, HALO], BF16) for i in range(KC)]
    sqp = Rot([k.alloc(f"sq{i}", [128, W], BF16) for i in range(2)])
    bfp = Rot([k.alloc(f"bfp{i}", [128, W], BF16) for i in range(4)])
    wtA = Rot([k.alloc(f"wtA{i}", [128, KC, PP * 128], BF16) for i in range(2)])
    wtB = Rot([k.alloc(f"wtB{i}", [128, KC, PP * 128], BF16) for i in range(2)])
    cwp = Rot([k.alloc(f"cw{i}", [128, HALO + W], BF16) for i in range(2)])
    dgp = Rot([k.alloc(f"dg{i}", [128, CK, 128], BF16) for i in range(2)])
    identb = k.alloc("identb", [128, 128], BF16)
    k.copy(identb.v(), c["ident"].v())
    rwp = Rot([k.alloc(f"rw{i}", [128, HALO + W], F32) for i in range(2)])
    ccb = [k.alloc(f"cc{j}", [128, W], F32) for j in range(NJ)]
    fp = Rot([k.alloc(f"fp{i}", [128, W], F32) for i in range(20)])
    rstd = k.alloc("rstd", [128, W], F32)
    mu = k.alloc("mu", [128, W], F32)
    rs2 = k.alloc("rs2", [128, W], F32)
    ycv = Rot([k.alloc(f"ycv{i}", [128, NJ, W], BF16) for i in range(2)])
    ps_ss, ps_s1, ps_s2 = k.banks[6], k.banks[6], k.banks[7]
    ones = c["ones"].v()
    flags = c["flags"]
    w_in = dw["w_in"]

    def lru_part2(j, ti, gx, rc, ml, a_, G):
        bb = fp.next()
        k.tt(bb[:, :], gx[:, :], rc[:, :], ALU.mult)
        k.tt(bb[:, :], bb[:, :], ml[:, :], ALU.mult)
        hl, Pc = fp.next(), fp.next()
        k.scan(hl[:, :], a_[:, :], bb[:, :], hst[:, j:j + 1], ALU.mult, ALU.add)
        k.copy(hst[:, j:j + 1], hl[:, W - 1:W])
        k.scan(Pc[:, :], a_[:, :], c["zeros"][:, :W], pst[:, j:j + 1], ALU.mult, ALU.add)
        k.copy(pst[:, j:j + 1], Pc[:, W - 1:W])
        k.tt(hl[:, :], hl[:, :], G[:, :], ALU.mult)
        k.tt(Pc[:, :], Pc[:, :], G[:, :], ALU.mult)
        k.dma("sp", YL[j * 128:(j + 1) * 128, ti * W:(ti + 1) * W], hl[:, :])
        k.dma("sp", PG[j * 128:(j + 1) * 128, ti * W:(ti + 1) * W], Pc[:, :])

    def conv_part2(j, dg, cw):
        pcv = k.bank()
        for kk in range(CK):
            k.mm(pcv[:, :W], dg[:, kk, :], cw[:, 2 + kk:2 + kk + W], kk == 0, kk == CK - 1)
        k.copy(chalo[j][:, :], cw[:, W:W + HALO])
        cc = ccb[j]
        k.act(cc[:, :], pcv[:, :W], AF.Identity, bias=vcol("cb", j))
        b1, b2 = bfp.next(), bfp.next()
        k.act(b1[:, :], cc[:, :], AF.Identity)
        k.act(b2[:, :], cc[:, :], AF.Square)
        k.mm(ps_s1[:, :W], ones, b1[:, :], j == 0, j == NJ - 1)
        k.mm(ps_s2[:, :W], ones, b2[:, :], j == 0, j == NJ - 1)

    pend_c = None
    pend = None
    grps = [[("halo", 0, HALO), ("main", 0, W)]] + [[("main", i, W)] for i in range(1, T // W)]
    for grp in grps:
      for kind, ti, w in grp:
        xt_, hT_ = (xth, hTh) if kind == "halo" else (xt, hT)
        for g in range(KC // XG):
            if kind == "halo" and halo_gathered:
                for jj in range(3):
                    k.dma("sp", xhp[jj].v(), rows(xhalo, jj * D + g * XG * 128, XG * 128, 0, w))
                k.ts(xt_[g][:, :, 0:w], xhp[0].v(), flags[:, 5:6], None, ALU.mult)
                k.stt(xt_[g][:, :, 0:w], xhp[1].v(), flags[:, 6:7], xt_[g][:, :, 0:w], ALU.mult, ALU.add)
                k.stt(xt_[g][:, :, 0:w], xhp[2].v(), flags[:, 7:8], xt_[g][:, :, 0:w], ALU.mult, ALU.add)
                continue
            if kind == "halo":
                src = rows(xhalo, g * XG * 128, XG * 128, xhoff, w)
            else:
                src = rows(xmain, g * XG * 128, XG * 128, xoff + ti * W, w)
            k.dma("sp", xt_[g][:, :, 0:w], src)
        for kc in range(KC):
            sq = sqp.next()
            k.act(sq[:, :w], xt_[kc // XG][:, kc % XG, 0:w], AF.Square)
            k.mm(ps_ss[:, :w], ones, sq[:, :w], kc == 0, kc == KC - 1)
        rt = fp.next()
        k.act(rt[:, :w], ps_ss[:, :w], AF.Sqrt, bias=EPS, scale=1.0 / D)
        k.recip(rstd[:, :w], rt[:, :w])
        for kc in range(KC):
            k.stt(hT_[kc][:, :w], xt_[kc // XG][:, kc % XG, 0:w], vcol("mixn", kc), rstd[:, :w],
                  ALU.mult, ALU.mult)

      for branch in (0, 1):
            for q in range(NJ // PP):
                A, B = wtA.next(), wtB.next()
                ca0 = branch * 2 * CW + q * PP * 128
                cb0 = ca0 + CW
                k.dma("pool", A.v(), rows(w_in, 0, D, ca0, PP * 128))
                k.dma("pool", B.v(), rows(w_in, 0, D, cb0, PP * 128))
                for pr in range(PP):
                    j = q * PP + pr
                    for kind, ti, w in grp:
                        hT_ = hTh if kind == "halo" else hT
                        psa, psb = k.bank(), k.bank()
                        for kc in range(KC):
                            k.mm(psa[:, :w], A[:, kc, pr * 128:(pr + 1) * 128], hT_[kc][:, :w], kc == 0, kc == KC - 1)
                        need_b = not (branch == 1 and kind == "halo")
                        if need_b:
                            for kc in range(KC):
                                k.mm(psb[:, :w], B[:, kc, pr * 128:(pr + 1) * 128], hT_[kc][:, :w], kc == 0,
                                     kc == KC - 1)
                        if branch == 0:
                            sgt = fp.next()
                            k.act(sgt[:, :w], psb[:, :w], AF.Sigmoid)
                            if kind == "halo":
                                k.tt(chalo[j][:, :], psa[:, :w], sgt[:, :w], ALU.mult)
                                continue
                            cw = cwp.next()
                            k.copy(cw[:, 0:HALO], chalo[j][:, :])
                            k.tt(cw[:, HALO:HALO + W], psa[:, :W], sgt[:, :W], ALU.mult)
                            dg = dgp.next()
                            for kk in range(CK):
                                k.ts(dg[:, kk, :], identb[:, :], vcol("cw", j * CK + kk), None, ALU.mult)
                            if pend_c is not None:
                                conv_part2(*pend_c)
                            pend_c = (j, dg, cw)
                        else:
                            if kind == "halo":
                                k.act(rhalo[j][:, :], psa[:, :w], AF.Identity)
                                continue
                            rw = rwp.next()
                            k.copy(rw[:, 0:HALO], rhalo[j][:, :])
                            k.act(rw[:, HALO:HALO + W], psa[:, :W], AF.Identity)
                            G = fp.next()
                            k.act(G[:, :], psb[:, :W], AF.Gelu_apprx_tanh)
                            rc = fp.next()
                            k.ts(rc[:, :], rw[:, HALO - 3:HALO - 3 + W], vcol("lw", j * LK), vcol("lcb", j),
                                 ALU.mult, ALU.add)
                            for kk in range(1, LK):
                                k.stt(rc[:, :], rw[:, HALO - 3 + kk:HALO - 3 + kk + W], vcol("lw", j * LK + kk),
                                      rc[:, :], ALU.mult, ALU.add)
                            k.copy(rhalo[j][:, :], rw[:, W:W + HALO])
                            rcb = bfp.next()
                            k.act(rcb[:, :], rc[:, :], AF.Identity)
                            pga, pgx = k.bank(), k.bank()
                            k.mm(pga[:, :W], wa[:, j, :], rcb[:, :], True, True)
                            k.mm(pgx[:, :W], wx[:, j, :], rcb[:, :], True, True)
                            ga, gx, a_, a2, ml = fp.next(), fp.next(), fp.next(), fp.next(), fp.next()
                            k.act(ga[:, :], pga[:, :W], AF.Sigmoid, bias=vcol("ba", j))
                            k.act(gx[:, :], pgx[:, :W], AF.Sigmoid, bias=vcol("bx", j))
                            k.act(a_[:, :], ga[:, :], AF.Exp, scale=nsp[:, j:j + 1])
                            k.act(a2[:, :], ga[:, :], AF.Exp, scale=nsp2[:, j:j + 1])
                            k.act(ml[:, :], a2[:, :], AF.Sqrt, bias=1.0, scale=-1.0)
                            if ti == 0:
                                k.ts(ml[:, 0:1], ml[:, 0:1], flags[:, 4:5], flags[:, 3:4], ALU.mult, ALU.add)
                            if pend is not None:
                                lru_part2(*pend)
                            pend = (j, ti, gx, rc, ml, a_, G)
            if branch == 1 and pend is not None:
                lru_part2(*pend)
                pend = None
            if branch == 0:
                if pend_c is not None:
                    conv_part2(*pend_c)
                    pend_c = None
                ti = grp[-1][1]
                mu2, var, sd = fp.next(), fp.next(), fp.next()
                k.act(mu[:, :], ps_s1[:, :W], AF.Identity, scale=1.0 / CW)
                k.tt(mu2[:, :], mu[:, :], mu[:, :], ALU.mult)
                k.stt(var[:, :], ps_s2[:, :W], 1.0 / CW, mu2[:, :], ALU.mult, ALU.subtract)
                k.act(sd[:, :], var[:, :], AF.Sqrt, bias=EPS)
                k.recip(rs2[:, :], sd[:, :])
                yc = ycv.next()
                for j in range(NJ):
                    t_ = fp.next()
                    k.tt(t_[:, :], ccb[j][:, :], mu[:, :], ALU.subtract)
                    k.tt(t_[:, :], t_[:, :], rs2[:, :], ALU.mult)
                    k.act(yc[:, j, :], t_[:, :], AF.Silu, bias=vcol("lnb", j), scale=vcol("lng", j))
                k.dma("sp", rows(yT, 0, CW, ti * W, W), yc.v())
    k.dma("sp", cst[0:128, :], pst.v())
    k.dma("sp", cst[128:256, :], hst.v())


def stage_B(k, cfg, c, dw, xres, xroff, yT, YL, PG, carr, xout, xlast, outT, is_moe, is_last):
    D, KC, NJ, T, W, CW, TH = cfg["D"], cfg["KC"], cfg["NJ"], cfg["T"], cfg["W"], cfg["CW"], cfg["TH"]
    off, DGW = cfg["off"], cfg["DGW"]
    NE = cfg["NE"] if is_moe else 1
    F = cfg["FE"] if is_moe else cfg["F"]
    NTH = TH // W
    k.ptr = k.persist
    vec = k.alloc("vec", [128, cfg["NV"]], F32)
    k.dma("sp", vec.v(), dw["vecs"].v())

    def vcol(name, i, n=1):
        return vec[:, off[name] + i: off[name] + i + n]

    flags = c["flags"]
    ones = c["ones"].v()
    ca = k.alloc("ca", [128, 8, NJ], F32)
    k.dma("sp", ca.v(), carr.v().re("(sa p) j -> p sa j", p=128))
    carry = k.alloc("carry", [128, NJ], F32)
    ctmp = k.alloc("ctmp", [128, NJ], F32)
    k.memset(carry.v(), 0.0)
    for s in range(3):
        k.tt(ctmp[:, :], ca[:, 2 * s, :], carry[:, :], ALU.mult)
        k.tt(ctmp[:, :], ctmp[:, :], ca[:, 2 * s + 1, :], ALU.add)
        k.tt(ctmp[:, :], ctmp[:, :], carry[:, :], ALU.subtract)
        k.stt(carry[:, :], ctmp[:, :], flags[:, s:s + 1], carry[:, :], ALU.mult, ALU.add)

    if is_moe:
        wr = k.alloc("wr", [128, KC, 8], F32)
        k.dma("sp", wr.v(), dw["w_router"].v().re("(kc p) e -> p kc e", p=128))
        lgT = k.alloc("lgT", [8, TH], F32)
        gwT = k.alloc("gwT", [8, TH], F32)
        smalp = Rot([k.alloc(f"smal{i}", [128, 64], F32) for i in range(4)])
    h2T = [k.alloc(f"h2T{i}", [128, TH], BF16) for i in range(KC)]
    acc = [k.alloc(f"acc{i}", [128, TH], F32) for i in range(KC)]
    base = k.ptr
    ps_ss = k.banks[6]

    for hh in range(2):
        k.barrier()
        k.ptr = base
        assert NTH <= 2
        yts = [k.alloc(f"yt{t}", [128, 2 * NJ, W], BF16) for t in range(NTH)]
        ylp = Rot([k.alloc(f"yl{i}", [128, W], F32) for i in range(2)])
        pgp = Rot([k.alloc(f"pg{i}", [128, W], F32) for i in range(2)])
        wop = Rot([k.alloc(f"wo{i}", [128, 2 * NJ, DGW], BF16) for i in range(2)])
        sqp = Rot([k.alloc(f"sqb{i}", [128, W], BF16) for i in range(2)])
        rt = k.alloc("rtb", [128, W], F32)
        rstd = k.alloc("rstdb", [128, W], F32)
        hfp = Rot([k.alloc(f"hf{i}", [128, W], F32) for i in range(2)])
        ps_st = [k.banks[6], k.banks[7]]
        for tt in range(NTH):
            col = hh * TH + tt * W
            lc = tt * W
            yt = yts[tt]
            for d in range(KC):
                k.dma("sp", acc[d][:, lc:lc + W], xres[d * 128:(d + 1) * 128, xroff + col:xroff + col + W])
            k.dma("sp", yt[:, 0:NJ, :], rows(yT, 0, CW, col, W))
            for j in range(NJ):
                yl, pg = ylp.next(), pgp.next()
                k.dma("sp", yl[:, :], YL[j * 128:(j + 1) * 128, col:col + W])
                k.dma("sp", pg[:, :], PG[j * 128:(j + 1) * 128, col:col + W])
                k.stt(yt[:, NJ + j, :], pg[:, :], carry[:, j:j + 1], yl[:, :], ALU.mult, ALU.add)
        for dg in range(D // DGW):
            wo = wop.next()
            k.dma("pool", wo.v(), rows(dw["w_out"], 0, D, dg * DGW, DGW))
            for dc in range(DGW // 128):
                d = dg * (DGW // 128) + dc
                for tt in range(NTH):
                    lc = tt * W
                    ps = k.bank()
                    for e in range(2 * NJ):
                        k.mm(ps[:, :W], wo[:, e, dc * 128:(dc + 1) * 128], yts[tt][:, e, :], e == 0, e == 2 * NJ - 1)
                    k.tt(acc[d][:, lc:lc + W], ps[:, :W], acc[d][:, lc:lc + W], ALU.add)
                    sq = sqp.next()
                    k.act(sq[:, :], acc[d][:, lc:lc + W], AF.Square)
                    k.mm(ps_st[tt][:, :W], ones, sq[:, :], d == 0, d == KC - 1)
        for tt in range(NTH):
            lc = tt * W
            k.act(rt[:, :], ps_st[tt][:, :W], AF.Sqrt, bias=EPS, scale=1.0 / D)
            k.recip(rstd[:, :], rt[:, :])
            for kc in range(KC):
                k.stt(h2T[kc][:, lc:lc + W], acc[kc][:, lc:lc + W], vcol("ffnn", kc), rstd[:, :],
                      ALU.mult, ALU.mult)
            if is_moe:
                pl = k.bank()
                for kc in range(KC):
                    hf = hfp.next()
                    k.stt(hf[:, :], acc[kc][:, lc:lc + W], vcol("ffnn", kc), rstd[:, :], ALU.mult, ALU.mult)
                    k.mm(pl[0:8, :W], wr[:, kc, :], hf[:, :], kc == 0, kc == KC - 1)
                k.act(lgT[0:8, lc:lc + W], pl[0:8, :W], AF.Identity)
        if is_moe:
            for blk in range(TH // 128):
                sl = slice(blk * 128, (blk + 1) * 128)
                smal = smalp.next()
                lg, m8, msk, nl1 = smal[:, 0:8], smal[:, 8:16], smal[:, 16:24], smal[:, 24:25]
                ex, e2, den, rden, gw = smal[:, 32:40], smal[:, 25:26], smal[:, 26:27], smal[:, 27:28], smal[:, 40:48]
                pl = k.bank()
                k.mm(pl[:, 0:8], lgT[0:8, sl], c["ident"][0:8, 0:8], True, True)
                k.act(lg, pl[:, 0:8], AF.Identity)
                k.vmax(m8, lg)
                k.ts(msk, lg, m8[:, 1:2], None, ALU.is_ge)
                k.ts(nl1, m8[:, 0:1], -1.0, None, ALU.mult)
                k.act(ex, lg, AF.Exp, bias=nl1)
                k.act(e2, m8[:, 1:2], AF.Exp, bias=nl1)
                k.ts(den, e2, 1.0, None, ALU.add)
                k.recip(rden, den)
                k.stt(gw, ex, rden, msk, ALU.mult, ALU.mult)
                pt = k.bank()
                k.mm(pt[0:8, 0:128], gw, c["ident"][:, :], True, True)
                k.act(gwT[0:8, sl], pt[0:8, 0:128], AF.Identity)

        k.barrier()
        k.ptr = base
        wgp = Rot([k.alloc(f"wg{i}", [128, KC, 256], BF16) for i in range(2)])
        wup = Rot([k.alloc(f"wu{i}", [128, KC, 256], BF16) for i in range(2)])
        wdp = Rot([k.alloc(f"wd{i}", [128, 4, D], BF16) for i in range(2)])
        actT = [k.alloc(f"actT{i}", [128, TH], BF16) for i in range(4)]
        sgp = Rot([k.alloc(f"sg{i}", [128, W], F32) for i in range(2)])
        tmpp = Rot([k.alloc(f"tm{i}", [128, W], F32) for i in range(2)])
        if is_moe:
            gwBp = Rot([k.alloc(f"gwB{i}", [128, TH], F32) for i in range(2)])
        for ex_i in range(NE):
            if is_moe:
                wg_d, wu_d, wd_d = (V(dw[n], dw[n].ap[ex_i]) for n in ("wg", "wu", "wd"))
                gwB = gwBp.next()
                for tt in range(NTH):
                    pb = k.bank()
                    k.mm(pb[:, :W], c["sel"][0:8, ex_i * 128:(ex_i + 1) * 128], gwT[0:8, tt * W:(tt + 1) * W],
                         True, True)
                    k.act(gwB[:, tt * W:(tt + 1) * W], pb[:, :W], AF.Identity)
            else:
                wg_d, wu_d, wd_d = (dw[n].v() for n in ("wg", "wu", "wd"))
            for g in range(F // 512):
                wd_t = wdp.next()
                k.dma("pool", wd_t.v(), wd_d[g * 512:(g + 1) * 512, :].re("(f p) d -> p f d", p=128))
                for pr in range(2):
                    wg_t, wu_t = wgp.next(), wup.next()
                    f0 = g * 512 + pr * 256
                    k.dma("pool", wg_t.v(), wg_d[:, f0:f0 + 256].re("(kc p) f -> p kc f", p=128))
                    k.dma("pool", wu_t.v(), wu_d[:, f0:f0 + 256].re("(kc p) f -> p kc f", p=128))
                    for fc2 in range(2):
                        fc = pr * 2 + fc2
                        for tt in range(NTH):
                            cs = slice(tt * W, (tt + 1) * W)
                            pg_, pu_ = k.bank(), k.bank()
                            for kc in range(KC):
                                k.mm(pg_[:, :W], wg_t[:, kc, fc2 * 128:(fc2 + 1) * 128], h2T[kc][:, cs], kc == 0,
                                     kc == KC - 1)
                            for kc in range(KC):
                                k.mm(pu_[:, :W], wu_t[:, kc, fc2 * 128:(fc2 + 1) * 128], h2T[kc][:, cs], kc == 0,
                                     kc == KC - 1)
                            sg = sgp.next()
                            k.act(sg[:, :], pg_[:, :W], AF.Silu)
                            if is_moe:
                                tm = tmpp.next()
                                k.tt(tm[:, :], pu_[:, :W], sg[:, :], ALU.mult)
                                k.tt(actT[fc][:, cs], tm[:, :], gwB[:, cs], ALU.mult)
                            else:
                                k.tt(actT[fc][:, cs], pu_[:, :W], sg[:, :], ALU.mult)
                for d in range(KC):
                    for tt in range(NTH):
                        cs = slice(tt * W, (tt + 1) * W)
                        pd = k.bank()
                        for fc in range(4):
                            k.mm(pd[:, :W], wd_t[:, fc, d * 128:(d + 1) * 128], actT[fc][:, cs], fc == 0, fc == 3)
                        k.tt(acc[d][:, cs], pd[:, :W], acc[d][:, cs], ALU.add)
        if not is_last:
            for d in range(KC):
                k.dma("sp", xout[d * 128:(d + 1) * 128, hh * TH:(hh + 1) * TH], acc[d][:, :])
                if hh == 1:
                    k.dma("sp", xlast[d * 128:(d + 1) * 128, :], acc[d][:, TH - HALO:TH])
        else:
            k.barrier()
            k.ptr = base
            sqf = Rot([k.alloc(f"sqf{i}", [128, W], BF16) for i in range(2)])
            otp = Rot([k.alloc(f"ot{i}", [128, W], F32) for i in range(2)])
            rtf = k.alloc("rtf", [128, W], F32)
            rsf = k.alloc("rsf", [128, W], F32)
            for tt in range(NTH):
                cs = slice(tt * W, (tt + 1) * W)
                for d in range(KC):
                    sq = sqf.next()
                    k.act(sq[:, :], acc[d][:, cs], AF.Square)
                    k.mm(ps_ss[:, :W], ones, sq[:, :], d == 0, d == KC - 1)
                k.act(rtf[:, :], ps_ss[:, :W], AF.Sqrt, bias=EPS, scale=1.0 / D)
                k.recip(rsf[:, :], rtf[:, :])
                for d in range(KC):
                    ot = otp.next()
                    k.stt(ot[:, :], acc[d][:, cs], vcol("finn", d), rsf[:, :], ALU.mult, ALU.mult)
                    k.dma("sp", outT[d * 128:(d + 1) * 128, hh * TH + tt * W: hh * TH + (tt + 1) * W], ot[:, :])


ARENA_WORDS = 52800


def build_program(cfg, stages):
    nc = bass.Bass("TRN2", target_bir_lowering=False)
    D, KC, NJ, T, CW = cfg["D"], cfg["KC"], cfg["NJ"], cfg["T"], cfg["CW"]
    fused = len(stages) == 4
    groups = [[0, 1, 2, 3], [4, 5, 6, 7]]
    final_bufs = []
    with ExitStack() as st:
        k = K(nc, st, ARENA_WORDS)

        def ext_in(name, shape, dtype=F32):
            return k.dram(name, shape, dtype, "ExternalInput")

        def inter(name, shape, dtype, producer_stage, consumer_stages):
            if producer_stage in stages:
                if all(s in stages for s in consumer_stages):
                    b = k.dram(name, shape, dtype, "Internal")
                else:
                    b = k.dram(name, shape, dtype, "ExternalOutput")
                    final_bufs.append(b)
                return b
            if any(s in stages for s in consumer_stages):
                return ext_in(name, shape, dtype)
            return None

        din = {"ident": ext_in("ident", [128, 128]), "sel": ext_in("sel", [8, 1024]),
               "flags": ext_in("flags", [128, 8])}
        c = setup_consts(k, cfg, din)
        xTin = ext_in("xTin", [D, HALO + T]) if ("A0" in stages or "B0" in stages) else None
        t = {}
        for l in (0, 1):
            t[f"yT{l}"] = inter(f"yT{l}", [CW, T], BF16, f"A{l}", [f"B{l}"])
            t[f"YL{l}"] = inter(f"YL{l}", [CW, T], F32, f"A{l}", [f"B{l}"])
            t[f"PG{l}"] = inter(f"PG{l}", [CW, T], F32, f"A{l}", [f"B{l}"])
            if fused:
                t[f"cst{l}"] = k.dram(f"cst{l}", [256, NJ], F32, "Internal")
                t[f"carr{l}"] = k.dram(f"carr{l}", [1024, NJ], F32, "Internal")
            else:
                t[f"cst{l}"] = inter(f"cst{l}", [256, NJ], F32, f"A{l}", ["host"])
                t[f"carr{l}"] = ext_in(f"carr{l}", [1024, NJ]) if f"B{l}" in stages else None
        t["xT1"] = inter("xT1", [D, T], F32, "B0", ["A1", "B1"])
        if fused:
            t["xlast"] = k.dram("xlast", [D, HALO], F32, "Internal")
            t["xh1"] = k.dram("xhall", [4 * D, HALO], F32, "Internal")
        else:
            t["xlast"] = inter("xlast", [D, HALO], F32, "B0", ["host"])
            t["xh1"] = ext_in("xh1", [D, HALO]) if "A1" in stages else None
        if "B1" in stages:
            outT = k.dram("outT", [D, T], F32, "ExternalOutput")
            final_bufs.append(outT)

        def layer_w(l, names):
            return {n: ext_in(f"{n}{l}", shp, F32) for n, shp in names}

        for sname in stages:
            l = int(sname[1])
            k.barrier()
            if sname[0] == "A":
                dw = layer_w(l, [("vecs", [128, cfg["NV"]]), ("w_in", [D, cfg["EIN"]]),
                                 ("wa", [NJ, 128, 128]), ("wx", [NJ, 128, 128])])
                if l == 0:
                    stage_A(k, cfg, c, dw, xTin, HALO, xTin, 0, t["yT0"], t["YL0"], t["PG0"], t["cst0"])
                else:
                    stage_A(k, cfg, c, dw, t["xT1"], 0, t["xh1"], 0, t["yT1"], t["YL1"], t["PG1"], t["cst1"],
                            halo_gathered=fused)
                if fused:
                    k.collective(t[f"carr{l}"], t[f"cst{l}"], groups)
            else:
                names = [("vecsB", [128, cfg["NV"]]), ("w_out", [D, D])]
                if l == 0:
                    names += [("wg", [D, cfg["F"]]), ("wu", [D, cfg["F"]]), ("wd", [cfg["F"], D])]
                else:
                    names += [("w_router", [D, 8]), ("wg", [cfg["NE"], D, cfg["FE"]]),
                              ("wu", [cfg["NE"], D, cfg["FE"]]), ("wd", [cfg["NE"], cfg["FE"], D])]
                dw = layer_w(l, names)
                dw["vecs"] = dw["vecsB"]
                if l == 0:
                    stage_B(k, cfg, c, dw, xTin, HALO, t["yT0"], t["YL0"], t["PG0"], t["carr0"],
                            t["xT1"], t["xlast"], None, False, False)
                    if fused:
                        k.collective(t["xh1"], t["xlast"], groups)
                else:
                    stage_B(k, cfg, c, dw, t["xT1"], 0, t["yT1"], t["YL1"], t["PG1"], t["carr1"],
                            None, None, outT, True, True)
        k.emit(final_bufs)
        build_program.last_peak = k.peak
    return nc


def vec2d(v, n):
    return np.ascontiguousarray(np.asarray(v, np.float32).reshape(n, 128).T)


def pack_vecs(cfg, inp, l):
    KC, NJ = cfg["KC"], cfg["NJ"]
    cw = np.asarray(inp["conv_w"][l], np.float32)
    lw = np.asarray(inp["lru_conv_w"][l], np.float32)
    parts = [vec2d(inp["mix_norm"][l], KC), vec2d(inp["ffn_norm"][l], KC), vec2d(inp["final_norm"], KC),
             vec2d(inp["conv_b"][l], NJ), vec2d(inp["conv_ln_g"][l], NJ), vec2d(inp["conv_ln_b"][l], NJ),
             vec2d(inp["lru_conv_b"][l], NJ), vec2d(inp["lru_ba"][l], NJ), vec2d(inp["lru_bx"][l], NJ),
             vec2d(inp["lru_lambda"][l], NJ),
             cw.reshape(CK, NJ, 128).transpose(2, 1, 0).reshape(128, NJ * CK),
             lw.reshape(LK, NJ, 128).transpose(2, 1, 0).reshape(128, NJ * LK)]
    return np.ascontiguousarray(np.concatenate(parts, axis=1).astype(np.float32))


def host_consts():
    ident = np.eye(128, dtype=np.float32)
    sel = np.zeros((8, 8, 128), np.float32)
    for e in range(8):
        sel[e, e, :] = 1.0
    return ident, sel.reshape(8, 1024)


def core_flags(s):
    f = np.zeros((128, 8), np.float32)
    for j in range(3):
        f[:, j] = 1.0 if j < s else 0.0
    f[:, 3] = 1.0 if s == 0 else 0.0
    f[:, 4] = 0.0 if s == 0 else 1.0
    for j in range(3):
        f[:, 5 + j] = 1.0 if j == s - 1 else 0.0
    return f


_PROG_CACHE = {}


def get_prog(cfg, stages):
    key = (cfg["D"], cfg["T"], cfg["F"], cfg["FE"], cfg["W"], tuple(stages))
    if key not in _PROG_CACHE:
        _PROG_CACHE[key] = build_program(cfg, list(stages))
    return _PROG_CACHE[key]


def run_unfused(cfg, inp, n_cores=8):
    D, T, NJ = cfg["D"], cfg["T"], cfg["NJ"]
    x = np.asarray(inp["x"], np.float32)
    B, S, _ = x.shape
    nseg = S // T
    assert B * nseg == n_cores and nseg == 4
    ident, sel = host_consts()
    common = []
    for cidx in range(n_cores):
        b, s = divmod(cidx, nseg)
        xT = np.zeros((D, HALO + T), np.float32)
        lo = s * T - HALO
        if s == 0:
            xT[:, HALO:] = x[b, 0:T, :].T
        else:
            xT[:, :] = x[b, lo:lo + HALO + T, :].T
        common.append({"ident": ident, "sel": sel, "flags": core_flags(s), "xTin": xT})
    vecs = [pack_vecs(cfg, inp, l) for l in (0, 1)]
    f32 = lambda a: np.ascontiguousarray(np.asarray(a, np.float32))

    def wA(l):
        return {f"vecs{l}": vecs[l], f"w_in{l}": f32(inp["w_in"][l]), f"wa{l}": f32(inp["lru_wa"][l]),
                f"wx{l}": f32(inp["lru_wx"][l])}

    def wB(l):
        d = {f"vecsB{l}": vecs[l], f"w_out{l}": f32(inp["w_out"][l])}
        if l == 0:
            d.update({"wg0": f32(inp["dense_wg"][0]), "wu0": f32(inp["dense_wu"][0]), "wd0": f32(inp["dense_wd"][0])})
        else:
            d.update({"w_router1": f32(inp["w_router"][0]), "wg1": f32(inp["moe_wg"][0]),
                      "wu1": f32(inp["moe_wu"][0]), "wd1": f32(inp["moe_wd"][0])})
        return d

    def launch(stage, maps):
        nc = get_prog(cfg, [stage])
        res = run_bass_kernel_spmd(nc, maps, core_ids=list(range(n_cores)))
        return res.results

    def gather_carry(res, l):
        out = []
        for cidx in range(n_cores):
            b = cidx // nseg
            out.append(np.ascontiguousarray(np.concatenate([res[b * nseg + j][f"cst{l}"] for j in range(nseg)], 0)))
        return out

    keepA = ("ident", "sel", "flags")
    rA0 = launch("A0", [dict(common[i], **wA(0)) for i in range(n_cores)])
    carr0 = gather_carry(rA0, 0)
    mB0 = [dict(common[i], **wB(0), yT0=rA0[i]["yT0"], YL0=rA0[i]["YL0"], PG0=rA0[i]["PG0"], carr0=carr0[i])
           for i in range(n_cores)]
    rB0 = launch("B0", mB0)
    mA1 = []
    for i in range(n_cores):
        s = i % nseg
        xh = rB0[i - 1]["xlast"] if s > 0 else np.zeros((D, HALO), np.float32)
        m = {kk: common[i][kk] for kk in keepA}
        m.update(wA(1))
        m.update(xT1=rB0[i]["xT1"], xh1=np.ascontiguousarray(xh))
        mA1.append(m)
    rA1 = launch("A1", mA1)
    carr1 = gather_carry(rA1, 1)
    mB1 = []
    for i in range(n_cores):
        m = {kk: common[i][kk] for kk in keepA}
        m.update(wB(1))
        m.update(xT1=rB0[i]["xT1"], yT1=rA1[i]["yT1"], YL1=rA1[i]["YL1"], PG1=rA1[i]["PG1"], carr1=carr1[i])
        mB1.append(m)
    rB1 = launch("B1", mB1)
    out = np.empty((B, S, D), np.float32)
    for i in range(n_cores):
        b, s = divmod(i, nseg)
        out[b, s * T:(s + 1) * T, :] = rB1[i]["outT"].T
    return out


def run_fused(cfg, inp, n_cores=8):
    D, T = cfg["D"], cfg["T"]
    x = np.asarray(inp["x"], np.float32)
    B, S, _ = x.shape
    nseg = S // T
    assert B * nseg == n_cores and nseg == 4
    ident, sel = host_consts()
    f32 = lambda a: np.ascontiguousarray(np.asarray(a, np.float32))
    vecs = [pack_vecs(cfg, inp, l) for l in (0, 1)]
    shared = {"ident": ident, "sel": sel}
    for l in (0, 1):
        shared.update({f"vecs{l}": vecs[l], f"vecsB{l}": vecs[l], f"w_in{l}": f32(inp["w_in"][l]),
                       f"wa{l}": f32(inp["lru_wa"][l]), f"wx{l}": f32(inp["lru_wx"][l]),
                       f"w_out{l}": f32(inp["w_out"][l])})
    shared.update({"wg0": f32(inp["dense_wg"][0]), "wu0": f32(inp["dense_wu"][0]), "wd0": f32(inp["dense_wd"][0]),
                   "w_router1": f32(inp["w_router"][0]), "wg1": f32(inp["moe_wg"][0]),
                   "wu1": f32(inp["moe_wu"][0]), "wd1": f32(inp["moe_wd"][0])})
    maps = []
    for cidx in range(n_cores):
        b, s = divmod(cidx, nseg)
        xT = np.zeros((D, HALO + T), np.float32)
        if s == 0:
            xT[:, HALO:] = x[b, 0:T, :].T
        else:
            xT[:, :] = x[b, s * T - HALO:(s + 1) * T, :].T
        maps.append(dict(shared, flags=core_flags(s), xTin=xT))
    nc = get_prog(cfg, ["A0", "B0", "A1", "B1"])
    res = run_bass_kernel_spmd(nc, maps, core_ids=list(range(n_cores))).results
    out = np.empty((B, S, D), np.float32)
    for i in range(n_cores):
        b, s = divmod(i, nseg)
        out[b, s * T:(s + 1) * T, :] = res[i]["outT"].T
    return out


FULL_CFG = make_cfg()


def kernel(**inputs):
    return run_fused(FULL_CFG, inputs)
```

```python
import numpy as np
from contextlib import ExitStack
import ml_dtypes
import concourse.bass as bass
import concourse.mybir as mybir
from concourse.bass_utils import run_bass_kernel_spmd

F32 = mybir.dt.float32
BF16 = mybir.dt.bfloat16
AF = mybir.ActivationFunctionType
ALU = mybir.AluOpType
EPS = 1e-6
HALO = 32
CK = 31
LK = 4


def make_cfg(D=2048, T=2048, F=6144, FE=6144, NE=8, W=512):
    c = dict(D=D, KC=D // 128, CW=D // 2, NJ=D // 256, EIN=2 * D, T=T, W=W, F=F, FE=FE, NE=NE,
             TH=T // 2)
    c["PP"] = min(2, c["NJ"])
    c["XG"] = min(4, c["KC"])
    c["DGW"] = min(512, D)
    KC, NJ = c["KC"], c["NJ"]
    off = {}
    p = 0
    for name, n in (("mixn", KC), ("ffnn", KC), ("finn", KC), ("cb", NJ), ("lng", NJ), ("lnb", NJ),
                    ("lcb", NJ), ("ba", NJ), ("bx", NJ), ("lam", NJ), ("cw", NJ * CK), ("lw", NJ * LK)):
        off[name] = p
        p += n
    c["off"] = off
    c["NV"] = p
    return c


class Buf:
    _n = 0

    def __init__(self, ap, kind, name):
        self.ap, self.kind, self.name = ap, kind, name
        self.last_w = None
        self.reads = {}
        self.dcnt = 0
        Buf._n += 1
        self.id = Buf._n

    def __getitem__(self, idx):
        return V(self, self.ap[idx])

    def v(self):
        return V(self, self.ap)


class V:
    def __init__(self, buf, ap):
        self.buf, self.ap = buf, ap

    def __getitem__(self, idx):
        return V(self.buf, self.ap[idx])

    def re(self, s, **kw):
        return V(self.buf, self.ap.rearrange(s, **kw))


class Rot:
    def __init__(self, bufs):
        self.bufs, self.i = bufs, 0

    def next(self):
        b = self.bufs[self.i % len(self.bufs)]
        self.i += 1
        return b


COMPUTE = ("pe", "act", "dve", "pool")
STREAMS = ("pe", "act", "dve", "pool", "sp")


class K:
    def __init__(self, nc, st, arena_words):
        self.nc = nc
        self.arena = st.enter_context(nc.sbuf_tensor("arena", [128, arena_words], F32))[:]
        self.arena_words = arena_words
        self.ptr = 0
        self.ops = []
        self.cnt = {e: 0 for e in COMPUTE}
        self.waited = {e: {} for e in STREAMS}
        self.slot_cnt = []
        self.free_slots = []
        self.live_dbufs = []
        self.cbufs = {}
        self.banks = [Buf(st.enter_context(nc.psum_tensor(f"ps{i}", [128, 512], F32))[:], "psum", f"ps{i}")
                      for i in range(8)]
        self.rot = 0
        self.peak = 0

    def alloc(self, name, shape, dtype):
        n = 1
        for s in shape[1:]:
            n *= s
        words = (n + 1) // 2 if dtype == BF16 else n
        words = (words + 7) // 8 * 8
        off = self.ptr
        self.ptr += words
        self.peak = max(self.peak, self.ptr)
        assert self.ptr <= self.arena_words, f"SBUF arena overflow at {name}: {self.ptr} > {self.arena_words}"
        ap = self.arena[:, off:off + words]
        if dtype == BF16:
            ap = ap.bitcast(BF16)
        ap = ap[:, :n]
        if shape[0] < 128:
            ap = ap[0:shape[0]]
        if len(shape) == 3:
            ap = ap.rearrange("p (a b) -> p a b", a=shape[1])
        elif len(shape) == 4:
            ap = ap.rearrange("p (a b c) -> p a b c", a=shape[1], b=shape[2])
        return Buf(ap, "sbuf", name)

    def bank(self):
        b = self.banks[self.rot % 6]
        self.rot += 1
        return b

    def dram(self, name, shape, dtype, kind):
        return Buf(self.nc.dram_tensor(name, list(shape), dtype, kind=kind).ap(), "dram", name)

    def _collect(self, eng, reads, writes):
        need = {}

        def add(tok):
            if tok is not None:
                need[tok[0]] = max(need.get(tok[0], 0), tok[1])

        for b in reads:
            add(b.last_w)
        for b in writes:
            if b.kind != "dram":
                add(b.last_w)
            for kk, v in b.reads.items():
                add((kk, v))
        out = []
        for kk, v in need.items():
            if kk == eng and eng == "pe":
                continue
            if self.waited[eng].get(kk, 0) >= v:
                continue
            self.waited[eng][kk] = v
            out.append((kk, v))
        return out

    def op(self, eng, fn, reads, writes):
        reads = [r.buf for r in reads if isinstance(r, V)]
        writes = [w.buf for w in writes]
        waits = self._collect(eng, reads, writes)
        n = self.cnt[eng] + 1
        self.cnt[eng] = n
        self.ops.append((eng, fn, waits, (eng, 1)))
        for b in reads:
            b.reads[eng] = max(b.reads.get(eng, 0), n)
        for b in writes:
            b.last_w = (eng, n)
            b.reads = {}

    def dma(self, q, out, in_):
        waits = self._collect(q, [in_.buf], [out.buf])
        dst = out.buf
        if getattr(dst, "dslot", None) is None:
            if self.free_slots:
                dst.dslot = self.free_slots.pop()
            else:
                dst.dslot = len(self.slot_cnt)
                self.slot_cnt.append(0)
            self.live_dbufs.append(dst)
        slot = dst.dslot
        key = ("d", slot)
        self.slot_cnt[slot] += 16
        cntv = self.slot_cnt[slot]
        o_ap, i_ap = out.ap, in_.ap
        self.ops.append((q, lambda e: e.dma_start(out=o_ap, in_=i_ap), waits, (key, 16)))
        in_.buf.reads[key] = max(in_.buf.reads.get(key, 0), cntv)
        dst.last_w = (key, cntv)
        dst.reads = {}

    def collective(self, out_buf, in_buf, groups):
        waits = self._collect("pool", [in_buf], [out_buf])
        key = ("c", out_buf.id)
        self.cbufs[out_buf.id] = out_buf
        o_ap, i_ap = out_buf.ap, in_buf.ap
        self.ops.append(("pool", lambda e: e.collective_compute("AllGather", ALU.bypass, replica_groups=groups,
                                                                  ins=[i_ap], outs=[o_ap]), waits, (key, None)))
        in_buf.reads[key] = 1
        out_buf.last_w = (key, 1)
        out_buf.reads = {}

    def barrier(self):
        allw = [(e, self.cnt[e]) for e in COMPUTE if self.cnt[e] > 0]
        allw += [(("d", i), v) for i, v in enumerate(self.slot_cnt) if v > 0]
        allw += [(("c", b.id), 1) for b in self.cbufs.values()]
        for b in self.live_dbufs:
            self.free_slots.append(b.dslot)
            b.dslot = None
        self.live_dbufs = []
        for e in STREAMS:
            waits = []
            for kk, v in allw:
                if self.waited[e].get(kk, 0) < v:
                    self.waited[e][kk] = v
                    waits.append((kk, v))
            self.ops.append((e, None, waits, None))

    @staticmethod
    def _a(x):
        return x.ap if isinstance(x, V) else x

    def mm(self, out, lhsT, rhs, start, stop):
        o, l, r = out.ap, lhsT.ap, rhs.ap
        self.op("pe", lambda e: e.matmul(o, l, r, start=start, stop=stop), [lhsT, rhs], [out])

    def act(self, out, in_, func, bias=None, scale=None):
        o, i = out.ap, in_.ap
        kw = {}
        if bias is not None:
            kw["bias"] = self._a(bias)
        if scale is not None:
            kw["scale"] = self._a(scale)
        self.op("act", lambda e: e.activation(out=o, in_=i, func=func, **kw), [in_, bias, scale], [out])

    def tt(self, out, in0, in1, op, eng="dve"):
        o, a, b = out.ap, in0.ap, in1.ap
        self.op(eng, lambda e: e.tensor_tensor(out=o, in0=a, in1=b, op=op), [in0, in1], [out])

    def stt(self, out, in0, scalar, in1, op0, op1):
        o, a, s, b = out.ap, in0.ap, self._a(scalar), in1.ap
        self.op("dve", lambda e: e.scalar_tensor_tensor(out=o, in0=a, scalar=s, in1=b, op0=op0, op1=op1),
                [in0, scalar, in1], [out])

    def ts(self, out, in0, s1, s2, op0, op1=None, eng="dve"):
        o, a, x1, x2 = out.ap, in0.ap, self._a(s1), self._a(s2)
        if op1 is None:
            fn = lambda e: e.tensor_scalar(out=o, in0=a, scalar1=x1, scalar2=None, op0=op0)
        else:
            fn = lambda e: e.tensor_scalar(out=o, in0=a, scalar1=x1, scalar2=x2, op0=op0, op1=op1)
        self.op(eng, fn, [in0, s1, s2], [out])

    def scan(self, out, d0, d1, initial, op0, op1):
        o, a, b, i = out.ap, d0.ap, d1.ap, self._a(initial)
        self.op("dve", lambda e: e.tensor_tensor_scan(out=o, data0=a, data1=b, initial=i, op0=op0, op1=op1),
                [d0, d1, initial], [out])

    def copy(self, out, in_, eng="dve"):
        o, i = out.ap, in_.ap
        self.op(eng, lambda e: e.tensor_copy(out=o, in_=i), [in_], [out])

    def recip(self, out, in_):
        o, i = out.ap, in_.ap
        self.op("dve", lambda e: e.reciprocal(out=o, in_=i), [in_], [out])

    def memset(self, v, val, eng="dve"):
        a = v.ap
        self.op(eng, lambda e: e.memset(a, val), [], [v])

    def vmax(self, out, in_):
        o, i = out.ap, in_.ap
        self.op("dve", lambda e: e.max(out=o, in_=i), [in_], [out])

    def emit(self, final_bufs):
        nc = self.nc
        waits = [b.last_w for b in final_bufs if b.last_w is not None]
        self.ops.append(("sp", None, waits, None))
        with ExitStack() as st:
            sems = {}
            for e in COMPUTE:
                sems[e] = st.enter_context(nc.semaphore("s_" + e))
            print("free sems", nc.free_len(), "dma slots", len(self.slot_cnt))
            for i in range(len(self.slot_cnt)):
                sems[("d", i)] = st.enter_context(nc.semaphore(f"d{i}"))
            for b in self.cbufs.values():
                sems[("c", b.id)] = st.enter_context(nc.semaphore(f"c{b.id}"))
            block = st.enter_context(nc.Block())
            ops = self.ops

            def mk(name):
                def body(eng):
                    for (e, fn, waits, inc) in ops:
                        if e != name:
                            continue
                        for (kk, v) in waits:
                            eng.wait_ge(sems[kk], v)
                        if fn is None:
                            continue
                        ins = fn(eng)
                        if inc[1] is None:
                            ins.then_inc(sems[inc[0]])
                        else:
                            ins.then_inc(sems[inc[0]], inc[1])
                return body

            block.tensor(mk("pe"))
            block.scalar(mk("act"))
            block.vector(mk("dve"))
            block.gpsimd(mk("pool"))
            block.sync(mk("sp"))


def setup_consts(k, cfg, din):
    c = {}
    c["ones"] = k.alloc("ones", [128, 128], BF16)
    k.memset(c["ones"].v(), 1.0)
    c["zeros"] = k.alloc("zeros", [128, 512], F32)
    k.memset(c["zeros"].v(), 0.0)
    c["ident"] = k.alloc("ident", [128, 128], F32)
    k.dma("sp", c["ident"].v(), din["ident"].v())
    c["sel"] = k.alloc("sel", [8, 8 * 128], F32)
    k.dma("sp", c["sel"].v(), din["sel"].v())
    c["flags"] = k.alloc("flags", [128, 8], F32)
    k.dma("sp", c["flags"].v(), din["flags"].v())
    k.persist = k.ptr
    return c


def rows(buf, r0, nr, c0, nc_):
    return V(buf, buf.ap[r0:r0 + nr, c0:c0 + nc_].rearrange("(k p) t -> p k t", p=128))


def stage_A(k, cfg, c, dw, xmain, xoff, xhalo, xhoff, yT, YL, PG, cst, halo_gathered=False):
    D, KC, NJ, T, W, CW = cfg["D"], cfg["KC"], cfg["NJ"], cfg["T"], cfg["W"], cfg["CW"]
    PP, XG, off = cfg["PP"], cfg["XG"], cfg["off"]
    k.ptr = k.persist
    vec = k.alloc("vec", [128, cfg["NV"]], F32)
    k.dma("sp", vec.v(), dw["vecs"].v())
    wa = k.alloc("wa", [128, NJ, 128], BF16)
    wx = k.alloc("wx", [128, NJ, 128], BF16)
    k.dma("pool", wa.v(), dw["wa"].v().re("h i j -> i h j"))
    k.dma("pool", wx.v(), dw["wx"].v().re("h i j -> i h j"))

    def vcol(name, i, n=1):
        return vec[:, off[name] + i: off[name] + i + n]

    sm = k.alloc("sm", [128, 10 * NJ], F32)
    s_ = [sm[:, i * NJ:(i + 1) * NJ] for i in range(10)]
    e_, ln_, t1, msk, t2, spv, nsp, nsp2 = s_[0], s_[1], s_[2], s_[3], s_[4], s_[5], s_[6], s_[7]
    lam = vcol("lam", 0, NJ)
    k.act(e_, lam, AF.Exp, scale=-1.0)
    k.act(ln_, e_, AF.Ln, bias=1.0)
    k.ts(t1, e_, -1.0 / 3.0, 0.5, ALU.mult, ALU.add)
    k.tt(t1, t1, e_, ALU.mult)
    k.ts(t1, t1, -1.0, 1.0, ALU.mult, ALU.add)
    k.tt(t1, t1, e_, ALU.mult)
    k.ts(msk, e_, 0.05, None, ALU.is_lt)
    k.tt(t2, t1, ln_, ALU.subtract)
    k.tt(t2, t2, msk, ALU.mult)
    k.tt(spv, t2, ln_, ALU.add)
    k.ts(nsp, spv, -8.0, None, ALU.mult)
    k.ts(nsp2, spv, -16.0, None, ALU.mult)

    hst = k.alloc("hst", [128, NJ], F32)
    pst = k.alloc("pst", [128, NJ], F32)
    k.memset(hst.v(), 0.0)
    k.memset(pst.v(), 1.0)
    chalo = [k.alloc(f"chalo{j}", [128, HALO], BF16) for j in range(NJ)]
    rhalo = [k.alloc(f"rhalo{j}", [128, HALO], F32) for j in range(NJ)]

    xt = [k.alloc(f"xt{g}", [128, XG, W], F32) for g in range(KC // XG)]
    if halo_gathered:
        xhp = [k.alloc(f"xhp{i}", [128, XG, HALO], F32) for i in range(3)]
    hT = [k.alloc(f"hT{i}", [128, W], BF16) for i in range(KC)]
    xth = [k.alloc(f"xth{g}", [128, XG, HALO], F32) for g in range(KC // XG)]
    hTh = [k.alloc(f"hTh{i}", [128, HALO], BF16) for i in range(KC)]
    sqp = Rot([k.alloc(f"sq{i}", [128, W], BF16) for i in range(2)])
    bfp = Rot([k.alloc(f"bfp{i}", [128, W], BF16) for i in range(4)])
    wtA = Rot([k.alloc(f"wtA{i}", [128, KC, PP * 128], BF16) for i in range(2)])
    wtB = Rot([k.alloc(f"wtB{i}", [128, KC, PP * 128], BF16) for i in range(2)])
    cwp = Rot([k.alloc(f"cw{i}", [128, HALO + W], BF16) for i in range(2)])
    dgp = Rot([k.alloc(f"dg{i}", [128, CK, 128], BF16) for i in range(2)])
    identb = k.alloc("identb", [128, 128], BF16)
    k.copy(identb.v(), c["ident"].v())
    rwp = Rot([k.alloc(f"rw{i}", [128, HALO + W], F32) for i in range(2)])
    ccb = [k.alloc(f"cc{j}", [128, W], F32) for j in range(NJ)]
    fp = Rot([k.alloc(f"fp{i}", [128, W], F32) for i in range(20)])
    rstd = k.alloc("rstd", [128, W], F32)
    mu = k.alloc("mu", [128, W], F32)
    rs2 = k.alloc("rs2", [128, W], F32)
    ycv = Rot([k.alloc(f"ycv{i}", [128, NJ, W], BF16) for i in range(2)])
    ps_ss, ps_s1, ps_s2 = k.banks[6], k.banks[6], k.banks[7]
    ones = c["ones"].v()
    flags = c["flags"]
    w_in = dw["w_in"]

    def lru_part2(j, ti, gx, rc, ml, a_, G):
        bb = fp.next()
        k.tt(bb[:, :], gx[:, :], rc[:, :], ALU.mult)
        k.tt(bb[:, :], bb[:, :], ml[:, :], ALU.mult)
        hl, Pc = fp.next(), fp.next()
        k.scan(hl[:, :], a_[:, :], bb[:, :], hst[:, j:j + 1], ALU.mult, ALU.add)
        k.copy(hst[:, j:j + 1], hl[:, W - 1:W])
        k.scan(Pc[:, :], a_[:, :], c["zeros"][:, :W], pst[:, j:j + 1], ALU.mult, ALU.add)
        k.copy(pst[:, j:j + 1], Pc[:, W - 1:W])
        k.tt(hl[:, :], hl[:, :], G[:, :], ALU.mult)
        k.tt(Pc[:, :], Pc[:, :], G[:, :], ALU.mult)
        k.dma("sp", YL[j * 128:(j + 1) * 128, ti * W:(ti + 1) * W], hl[:, :])
        k.dma("sp", PG[j * 128:(j + 1) * 128, ti * W:(ti + 1) * W], Pc[:, :])

    def conv_part2(j, dg, cw):
        pcv = k.bank()
        for kk in range(CK):
            k.mm(pcv[:, :W], dg[:, kk, :], cw[:, 2 + kk:2 + kk + W], kk == 0, kk == CK - 1)
        k.copy(chalo[j][:, :], cw[:, W:W + HALO])
        cc = ccb[j]
        k.act(cc[:, :], pcv[:, :W], AF.Identity, bias=vcol("cb", j))
        b1, b2 = bfp.next(), bfp.next()
        k.act(b1[:, :], cc[:, :], AF.Identity)
        k.act(b2[:, :], cc[:, :], AF.Square)
        k.mm(ps_s1[:, :W], ones, b1[:, :], j == 0, j == NJ - 1)
        k.mm(ps_s2[:, :W], ones, b2[:, :], j == 0, j == NJ - 1)

    pend_c = None
    pend = None
    grps = [[("halo", 0, HALO), ("main", 0, W)]] + [[("main", i, W)] for i in range(1, T // W)]
    for grp in grps:
      for kind, ti, w in grp:
        xt_, hT_ = (xth, hTh) if kind == "halo" else (xt, hT)
        for g in range(KC // XG):
            if kind == "halo" and halo_gathered:
                for jj in range(3):
                    k.dma("sp", xhp[jj].v(), rows(xhalo, jj * D + g * XG * 128, XG * 128, 0, w))
                k.ts(xt_[g][:, :, 0:w], xhp[0].v(), flags[:, 5:6], None, ALU.mult)
                k.stt(xt_[g][:, :, 0:w], xhp[1].v(), flags[:, 6:7], xt_[g][:, :, 0:w], ALU.mult, ALU.add)
                k.stt(xt_[g][:, :, 0:w], xhp[2].v(), flags[:, 7:8], xt_[g][:, :, 0:w], ALU.mult, ALU.add)
                continue
            if kind == "halo":
                src = rows(xhalo, g * XG * 128, XG * 128, xhoff, w)
            else:
                src = rows(xmain, g * XG * 128, XG * 128, xoff + ti * W, w)
            k.dma("sp", xt_[g][:, :, 0:w], src)
        for kc in range(KC):
            sq = sqp.next()
            k.act(sq[:, :w], xt_[kc // XG][:, kc % XG, 0:w], AF.Square)
            k.mm(ps_ss[:, :w], ones, sq[:, :w], kc == 0, kc == KC - 1)
        rt = fp.next()
        k.act(rt[:, :w], ps_ss[:, :w], AF.Sqrt, bias=EPS, scale=1.0 / D)
        k.recip(rstd[:, :w], rt[:, :w])
        for kc in range(KC):
            k.stt(hT_[kc][:, :w], xt_[kc // XG][:, kc % XG, 0:w], vcol("mixn", kc), rstd[:, :w],
                  ALU.mult, ALU.mult)

      for branch in (0, 1):
            for q in range(NJ // PP):
                A, B = wtA.next(), wtB.next()
                ca0 = branch * 2 * CW + q * PP * 128
                cb0 = ca0 + CW
                k.dma("pool", A.v(), rows(w_in, 0, D, ca0, PP * 128))
                k.dma("pool", B.v(), rows(w_in, 0, D, cb0, PP * 128))
                for pr in range(PP):
                    j = q * PP + pr
                    for kind, ti, w in grp:
                        hT_ = hTh if kind == "halo" else hT
                        psa, psb = k.bank(), k.bank()
                        for kc in range(KC):
                            k.mm(psa[:, :w], A[:, kc, pr * 128:(pr + 1) * 128], hT_[kc][:, :w], kc == 0, kc == KC - 1)
                        need_b = not (branch == 1 and kind == "halo")
                        if need_b:
                            for kc in range(KC):
                                k.mm(psb[:, :w], B[:, kc, pr * 128:(pr + 1) * 128], hT_[kc][:, :w], kc == 0,
                                     kc == KC - 1)
                        if branch == 0:
                            sgt = fp.next()
                            k.act(sgt[:, :w], psb[:, :w], AF.Sigmoid)
                            if kind == "halo":
                                k.tt(chalo[j][:, :], psa[:, :w], sgt[:, :w], ALU.mult)
                                continue
                            cw = cwp.next()
                            k.copy(cw[:, 0:HALO], chalo[j][:, :])
                            k.tt(cw[:, HALO:HALO + W], psa[:, :W], sgt[:, :W], ALU.mult)
                            dg = dgp.next()
                            for kk in range(CK):
                                k.ts(dg[:, kk, :], identb[:, :], vcol("cw", j * CK + kk), None, ALU.mult)
                            if pend_c is not None:
                                conv_part2(*pend_c)
                            pend_c = (j, dg, cw)
                        else:
                            if kind == "halo":
                                k.act(rhalo[j][:, :], psa[:, :w], AF.Identity)
                                continue
                            rw = rwp.next()
                            k.copy(rw[:, 0:HALO], rhalo[j][:, :])
                            k.act(rw[:, HALO:HALO + W], psa[:, :W], AF.Identity)
                            G = fp.next()
                            k.act(G[:, :], psb[:, :W], AF.Gelu_apprx_tanh)
                            rc = fp.next()
                            k.ts(rc[:, :], rw[:, HALO - 3:HALO - 3 + W], vcol("lw", j * LK), vcol("lcb", j),
                                 ALU.mult, ALU.add)
                            for kk in range(1, LK):
                                k.stt(rc[:, :], rw[:, HALO - 3 + kk:HALO - 3 + kk + W], vcol("lw", j * LK + kk),
                                      rc[:, :], ALU.mult, ALU.add)
                            k.copy(rhalo[j][:, :], rw[:, W:W + HALO])
                            rcb = bfp.next()
                            k.act(rcb[:, :], rc[:, :], AF.Identity)
                            pga, pgx = k.bank(), k.bank()
                            k.mm(pga[:, :W], wa[:, j, :], rcb[:, :], True, True)
                            k.mm(pgx[:, :W], wx[:, j, :], rcb[:, :], True, True)
                            ga, gx, a_, a2, ml = fp.next(), fp.next(), fp.next(), fp.next(), fp.next()
                            k.act(ga[:, :], pga[:, :W], AF.Sigmoid, bias=vcol("ba", j))
                            k.act(gx[:, :], pgx[:, :W], AF.Sigmoid, bias=vcol("bx", j))
                            k.act(a_[:, :], ga[:, :], AF.Exp, scale=nsp[:, j:j + 1])
                            k.act(a2[:, :], ga[:, :], AF.Exp, scale=nsp2[:, j:j + 1])
                            k.act(ml[:, :], a2[:, :], AF.Sqrt, bias=1.0, scale=-1.0)
                            if ti == 0:
                                k.ts(ml[:, 0:1], ml[:, 0:1], flags[:, 4:5], flags[:, 3:4], ALU.mult, ALU.add)
                            if pend is not None:
                                lru_part2(*pend)
                            pend = (j, ti, gx, rc, ml, a_, G)
            if branch == 1 and pend is not None:
                lru_part2(*pend)
                pend = None
            if branch == 0:
                if pend_c is not None:
                    conv_part2(*pend_c)
                    pend_c = None
                ti = grp[-1][1]
                mu2, var, sd = fp.next(), fp.next(), fp.next()
                k.act(mu[:, :], ps_s1[:, :W], AF.Identity, scale=1.0 / CW)
                k.tt(mu2[:, :], mu[:, :], mu[:, :], ALU.mult)
                k.stt(var[:, :], ps_s2[:, :W], 1.0 / CW, mu2[:, :], ALU.mult, ALU.subtract)
                k.act(sd[:, :], var[:, :], AF.Sqrt, bias=EPS)
                k.recip(rs2[:, :], sd[:, :])
                yc = ycv.next()
                for j in range(NJ):
                    t_ = fp.next()
                    k.tt(t_[:, :], ccb[j][:, :], mu[:, :], ALU.subtract)
                    k.tt(t_[:, :], t_[:, :], rs2[:, :], ALU.mult)
                    k.act(yc[:, j, :], t_[:, :], AF.Silu, bias=vcol("lnb", j), scale=vcol("lng", j))
                k.dma("sp", rows(yT, 0, CW, ti * W, W), yc.v())
    k.dma("sp", cst[0:128, :], pst.v())
    k.dma("sp", cst[128:256, :], hst.v())


def stage_B(k, cfg, c, dw, xres, xroff, yT, YL, PG, carr, xout, xlast, outT, is_moe, is_last):
    D, KC, NJ, T, W, CW, TH = cfg["D"], cfg["KC"], cfg["NJ"], cfg["T"], cfg["W"], cfg["CW"], cfg["TH"]
    off, DGW = cfg["off"], cfg["DGW"]
    NE = cfg["NE"] if is_moe else 1
    F = cfg["FE"] if is_moe else cfg["F"]
    NTH = TH // W
    k.ptr = k.persist
    vec = k.alloc("vec", [128, cfg["NV"]], F32)
    k.dma("sp", vec.v(), dw["vecs"].v())

    def vcol(name, i, n=1):
        return vec[:, off[name] + i: off[name] + i + n]

    flags = c["flags"]
    ones = c["ones"].v()
    ca = k.alloc("ca", [128, 8, NJ], F32)
    k.dma("sp", ca.v(), carr.v().re("(sa p) j -> p sa j", p=128))
    carry = k.alloc("carry", [128, NJ], F32)
    ctmp = k.alloc("ctmp", [128, NJ], F32)
    k.memset(carry.v(), 0.0)
    for s in range(3):
        k.tt(ctmp[:, :], ca[:, 2 * s, :], carry[:, :], ALU.mult)
        k.tt(ctmp[:, :], ctmp[:, :], ca[:, 2 * s + 1, :], ALU.add)
        k.tt(ctmp[:, :], ctmp[:, :], carry[:, :], ALU.subtract)
        k.stt(carry[:, :], ctmp[:, :], flags[:, s:s + 1], carry[:, :], ALU.mult, ALU.add)

    if is_moe:
        wr = k.alloc("wr", [128, KC, 8], F32)
        k.dma("sp", wr.v(), dw["w_router"].v().re("(kc p) e -> p kc e", p=128))
        lgT = k.alloc("lgT", [8, TH], F32)
        gwT = k.alloc("gwT", [8, TH], F32)
        smalp = Rot([k.alloc(f"smal{i}", [128, 64], F32) for i in range(4)])
    h2T = [k.alloc(f"h2T{i}", [128, TH], BF16) for i in range(KC)]
    acc = [k.alloc(f"acc{i}", [128, TH], F32) for i in range(KC)]
    h2d = k.dram("h2d", [D, TH], BF16, "Internal") if is_moe else None
    base = k.ptr
    ps_ss = k.banks[6]

    for hh in range(2):
        k.barrier()
        k.ptr = base
        assert NTH <= 2
        yts = [k.alloc(f"yt{t}", [128, 2 * NJ, W], BF16) for t in range(NTH)]
        ylp = Rot([k.alloc(f"yl{i}", [128, W], F32) for i in range(2)])
        pgp = Rot([k.alloc(f"pg{i}", [128, W], F32) for i in range(2)])
        wop = Rot([k.alloc(f"wo{i}", [128, 2 * NJ, DGW], BF16) for i in range(2)])
        sqp = Rot([k.alloc(f"sqb{i}", [128, W], BF16) for i in range(2)])
        rt = k.alloc("rtb", [128, W], F32)
        rstd = k.alloc("rstdb", [128, W], F32)
        hfp = Rot([k.alloc(f"hf{i}", [128, W], F32) for i in range(2)])
        ps_st = [k.banks[6], k.banks[7]]
        for tt in range(NTH):
            col = hh * TH + tt * W
            lc = tt * W
            yt = yts[tt]
            for d in range(KC):
                k.dma("sp", acc[d][:, lc:lc + W], xres[d * 128:(d + 1) * 128, xroff + col:xroff + col + W])
            k.dma("sp", yt[:, 0:NJ, :], rows(yT, 0, CW, col, W))
            for j in range(NJ):
                yl, pg = ylp.next(), pgp.next()
                k.dma("sp", yl[:, :], YL[j * 128:(j + 1) * 128, col:col + W])
                k.dma("sp", pg[:, :], PG[j * 128:(j + 1) * 128, col:col + W])
                k.stt(yt[:, NJ + j, :], pg[:, :], carry[:, j:j + 1], yl[:, :], ALU.mult, ALU.add)
        for dg in range(D // DGW):
            wo = wop.next()
            k.dma("pool", wo.v(), rows(dw["w_out"], 0, D, dg * DGW, DGW))
            for dc in range(DGW // 128):
                d = dg * (DGW // 128) + dc
                for tt in range(NTH):
                    lc = tt * W
                    ps = k.bank()
                    for e in range(2 * NJ):
                        k.mm(ps[:, :W], wo[:, e, dc * 128:(dc + 1) * 128], yts[tt][:, e, :], e == 0, e == 2 * NJ - 1)
                    k.tt(acc[d][:, lc:lc + W], ps[:, :W], acc[d][:, lc:lc + W], ALU.add)
                    sq = sqp.next()
                    k.act(sq[:, :], acc[d][:, lc:lc + W], AF.Square)
                    k.mm(ps_st[tt][:, :W], ones, sq[:, :], d == 0, d == KC - 1)
        for tt in range(NTH):
            lc = tt * W
            k.act(rt[:, :], ps_st[tt][:, :W], AF.Sqrt, bias=EPS, scale=1.0 / D)
            k.recip(rstd[:, :], rt[:, :])
            for kc in range(KC):
                k.stt(h2T[kc][:, lc:lc + W], acc[kc][:, lc:lc + W], vcol("ffnn", kc), rstd[:, :],
                      ALU.mult, ALU.mult)
            if is_moe:
                pl = k.bank()
                for kc in range(KC):
                    hf = hfp.next()
                    k.stt(hf[:, :], acc[kc][:, lc:lc + W], vcol("ffnn", kc), rstd[:, :], ALU.mult, ALU.mult)
                    k.mm(pl[0:8, :W], wr[:, kc, :], hf[:, :], kc == 0, kc == KC - 1)
                k.act(lgT[0:8, lc:lc + W], pl[0:8, :W], AF.Identity)
        if is_moe:
            for kc in range(KC):
                k.dma("sp", h2d[kc * 128:(kc + 1) * 128, :], h2T[kc][:, :])
        if is_moe:
            for blk in range(TH // 128):
                sl = slice(blk * 128, (blk + 1) * 128)
                smal = smalp.next()
                lg, m8, msk, nl1 = smal[:, 0:8], smal[:, 8:16], smal[:, 16:24], smal[:, 24:25]
                ex, e2, den, rden, gw = smal[:, 32:40], smal[:, 25:26], smal[:, 26:27], smal[:, 27:28], smal[:, 40:48]
                pl = k.bank()
                k.mm(pl[:, 0:8], lgT[0:8, sl], c["ident"][0:8, 0:8], True, True)
                k.act(lg, pl[:, 0:8], AF.Identity)
                k.vmax(m8, lg)
                k.ts(msk, lg, m8[:, 1:2], None, ALU.is_ge)
                k.ts(nl1, m8[:, 0:1], -1.0, None, ALU.mult)
                k.act(ex, lg, AF.Exp, bias=nl1)
                k.act(e2, m8[:, 1:2], AF.Exp, bias=nl1)
                k.ts(den, e2, 1.0, None, ALU.add)
                k.recip(rden, den)
                k.stt(gw, ex, rden, msk, ALU.mult, ALU.mult)
                pt = k.bank()
                k.mm(pt[0:8, 0:128], gw, c["ident"][:, :], True, True)
                k.act(gwT[0:8, sl], pt[0:8, 0:128], AF.Identity)

        k.barrier()
        k.ptr = base
        wgp = Rot([k.alloc(f"wg{i}", [128, KC, 256], BF16) for i in range(2)])
        wup = Rot([k.alloc(f"wu{i}", [128, KC, 256], BF16) for i in range(2)])
        wdp = Rot([k.alloc(f"wd{i}", [128, 4, D], BF16) for i in range(2)])
        actT = [k.alloc(f"actT{i}", [128, TH], BF16) for i in range(4)]
        sgp = Rot([k.alloc(f"sg{i}", [128, W], F32) for i in range(2)])
        tmpp = Rot([k.alloc(f"tm{i}", [128, W], F32) for i in range(2)])
        if is_moe:
            gwBp = Rot([k.alloc(f"gwB{i}", [128, TH], F32) for i in range(2)])
        for ex_i in range(NE):
            if is_moe:
                wg_d, wu_d, wd_d = (V(dw[n], dw[n].ap[ex_i]) for n in ("wg", "wu", "wd"))
                gwB = gwBp.next()
                for tt in range(NTH):
                    pb = k.bank()
                    k.mm(pb[:, :W], c["sel"][0:8, ex_i * 128:(ex_i + 1) * 128], gwT[0:8, tt * W:(tt + 1) * W],
                         True, True)
                    k.act(gwB[:, tt * W:(tt + 1) * W], pb[:, :W], AF.Identity)
                for kc in range(KC):
                    if ex_i > 0:
                        k.dma("sp", h2T[kc][:, :], h2d[kc * 128:(kc + 1) * 128, :])
                    k.stt(h2T[kc][:, :], gwB[:, :], 0.0, h2T[kc][:, :], ALU.is_gt, ALU.mult)
            else:
                wg_d, wu_d, wd_d = (dw[n].v() for n in ("wg", "wu", "wd"))
            for g in range(F // 512):
                wd_t = wdp.next()
                k.dma("pool", wd_t.v(), wd_d[g * 512:(g + 1) * 512, :].re("(f p) d -> p f d", p=128))
                for pr in range(2):
                    wg_t, wu_t = wgp.next(), wup.next()
                    f0 = g * 512 + pr * 256
                    k.dma("pool", wg_t.v(), wg_d[:, f0:f0 + 256].re("(kc p) f -> p kc f", p=128))
                    k.dma("pool", wu_t.v(), wu_d[:, f0:f0 + 256].re("(kc p) f -> p kc f", p=128))
                    for fc2 in range(2):
                        fc = pr * 2 + fc2
                        for tt in range(NTH):
                            cs = slice(tt * W, (tt + 1) * W)
                            pg_, pu_ = k.bank(), k.bank()
                            for kc in range(KC):
                                k.mm(pg_[:, :W], wg_t[:, kc, fc2 * 128:(fc2 + 1) * 128], h2T[kc][:, cs], kc == 0,
                                     kc == KC - 1)
                            for kc in range(KC):
                                k.mm(pu_[:, :W], wu_t[:, kc, fc2 * 128:(fc2 + 1) * 128], h2T[kc][:, cs], kc == 0,
                                     kc == KC - 1)
                            sg = sgp.next()
                            k.act(sg[:, :], pg_[:, :W], AF.Silu)
                            if is_moe:
                                tm = tmpp.next()
                                k.tt(tm[:, :], pu_[:, :W], sg[:, :], ALU.mult)
                                k.tt(actT[fc][:, cs], tm[:, :], gwB[:, cs], ALU.mult)
                            else:
                                k.tt(actT[fc][:, cs], pu_[:, :W], sg[:, :], ALU.mult)
                for d in range(KC):
                    for tt in range(NTH):
                        cs = slice(tt * W, (tt + 1) * W)
                        pd = k.bank()
                        for fc in range(4):
                            k.mm(pd[:, :W], wd_t[:, fc, d * 128:(d + 1) * 128], actT[fc][:, cs], fc == 0, fc == 3)
                        k.tt(acc[d][:, cs], pd[:, :W], acc[d][:, cs], ALU.add)
        if not is_last:
            for d in range(KC):
                k.dma("sp", xout[d * 128:(d + 1) * 128, hh * TH:(hh + 1) * TH], acc[d][:, :])
                if hh == 1:
                    k.dma("sp", xlast[d * 128:(d + 1) * 128, :], acc[d][:, TH - HALO:TH])
        else:
            k.barrier()
            k.ptr = base
            sqf = Rot([k.alloc(f"sqf{i}", [128, W], BF16) for i in range(2)])
            otp = Rot([k.alloc(f"ot{i}", [128, W], F32) for i in range(2)])
            rtf = k.alloc("rtf", [128, W], F32)
            rsf = k.alloc("rsf", [128, W], F32)
            for tt in range(NTH):
                cs = slice(tt * W, (tt + 1) * W)
                for d in range(KC):
                    sq = sqf.next()
                    k.act(sq[:, :], acc[d][:, cs], AF.Square)
                    k.mm(ps_ss[:, :W], ones, sq[:, :], d == 0, d == KC - 1)
                k.act(rtf[:, :], ps_ss[:, :W], AF.Sqrt, bias=EPS, scale=1.0 / D)
                k.recip(rsf[:, :], rtf[:, :])
                for d in range(KC):
                    ot = otp.next()
                    k.stt(ot[:, :], acc[d][:, cs], vcol("finn", d), rsf[:, :], ALU.mult, ALU.mult)
                    k.dma("sp", outT[d * 128:(d + 1) * 128, hh * TH + tt * W: hh * TH + (tt + 1) * W], ot[:, :])


ARENA_WORDS = 52800


def build_program(cfg, stages):
    nc = bass.Bass("TRN2", target_bir_lowering=False)
    D, KC, NJ, T, CW = cfg["D"], cfg["KC"], cfg["NJ"], cfg["T"], cfg["CW"]
    fused = len(stages) == 4
    groups = [[0, 1, 2, 3], [4, 5, 6, 7]]
    final_bufs = []
    with ExitStack() as st:
        k = K(nc, st, ARENA_WORDS)

        def ext_in(name, shape, dtype=F32):
            return k.dram(name, shape, dtype, "ExternalInput")

        def inter(name, shape, dtype, producer_stage, consumer_stages):
            if producer_stage in stages:
                if all(s in stages for s in consumer_stages):
                    b = k.dram(name, shape, dtype, "Internal")
                else:
                    b = k.dram(name, shape, dtype, "ExternalOutput")
                    final_bufs.append(b)
                return b
            if any(s in stages for s in consumer_stages):
                return ext_in(name, shape, dtype)
            return None

        din = {"ident": ext_in("ident", [128, 128]), "sel": ext_in("sel", [8, 1024]),
               "flags": ext_in("flags", [128, 8])}
        c = setup_consts(k, cfg, din)
        xTin = ext_in("xTin", [D, HALO + T]) if ("A0" in stages or "B0" in stages) else None
        t = {}
        for l in (0, 1):
            t[f"yT{l}"] = inter(f"yT{l}", [CW, T], BF16, f"A{l}", [f"B{l}"])
            t[f"YL{l}"] = inter(f"YL{l}", [CW, T], F32, f"A{l}", [f"B{l}"])
            t[f"PG{l}"] = inter(f"PG{l}", [CW, T], F32, f"A{l}", [f"B{l}"])
            if fused:
                t[f"cst{l}"] = k.dram(f"cst{l}", [256, NJ], F32, "Internal")
                t[f"carr{l}"] = k.dram(f"carr{l}", [1024, NJ], F32, "Internal")
            else:
                t[f"cst{l}"] = inter(f"cst{l}", [256, NJ], F32, f"A{l}", ["host"])
                t[f"carr{l}"] = ext_in(f"carr{l}", [1024, NJ]) if f"B{l}" in stages else None
        t["xT1"] = inter("xT1", [D, T], F32, "B0", ["A1", "B1"])
        if fused:
            t["xlast"] = k.dram("xlast", [D, HALO], F32, "Internal")
            t["xh1"] = k.dram("xhall", [4 * D, HALO], F32, "Internal")
        else:
            t["xlast"] = inter("xlast", [D, HALO], F32, "B0", ["host"])
            t["xh1"] = ext_in("xh1", [D, HALO]) if "A1" in stages else None
        if "B1" in stages:
            outT = k.dram("outT", [D, T], F32, "ExternalOutput")
            final_bufs.append(outT)

        def layer_w(l, names):
            return {n: ext_in(f"{n}{l}", shp, F32) for n, shp in names}

        for sname in stages:
            l = int(sname[1])
            k.barrier()
            if sname[0] == "A":
                dw = layer_w(l, [("vecs", [128, cfg["NV"]]), ("w_in", [D, cfg["EIN"]]),
                                 ("wa", [NJ, 128, 128]), ("wx", [NJ, 128, 128])])
                if l == 0:
                    stage_A(k, cfg, c, dw, xTin, HALO, xTin, 0, t["yT0"], t["YL0"], t["PG0"], t["cst0"])
                else:
                    stage_A(k, cfg, c, dw, t["xT1"], 0, t["xh1"], 0, t["yT1"], t["YL1"], t["PG1"], t["cst1"],
                            halo_gathered=fused)
                if fused:
                    k.collective(t[f"carr{l}"], t[f"cst{l}"], groups)
            else:
                names = [("vecsB", [128, cfg["NV"]]), ("w_out", [D, D])]
                if l == 0:
                    names += [("wg", [D, cfg["F"]]), ("wu", [D, cfg["F"]]), ("wd", [cfg["F"], D])]
                else:
                    names += [("w_router", [D, 8]), ("wg", [cfg["NE"], D, cfg["FE"]]),
                              ("wu", [cfg["NE"], D, cfg["FE"]]), ("wd", [cfg["NE"], cfg["FE"], D])]
                dw = layer_w(l, names)
                dw["vecs"] = dw["vecsB"]
                if l == 0:
                    stage_B(k, cfg, c, dw, xTin, HALO, t["yT0"], t["YL0"], t["PG0"], t["carr0"],
                            t["xT1"], t["xlast"], None, False, False)
                    if fused:
                        k.collective(t["xh1"], t["xlast"], groups)
                else:
                    stage_B(k, cfg, c, dw, t["xT1"], 0, t["yT1"], t["YL1"], t["PG1"], t["carr1"],
                            None, None, outT, True, True)
        k.emit(final_bufs)
        build_program.last_peak = k.peak
    return nc


def vec2d(v, n):
    return np.ascontiguousarray(np.asarray(v, np.float32).reshape(n, 128).T)


def pack_vecs(cfg, inp, l):
    KC, NJ = cfg["KC"], cfg["NJ"]
    cw = np.asarray(inp["conv_w"][l], np.float32)
    lw = np.asarray(inp["lru_conv_w"][l], np.float32)
    parts = [vec2d(inp["mix_norm"][l], KC), vec2d(inp["ffn_norm"][l], KC), vec2d(inp["final_norm"], KC),
             vec2d(inp["conv_b"][l], NJ), vec2d(inp["conv_ln_g"][l], NJ), vec2d(inp["conv_ln_b"][l], NJ),
             vec2d(inp["lru_conv_b"][l], NJ), vec2d(inp["lru_ba"][l], NJ), vec2d(inp["lru_bx"][l], NJ),
             vec2d(inp["lru_lambda"][l], NJ),
             cw.reshape(CK, NJ, 128).transpose(2, 1, 0).reshape(128, NJ * CK),
             lw.reshape(LK, NJ, 128).transpose(2, 1, 0).reshape(128, NJ * LK)]
    return np.ascontiguousarray(np.concatenate(parts, axis=1).astype(np.float32))


def host_consts():
    ident = np.eye(128, dtype=np.float32)
    sel = np.zeros((8, 8, 128), np.float32)
    for e in range(8):
        sel[e, e, :] = 1.0
    return ident, sel.reshape(8, 1024)


def core_flags(s):
    f = np.zeros((128, 8), np.float32)
    for j in range(3):
        f[:, j] = 1.0 if j < s else 0.0
    f[:, 3] = 1.0 if s == 0 else 0.0
    f[:, 4] = 0.0 if s == 0 else 1.0
    for j in range(3):
        f[:, 5 + j] = 1.0 if j == s - 1 else 0.0
    return f


_PROG_CACHE = {}


def get_prog(cfg, stages):
    key = (cfg["D"], cfg["T"], cfg["F"], cfg["FE"], cfg["W"], tuple(stages))
    if key not in _PROG_CACHE:
        _PROG_CACHE[key] = build_program(cfg, list(stages))
    return _PROG_CACHE[key]


def run_unfused(cfg, inp, n_cores=8):
    D, T, NJ = cfg["D"], cfg["T"], cfg["NJ"]
    x = np.asarray(inp["x"], np.float32)
    B, S, _ = x.shape
    nseg = S // T
    assert B * nseg == n_cores and nseg == 4
    ident, sel = host_consts()
    common = []
    for cidx in range(n_cores):
        b, s = divmod(cidx, nseg)
        xT = np.zeros((D, HALO + T), np.float32)
        lo = s * T - HALO
        if s == 0:
            xT[:, HALO:] = x[b, 0:T, :].T
        else:
            xT[:, :] = x[b, lo:lo + HALO + T, :].T
        common.append({"ident": ident, "sel": sel, "flags": core_flags(s), "xTin": xT})
    vecs = [pack_vecs(cfg, inp, l) for l in (0, 1)]
    f32 = lambda a: np.ascontiguousarray(np.asarray(a, np.float32))

    def wA(l):
        return {f"vecs{l}": vecs[l], f"w_in{l}": f32(inp["w_in"][l]), f"wa{l}": f32(inp["lru_wa"][l]),
                f"wx{l}": f32(inp["lru_wx"][l])}

    def wB(l):
        d = {f"vecsB{l}": vecs[l], f"w_out{l}": f32(inp["w_out"][l])}
        if l == 0:
            d.update({"wg0": f32(inp["dense_wg"][0]), "wu0": f32(inp["dense_wu"][0]), "wd0": f32(inp["dense_wd"][0])})
        else:
            d.update({"w_router1": f32(inp["w_router"][0]), "wg1": f32(inp["moe_wg"][0]),
                      "wu1": f32(inp["moe_wu"][0]), "wd1": f32(inp["moe_wd"][0])})
        return d

    def launch(stage, maps):
        nc = get_prog(cfg, [stage])
        res = run_bass_kernel_spmd(nc, maps, core_ids=list(range(n_cores)))
        return res.results

    def gather_carry(res, l):
        out = []
        for cidx in range(n_cores):
            b = cidx // nseg
            out.append(np.ascontiguousarray(np.concatenate([res[b * nseg + j][f"cst{l}"] for j in range(nseg)], 0)))
        return out

    keepA = ("ident", "sel", "flags")
    rA0 = launch("A0", [dict(common[i], **wA(0)) for i in range(n_cores)])
    carr0 = gather_carry(rA0, 0)
    mB0 = [dict(common[i], **wB(0), yT0=rA0[i]["yT0"], YL0=rA0[i]["YL0"], PG0=rA0[i]["PG0"], carr0=carr0[i])
           for i in range(n_cores)]
    rB0 = launch("B0", mB0)
    mA1 = []
    for i in range(n_cores):
        s = i % nseg
        xh = rB0[i - 1]["xlast"] if s > 0 else np.zeros((D, HALO), np.float32)
        m = {kk: common[i][kk] for kk in keepA}
        m.update(wA(1))
        m.update(xT1=rB0[i]["xT1"], xh1=np.ascontiguousarray(xh))
        mA1.append(m)
    rA1 = launch("A1", mA1)
    carr1 = gather_carry(rA1, 1)
    mB1 = []
    for i in range(n_cores):
        m = {kk: common[i][kk] for kk in keepA}
        m.update(wB(1))
        m.update(xT1=rB0[i]["xT1"], yT1=rA1[i]["yT1"], YL1=rA1[i]["YL1"], PG1=rA1[i]["PG1"], carr1=carr1[i])
        mB1.append(m)
    rB1 = launch("B1", mB1)
    out = np.empty((B, S, D), np.float32)
    for i in range(n_cores):
        b, s = divmod(i, nseg)
        out[b, s * T:(s + 1) * T, :] = rB1[i]["outT"].T
    return out


def run_fused(cfg, inp, n_cores=8):
    D, T = cfg["D"], cfg["T"]
    x = np.asarray(inp["x"], np.float32)
    B, S, _ = x.shape
    nseg = S // T
    assert B * nseg == n_cores and nseg == 4
    ident, sel = host_consts()
    f32 = lambda a: np.ascontiguousarray(np.asarray(a, np.float32))
    vecs = [pack_vecs(cfg, inp, l) for l in (0, 1)]
    shared = {"ident": ident, "sel": sel}
    for l in (0, 1):
        shared.update({f"vecs{l}": vecs[l], f"vecsB{l}": vecs[l], f"w_in{l}": f32(inp["w_in"][l]),
                       f"wa{l}": f32(inp["lru_wa"][l]), f"wx{l}": f32(inp["lru_wx"][l]),
                       f"w_out{l}": f32(inp["w_out"][l])})
    shared.update({"wg0": f32(inp["dense_wg"][0]), "wu0": f32(inp["dense_wu"][0]), "wd0": f32(inp["dense_wd"][0]),
                   "w_router1": f32(inp["w_router"][0]), "wg1": f32(inp["moe_wg"][0]),
                   "wu1": f32(inp["moe_wu"][0]), "wd1": f32(inp["moe_wd"][0])})
    maps = []
    for cidx in range(n_cores):
        b, s = divmod(cidx, nseg)
        xT = np.zeros((D, HALO + T), np.float32)
        if s == 0:
            xT[:, HALO:] = x[b, 0:T, :].T
        else:
            xT[:, :] = x[b, s * T - HALO:(s + 1) * T, :].T
        maps.append(dict(shared, flags=core_flags(s), xTin=xT))
    nc = get_prog(cfg, ["A0", "B0", "A1", "B1"])
    res = run_bass_kernel_spmd(nc, maps, core_ids=list(range(n_cores))).results
    out = np.empty((B, S, D), np.float32)
    for i in range(n_cores):
        b, s = divmod(i, nseg)
        out[b, s * T:(s + 1) * T, :] = res[i]["outT"].T
    return out


FULL_CFG = make_cfg()


def kernel(**inputs):
    return run_fused(FULL_CFG, inputs)
```

```python
import numpy as np
from contextlib import ExitStack
import ml_dtypes
import concourse.bass as bass
import concourse.mybir as mybir
from concourse.bass_utils import run_bass_kernel_spmd

F32 = mybir.dt.float32
BF16 = mybir.dt.bfloat16
AF = mybir.ActivationFunctionType
ALU = mybir.AluOpType
EPS = 1e-6
HALO = 32
CK = 31
LK = 4
ND = 8


def make_cfg(D=2048, T=2048, F=6144, FE=6144, NE=8, W=512):
    c = dict(D=D, KC=D // 128, CW=D // 2, NJ=D // 256, EIN=2 * D, T=T, W=W, F=F, FE=FE, NE=NE,
             TH=T // 2)
    c["PP"] = min(2, c["NJ"])
    c["XG"] = min(4, c["KC"])
    c["DGW"] = min(512, D)
    KC, NJ = c["KC"], c["NJ"]
    off = {}
    p = 0
    for name, n in (("mixn", KC), ("ffnn", KC), ("finn", KC), ("cb", NJ), ("lng", NJ), ("lnb", NJ),
                    ("lcb", NJ), ("ba", NJ), ("bx", NJ), ("lam", NJ), ("cw", NJ * CK), ("lw", NJ * LK)):
        off[name] = p
        p += n
    c["off"] = off
    c["NV"] = p
    return c


class Buf:
    _n = 0

    def __init__(self, ap, kind, name):
        self.ap, self.kind, self.name = ap, kind, name
        self.last_w = None
        self.reads = {}
        self.dcnt = 0
        Buf._n += 1
        self.id = Buf._n

    def __getitem__(self, idx):
        return V(self, self.ap[idx])

    def v(self):
        return V(self, self.ap)


class V:
    def __init__(self, buf, ap):
        self.buf, self.ap = buf, ap

    def __getitem__(self, idx):
        return V(self.buf, self.ap[idx])

    def re(self, s, **kw):
        return V(self.buf, self.ap.rearrange(s, **kw))


class Rot:
    def __init__(self, bufs):
        self.bufs, self.i = bufs, 0

    def next(self):
        b = self.bufs[self.i % len(self.bufs)]
        self.i += 1
        return b


COMPUTE = ("pe", "act", "dve", "pool")
STREAMS = ("pe", "act", "dve", "pool", "sp")


class K:
    def __init__(self, nc, st, arena_words):
        self.nc = nc
        self.arena = st.enter_context(nc.sbuf_tensor("arena", [128, arena_words], F32))[:]
        self.arena_words = arena_words
        self.ptr = 0
        self.ops = []
        self.cnt = {e: 0 for e in COMPUTE}
        self.waited = {e: {} for e in STREAMS}
        self.slot_cnt = []
        self.free_slots = []
        self.live_dbufs = []
        self.cbufs = {}
        self.banks = [Buf(st.enter_context(nc.psum_tensor(f"ps{i}", [128, 512], F32))[:], "psum", f"ps{i}")
                      for i in range(8)]
        self.rot = 0
        self.peak = 0

    def alloc(self, name, shape, dtype):
        n = 1
        for s in shape[1:]:
            n *= s
        words = (n + 1) // 2 if dtype == BF16 else n
        words = (words + 7) // 8 * 8
        off = self.ptr
        self.ptr += words
        self.peak = max(self.peak, self.ptr)
        assert self.ptr <= self.arena_words, f"SBUF arena overflow at {name}: {self.ptr} > {self.arena_words}"
        ap = self.arena[:, off:off + words]
        if dtype == BF16:
            ap = ap.bitcast(BF16)
        ap = ap[:, :n]
        if shape[0] < 128:
            ap = ap[0:shape[0]]
        if len(shape) == 3:
            ap = ap.rearrange("p (a b) -> p a b", a=shape[1])
        elif len(shape) == 4:
            ap = ap.rearrange("p (a b c) -> p a b c", a=shape[1], b=shape[2])
        return Buf(ap, "sbuf", name)

    def bank(self):
        b = self.banks[self.rot % 6]
        self.rot += 1
        return b

    def dram(self, name, shape, dtype, kind):
        return Buf(self.nc.dram_tensor(name, list(shape), dtype, kind=kind).ap(), "dram", name)

    def _collect(self, eng, reads, writes):
        need = {}

        def add(tok):
            if tok is not None:
                need[tok[0]] = max(need.get(tok[0], 0), tok[1])

        for b in reads:
            add(b.last_w)
        for b in writes:
            if b.kind != "dram":
                add(b.last_w)
            for kk, v in b.reads.items():
                add((kk, v))
        out = []
        for kk, v in need.items():
            if kk == eng and eng == "pe":
                continue
            if self.waited[eng].get(kk, 0) >= v:
                continue
            self.waited[eng][kk] = v
            out.append((kk, v))
        return out

    def op(self, eng, fn, reads, writes):
        reads = [r.buf for r in reads if isinstance(r, V)]
        writes = [w.buf for w in writes]
        waits = self._collect(eng, reads, writes)
        n = self.cnt[eng] + 1
        self.cnt[eng] = n
        self.ops.append((eng, fn, waits, (eng, 1)))
        for b in reads:
            b.reads[eng] = max(b.reads.get(eng, 0), n)
        for b in writes:
            b.last_w = (eng, n)
            b.reads = {}

    def dma(self, q, out, in_):
        waits = self._collect(q, [in_.buf], [out.buf])
        dst = out.buf
        if getattr(dst, "dslot", None) is None:
            if self.free_slots:
                dst.dslot = self.free_slots.pop()
            else:
                dst.dslot = len(self.slot_cnt)
                self.slot_cnt.append(0)
            self.live_dbufs.append(dst)
        slot = dst.dslot
        key = ("d", slot)
        self.slot_cnt[slot] += 16
        cntv = self.slot_cnt[slot]
        o_ap, i_ap = out.ap, in_.ap
        self.ops.append((q, lambda e: e.dma_start(out=o_ap, in_=i_ap), waits, (key, 16)))
        in_.buf.reads[key] = max(in_.buf.reads.get(key, 0), cntv)
        dst.last_w = (key, cntv)
        dst.reads = {}

    def collective(self, out_buf, in_buf, groups):
        waits = self._collect("pool", [in_buf], [out_buf])
        key = ("c", out_buf.id)
        self.cbufs[out_buf.id] = out_buf
        o_ap, i_ap = out_buf.ap, in_buf.ap
        self.ops.append(("pool", lambda e: e.collective_compute("AllGather", ALU.bypass, replica_groups=groups,
                                                                  ins=[i_ap], outs=[o_ap]), waits, (key, None)))
        in_buf.reads[key] = 1
        out_buf.last_w = (key, 1)
        out_buf.reads = {}

    def barrier(self):
        allw = [(e, self.cnt[e]) for e in COMPUTE if self.cnt[e] > 0]
        allw += [(("d", i), v) for i, v in enumerate(self.slot_cnt) if v > 0]
        allw += [(("c", b.id), 1) for b in self.cbufs.values()]
        for b in self.live_dbufs:
            self.free_slots.append(b.dslot)
            b.dslot = None
        self.live_dbufs = []
        for e in STREAMS:
            waits = []
            for kk, v in allw:
                if self.waited[e].get(kk, 0) < v:
                    self.waited[e][kk] = v
                    waits.append((kk, v))
            self.ops.append((e, None, waits, None))

    @staticmethod
    def _a(x):
        return x.ap if isinstance(x, V) else x

    def mm(self, out, lhsT, rhs, start, stop):
        o, l, r = out.ap, lhsT.ap, rhs.ap
        self.op("pe", lambda e: e.matmul(o, l, r, start=start, stop=stop), [lhsT, rhs], [out])

    def act(self, out, in_, func, bias=None, scale=None):
        o, i = out.ap, in_.ap
        kw = {}
        if bias is not None:
            kw["bias"] = self._a(bias)
        if scale is not None:
            kw["scale"] = self._a(scale)
        self.op("act", lambda e: e.activation(out=o, in_=i, func=func, **kw), [in_, bias, scale], [out])

    def tt(self, out, in0, in1, op, eng="dve"):
        o, a, b = out.ap, in0.ap, in1.ap
        self.op(eng, lambda e: e.tensor_tensor(out=o, in0=a, in1=b, op=op), [in0, in1], [out])

    def stt(self, out, in0, scalar, in1, op0, op1):
        o, a, s, b = out.ap, in0.ap, self._a(scalar), in1.ap
        self.op("dve", lambda e: e.scalar_tensor_tensor(out=o, in0=a, scalar=s, in1=b, op0=op0, op1=op1),
                [in0, scalar, in1], [out])

    def ts(self, out, in0, s1, s2, op0, op1=None, eng="dve"):
        o, a, x1, x2 = out.ap, in0.ap, self._a(s1), self._a(s2)
        if op1 is None:
            fn = lambda e: e.tensor_scalar(out=o, in0=a, scalar1=x1, scalar2=None, op0=op0)
        else:
            fn = lambda e: e.tensor_scalar(out=o, in0=a, scalar1=x1, scalar2=x2, op0=op0, op1=op1)
        self.op(eng, fn, [in0, s1, s2], [out])

    def scan(self, out, d0, d1, initial, op0, op1):
        o, a, b, i = out.ap, d0.ap, d1.ap, self._a(initial)
        self.op("dve", lambda e: e.tensor_tensor_scan(out=o, data0=a, data1=b, initial=i, op0=op0, op1=op1),
                [d0, d1, initial], [out])

    def copy(self, out, in_, eng="dve"):
        o, i = out.ap, in_.ap
        self.op(eng, lambda e: e.tensor_copy(out=o, in_=i), [in_], [out])

    def recip(self, out, in_):
        o, i = out.ap, in_.ap
        self.op("dve", lambda e: e.reciprocal(out=o, in_=i), [in_], [out])

    def memset(self, v, val, eng="dve"):
        a = v.ap
        self.op(eng, lambda e: e.memset(a, val), [], [v])

    def vmax(self, out, in_):
        o, i = out.ap, in_.ap
        self.op("dve", lambda e: e.max(out=o, in_=i), [in_], [out])

    def emit(self, final_bufs):
        nc = self.nc
        waits = [b.last_w for b in final_bufs if b.last_w is not None]
        self.ops.append(("sp", None, waits, None))
        with ExitStack() as st:
            sems = {}
            for e in COMPUTE:
                sems[e] = st.enter_context(nc.semaphore("s_" + e))
            print("free sems", nc.free_len(), "dma slots", len(self.slot_cnt))
            for i in range(len(self.slot_cnt)):
                sems[("d", i)] = st.enter_context(nc.semaphore(f"d{i}"))
            for b in self.cbufs.values():
                sems[("c", b.id)] = st.enter_context(nc.semaphore(f"c{b.id}"))
            block = st.enter_context(nc.Block())
            ops = self.ops

            def mk(name):
                def body(eng):
                    for (e, fn, waits, inc) in ops:
                        if e != name:
                            continue
                        for (kk, v) in waits:
                            eng.wait_ge(sems[kk], v)
                        if fn is None:
                            continue
                        ins = fn(eng)
                        if inc[1] is None:
                            ins.then_inc(sems[inc[0]])
                        else:
                            ins.then_inc(sems[inc[0]], inc[1])
                return body

            block.tensor(mk("pe"))
            block.scalar(mk("act"))
            block.vector(mk("dve"))
            block.gpsimd(mk("pool"))
            block.sync(mk("sp"))


def setup_consts(k, cfg, din):
    c = {}
    c["ones"] = k.alloc("ones", [128, 128], BF16)
    k.memset(c["ones"].v(), 1.0)
    c["zeros"] = k.alloc("zeros", [128, 512], F32)
    k.memset(c["zeros"].v(), 0.0)
    c["ident"] = k.alloc("ident", [128, 128], F32)
    k.dma("sp", c["ident"].v(), din["ident"].v())
    c["sel"] = k.alloc("sel", [8, 8 * 128], F32)
    k.dma("sp", c["sel"].v(), din["sel"].v())
    c["flags"] = k.alloc("flags", [128, 8], F32)
    k.dma("sp", c["flags"].v(), din["flags"].v())
    k.persist = k.ptr
    return c


def rows(buf, r0, nr, c0, nc_):
    return V(buf, buf.ap[r0:r0 + nr, c0:c0 + nc_].rearrange("(k p) t -> p k t", p=128))


def stage_A(k, cfg, c, dw, xmain, xoff, xhalo, xhoff, yT, YL, PG, cst, halo_gathered=False):
    D, KC, NJ, T, W, CW = cfg["D"], cfg["KC"], cfg["NJ"], cfg["T"], cfg["W"], cfg["CW"]
    PP, XG, off = cfg["PP"], cfg["XG"], cfg["off"]
    k.ptr = k.persist
    vec = k.alloc("vec", [128, cfg["NV"]], F32)
    k.dma("sp", vec.v(), dw["vecs"].v())
    wa = k.alloc("wa", [128, NJ, 128], BF16)
    wx = k.alloc("wx", [128, NJ, 128], BF16)
    k.dma("pool", wa.v(), dw["wa"].v().re("h i j -> i h j"))
    k.dma("pool", wx.v(), dw["wx"].v().re("h i j -> i h j"))

    def vcol(name, i, n=1):
        return vec[:, off[name] + i: off[name] + i + n]

    sm = k.alloc("sm", [128, 10 * NJ], F32)
    s_ = [sm[:, i * NJ:(i + 1) * NJ] for i in range(10)]
    e_, ln_, t1, msk, t2, spv, nsp, nsp2 = s_[0], s_[1], s_[2], s_[3], s_[4], s_[5], s_[6], s_[7]
    lam = vcol("lam", 0, NJ)
    k.act(e_, lam, AF.Exp, scale=-1.0)
    k.act(ln_, e_, AF.Ln, bias=1.0)
    k.ts(t1, e_, -1.0 / 3.0, 0.5, ALU.mult, ALU.add)
    k.tt(t1, t1, e_, ALU.mult)
    k.ts(t1, t1, -1.0, 1.0, ALU.mult, ALU.add)
    k.tt(t1, t1, e_, ALU.mult)
    k.ts(msk, e_, 0.05, None, ALU.is_lt)
    k.tt(t2, t1, ln_, ALU.subtract)
    k.tt(t2, t2, msk, ALU.mult)
    k.tt(spv, t2, ln_, ALU.add)
    k.ts(nsp, spv, -8.0, None, ALU.mult)
    k.ts(nsp2, spv, -16.0, None, ALU.mult)

    hst = k.alloc("hst", [128, NJ], F32)
    pst = k.alloc("pst", [128, NJ], F32)
    k.memset(hst.v(), 0.0)
    k.memset(pst.v(), 1.0)
    chalo = [k.alloc(f"chalo{j}", [128, HALO], BF16) for j in range(NJ)]
    rhalo = [k.alloc(f"rhalo{j}", [128, HALO], F32) for j in range(NJ)]

    xt = [k.alloc(f"xt{g}", [128, XG, W], F32) for g in range(KC // XG)]
    if halo_gathered:
        xhp = [k.alloc(f"xhp{i}", [128, XG, HALO], F32) for i in range(3)]
    hT = [k.alloc(f"hT{i}", [128, W], BF16) for i in range(KC)]
    xth = [k.alloc(f"xth{g}", [128, XG, HALO], F32) for g in range(KC // XG)]
    hTh = [k.alloc(f"hTh{i}", [128, HALO], BF16) for i in range(KC)]
    sqp = Rot([k.alloc(f"sq{i}", [128, W], BF16) for i in range(2)])
    bfp = Rot([k.alloc(f"bfp{i}", [128, W], BF16) for i in range(4)])
    wtA = Rot([k.alloc(f"wtA{i}", [128, KC, PP * 128], BF16) for i in range(2)])
    wtB = Rot([k.alloc(f"wtB{i}", [128, KC, PP * 128], BF16) for i in range(2)])
    cwp = Rot([k.alloc(f"cw{i}", [128, HALO + W], BF16) for i in range(2)])
    dgp = Rot([k.alloc(f"dg{i}", [128, CK, 128], BF16) for i in range(2)])
    identb = k.alloc("identb", [128, 128], BF16)
    k.copy(identb.v(), c["ident"].v())
    rwp = Rot([k.alloc(f"rw{i}", [128, HALO + W], F32) for i in range(2)])
    ccb = [k.alloc(f"cc{j}", [128, W], F32) for j in range(NJ)]
    fp = Rot([k.alloc(f"fp{i}", [128, W], F32) for i in range(20)])
    rstd = k.alloc("rstd", [128, W], F32)
    mu = k.alloc("mu", [128, W], F32)
    rs2 = k.alloc("rs2", [128, W], F32)
    ycv = Rot([k.alloc(f"ycv{i}", [128, NJ, W], BF16) for i in range(2)])
    ps_ss, ps_s1, ps_s2 = k.banks[6], k.banks[6], k.banks[7]
    ones = c["ones"].v()
    flags = c["flags"]
    w_in = dw["w_in"]

    def lru_part2(j, ti, gx, rc, ml, a_, G):
        bb = fp.next()
        k.tt(bb[:, :], gx[:, :], rc[:, :], ALU.mult)
        k.tt(bb[:, :], bb[:, :], ml[:, :], ALU.mult)
        hl, Pc = fp.next(), fp.next()
        k.scan(hl[:, :], a_[:, :], bb[:, :], hst[:, j:j + 1], ALU.mult, ALU.add)
        k.copy(hst[:, j:j + 1], hl[:, W - 1:W])
        k.scan(Pc[:, :], a_[:, :], c["zeros"][:, :W], pst[:, j:j + 1], ALU.mult, ALU.add)
        k.copy(pst[:, j:j + 1], Pc[:, W - 1:W])
        k.tt(hl[:, :], hl[:, :], G[:, :], ALU.mult)
        k.tt(Pc[:, :], Pc[:, :], G[:, :], ALU.mult)
        k.dma("sp", YL[j * 128:(j + 1) * 128, ti * W:(ti + 1) * W], hl[:, :])
        k.dma("sp", PG[j * 128:(j + 1) * 128, ti * W:(ti + 1) * W], Pc[:, :])

    def conv_part2(j, dg, cw):
        pcv = k.bank()
        for kk in range(ND, CK):
            k.mm(pcv[:, :W], dg[:, kk, :], cw[:, 2 + kk:2 + kk + W], kk == ND, kk == CK - 1)
        k.copy(chalo[j][:, :], cw[:, W:W + HALO])
        cc = ccb[j]
        k.tt(cc[:, :], pcv[:, :W], cc[:, :], ALU.add)
        b1, b2 = bfp.next(), bfp.next()
        k.act(b1[:, :], cc[:, :], AF.Identity)
        k.act(b2[:, :], cc[:, :], AF.Square)
        k.mm(ps_s1[:, :W], ones, b1[:, :], j == 0, j == NJ - 1)
        k.mm(ps_s2[:, :W], ones, b2[:, :], j == 0, j == NJ - 1)

    pend_c = None
    pend = None
    grps = [[("halo", 0, HALO), ("main", 0, W)]] + [[("main", i, W)] for i in range(1, T // W)]
    for grp in grps:
      for kind, ti, w in grp:
        xt_, hT_ = (xth, hTh) if kind == "halo" else (xt, hT)
        for g in range(KC // XG):
            if kind == "halo" and halo_gathered:
                for jj in range(3):
                    k.dma("sp", xhp[jj].v(), rows(xhalo, jj * D + g * XG * 128, XG * 128, 0, w))
                k.ts(xt_[g][:, :, 0:w], xhp[0].v(), flags[:, 5:6], None, ALU.mult)
                k.stt(xt_[g][:, :, 0:w], xhp[1].v(), flags[:, 6:7], xt_[g][:, :, 0:w], ALU.mult, ALU.add)
                k.stt(xt_[g][:, :, 0:w], xhp[2].v(), flags[:, 7:8], xt_[g][:, :, 0:w], ALU.mult, ALU.add)
                continue
            if kind == "halo":
                src = rows(xhalo, g * XG * 128, XG * 128, xhoff, w)
            else:
                src = rows(xmain, g * XG * 128, XG * 128, xoff + ti * W, w)
            k.dma("sp", xt_[g][:, :, 0:w], src)
        for kc in range(KC):
            sq = sqp.next()
            k.act(sq[:, :w], xt_[kc // XG][:, kc % XG, 0:w], AF.Square)
            k.mm(ps_ss[:, :w], ones, sq[:, :w], kc == 0, kc == KC - 1)
        rt = fp.next()
        k.act(rt[:, :w], ps_ss[:, :w], AF.Sqrt, bias=EPS, scale=1.0 / D)
        k.recip(rstd[:, :w], rt[:, :w])
        for kc in range(KC):
            k.stt(hT_[kc][:, :w], xt_[kc // XG][:, kc % XG, 0:w], vcol("mixn", kc), rstd[:, :w],
                  ALU.mult, ALU.mult)

      for branch in (0, 1):
            for q in range(NJ // PP):
                A, B = wtA.next(), wtB.next()
                ca0 = branch * 2 * CW + q * PP * 128
                cb0 = ca0 + CW
                k.dma("pool", A.v(), rows(w_in, 0, D, ca0, PP * 128))
                k.dma("pool", B.v(), rows(w_in, 0, D, cb0, PP * 128))
                for pr in range(PP):
                    j = q * PP + pr
                    for kind, ti, w in grp:
                        hT_ = hTh if kind == "halo" else hT
                        psa, psb = k.bank(), k.bank()
                        for kc in range(KC):
                            k.mm(psa[:, :w], A[:, kc, pr * 128:(pr + 1) * 128], hT_[kc][:, :w], kc == 0, kc == KC - 1)
                        need_b = not (branch == 1 and kind == "halo")
                        if need_b:
                            for kc in range(KC):
                                k.mm(psb[:, :w], B[:, kc, pr * 128:(pr + 1) * 128], hT_[kc][:, :w], kc == 0,
                                     kc == KC - 1)
                        if branch == 0:
                            sgt = fp.next()
                            k.act(sgt[:, :w], psb[:, :w], AF.Sigmoid)
                            if kind == "halo":
                                k.tt(chalo[j][:, :], psa[:, :w], sgt[:, :w], ALU.mult)
                                continue
                            cw = cwp.next()
                            k.copy(cw[:, 0:HALO], chalo[j][:, :])
                            k.tt(cw[:, HALO:HALO + W], psa[:, :W], sgt[:, :W], ALU.mult)
                            dg = dgp.next()
                            for kk in range(ND, CK):
                                k.ts(dg[:, kk, :], identb[:, :], vcol("cw", j * CK + kk), None, ALU.mult)
                            cc = ccb[j]
                            k.ts(cc[:, :], cw[:, 2:2 + W], vcol("cw", j * CK), vcol("cb", j), ALU.mult, ALU.add)
                            for kk in range(1, ND):
                                k.stt(cc[:, :], cw[:, 2 + kk:2 + kk + W], vcol("cw", j * CK + kk), cc[:, :],
                                      ALU.mult, ALU.add)
                            if pend_c is not None:
                                conv_part2(*pend_c)
                            pend_c = (j, dg, cw)
                        else:
                            if kind == "halo":
                                k.act(rhalo[j][:, :], psa[:, :w], AF.Identity)
                                continue
                            rw = rwp.next()
                            k.copy(rw[:, 0:HALO], rhalo[j][:, :])
                            k.act(rw[:, HALO:HALO + W], psa[:, :W], AF.Identity)
                            G = fp.next()
                            k.act(G[:, :], psb[:, :W], AF.Gelu_apprx_tanh)
                            rc = fp.next()
                            k.ts(rc[:, :], rw[:, HALO - 3:HALO - 3 + W], vcol("lw", j * LK), vcol("lcb", j),
                                 ALU.mult, ALU.add)
                            for kk in range(1, LK):
                                k.stt(rc[:, :], rw[:, HALO - 3 + kk:HALO - 3 + kk + W], vcol("lw", j * LK + kk),
                                      rc[:, :], ALU.mult, ALU.add)
                            k.copy(rhalo[j][:, :], rw[:, W:W + HALO])
                            rcb = bfp.next()
                            k.act(rcb[:, :], rc[:, :], AF.Identity)
                            pga, pgx = k.bank(), k.bank()
                            k.mm(pga[:, :W], wa[:, j, :], rcb[:, :], True, True)
                            k.mm(pgx[:, :W], wx[:, j, :], rcb[:, :], True, True)
                            ga, gx, a_, a2, ml = fp.next(), fp.next(), fp.next(), fp.next(), fp.next()
                            k.act(ga[:, :], pga[:, :W], AF.Sigmoid, bias=vcol("ba", j))
                            k.act(gx[:, :], pgx[:, :W], AF.Sigmoid, bias=vcol("bx", j))
                            k.act(a_[:, :], ga[:, :], AF.Exp, scale=nsp[:, j:j + 1])
                            k.act(a2[:, :], ga[:, :], AF.Exp, scale=nsp2[:, j:j + 1])
                            k.act(ml[:, :], a2[:, :], AF.Sqrt, bias=1.0, scale=-1.0)
                            if ti == 0:
                                k.ts(ml[:, 0:1], ml[:, 0:1], flags[:, 4:5], flags[:, 3:4], ALU.mult, ALU.add)
                            if pend is not None:
                                lru_part2(*pend)
                            pend = (j, ti, gx, rc, ml, a_, G)
            if branch == 1 and pend is not None:
                lru_part2(*pend)
                pend = None
            if branch == 0:
                if pend_c is not None:
                    conv_part2(*pend_c)
                    pend_c = None
                ti = grp[-1][1]
                mu2, var, sd = fp.next(), fp.next(), fp.next()
                k.act(mu[:, :], ps_s1[:, :W], AF.Identity, scale=1.0 / CW)
                k.tt(mu2[:, :], mu[:, :], mu[:, :], ALU.mult)
                k.stt(var[:, :], ps_s2[:, :W], 1.0 / CW, mu2[:, :], ALU.mult, ALU.subtract)
                k.act(sd[:, :], var[:, :], AF.Sqrt, bias=EPS)
                k.recip(rs2[:, :], sd[:, :])
                yc = ycv.next()
                for j in range(NJ):
                    t_ = fp.next()
                    k.tt(t_[:, :], ccb[j][:, :], mu[:, :], ALU.subtract)
                    k.tt(t_[:, :], t_[:, :], rs2[:, :], ALU.mult)
                    k.act(yc[:, j, :], t_[:, :], AF.Silu, bias=vcol("lnb", j), scale=vcol("lng", j))
                k.dma("sp", rows(yT, 0, CW, ti * W, W), yc.v())
    k.dma("sp", cst[0:128, :], pst.v())
    k.dma("sp", cst[128:256, :], hst.v())


def stage_B(k, cfg, c, dw, xres, xroff, yT, YL, PG, carr, xout, xlast, outT, is_moe, is_last):
    D, KC, NJ, T, W, CW, TH = cfg["D"], cfg["KC"], cfg["NJ"], cfg["T"], cfg["W"], cfg["CW"], cfg["TH"]
    off, DGW = cfg["off"], cfg["DGW"]
    NE = cfg["NE"] if is_moe else 1
    F = cfg["FE"] if is_moe else cfg["F"]
    NTH = TH // W
    k.ptr = k.persist
    vec = k.alloc("vec", [128, cfg["NV"]], F32)
    k.dma("sp", vec.v(), dw["vecs"].v())

    def vcol(name, i, n=1):
        return vec[:, off[name] + i: off[name] + i + n]

    flags = c["flags"]
    ones = c["ones"].v()
    ca = k.alloc("ca", [128, 8, NJ], F32)
    k.dma("sp", ca.v(), carr.v().re("(sa p) j -> p sa j", p=128))
    carry = k.alloc("carry", [128, NJ], F32)
    ctmp = k.alloc("ctmp", [128, NJ], F32)
    k.memset(carry.v(), 0.0)
    for s in range(3):
        k.tt(ctmp[:, :], ca[:, 2 * s, :], carry[:, :], ALU.mult)
        k.tt(ctmp[:, :], ctmp[:, :], ca[:, 2 * s + 1, :], ALU.add)
        k.tt(ctmp[:, :], ctmp[:, :], carry[:, :], ALU.subtract)
        k.stt(carry[:, :], ctmp[:, :], flags[:, s:s + 1], carry[:, :], ALU.mult, ALU.add)

    if is_moe:
        wr = k.alloc("wr", [128, KC, 8], F32)
        k.dma("sp", wr.v(), dw["w_router"].v().re("(kc p) e -> p kc e", p=128))
        lgT = k.alloc("lgT", [8, TH], F32)
        gwT = k.alloc("gwT", [8, TH], F32)
        smalp = Rot([k.alloc(f"smal{i}", [128, 64], F32) for i in range(4)])
    h2T = [k.alloc(f"h2T{i}", [128, TH], BF16) for i in range(KC)]
    acc = [k.alloc(f"acc{i}", [128, TH], F32) for i in range(KC)]
    h2d = k.dram("h2d", [D, TH], BF16, "Internal") if is_moe else None
    base = k.ptr
    ps_ss = k.banks[6]

    for hh in range(2):
        k.barrier()
        k.ptr = base
        assert NTH <= 2
        yts = [k.alloc(f"yt{t}", [128, 2 * NJ, W], BF16) for t in range(NTH)]
        ylp = Rot([k.alloc(f"yl{i}", [128, W], F32) for i in range(2)])
        pgp = Rot([k.alloc(f"pg{i}", [128, W], F32) for i in range(2)])
        wop = Rot([k.alloc(f"wo{i}", [128, 2 * NJ, DGW], BF16) for i in range(2)])
        sqp = Rot([k.alloc(f"sqb{i}", [128, W], BF16) for i in range(2)])
        rt = k.alloc("rtb", [128, W], F32)
        rstd = k.alloc("rstdb", [128, W], F32)
        hfp = Rot([k.alloc(f"hf{i}", [128, W], F32) for i in range(2)])
        ps_st = [k.banks[6], k.banks[7]]
        for tt in range(NTH):
            col = hh * TH + tt * W
            lc = tt * W
            yt = yts[tt]
            for d in range(KC):
                k.dma("sp", acc[d][:, lc:lc + W], xres[d * 128:(d + 1) * 128, xroff + col:xroff + col + W])
            k.dma("sp", yt[:, 0:NJ, :], rows(yT, 0, CW, col, W))
            for j in range(NJ):
                yl, pg = ylp.next(), pgp.next()
                k.dma("sp", yl[:, :], YL[j * 128:(j + 1) * 128, col:col + W])
                k.dma("sp", pg[:, :], PG[j * 128:(j + 1) * 128, col:col + W])
                k.stt(yt[:, NJ + j, :], pg[:, :], carry[:, j:j + 1], yl[:, :], ALU.mult, ALU.add)
        for dg in range(D // DGW):
            wo = wop.next()
            k.dma("pool", wo.v(), rows(dw["w_out"], 0, D, dg * DGW, DGW))
            for dc in range(DGW // 128):
                d = dg * (DGW // 128) + dc
                for tt in range(NTH):
                    lc = tt * W
                    ps = k.bank()
                    for e in range(2 * NJ):
                        k.mm(ps[:, :W], wo[:, e, dc * 128:(dc + 1) * 128], yts[tt][:, e, :], e == 0, e == 2 * NJ - 1)
                    k.tt(acc[d][:, lc:lc + W], ps[:, :W], acc[d][:, lc:lc + W], ALU.add)
                    sq = sqp.next()
                    k.act(sq[:, :], acc[d][:, lc:lc + W], AF.Square)
                    k.mm(ps_st[tt][:, :W], ones, sq[:, :], d == 0, d == KC - 1)
        for tt in range(NTH):
            lc = tt * W
            k.act(rt[:, :], ps_st[tt][:, :W], AF.Sqrt, bias=EPS, scale=1.0 / D)
            k.recip(rstd[:, :], rt[:, :])
            for kc in range(KC):
                k.stt(h2T[kc][:, lc:lc + W], acc[kc][:, lc:lc + W], vcol("ffnn", kc), rstd[:, :],
                      ALU.mult, ALU.mult)
            if is_moe:
                pl = k.bank()
                for kc in range(KC):
                    hf = hfp.next()
                    k.stt(hf[:, :], acc[kc][:, lc:lc + W], vcol("ffnn", kc), rstd[:, :], ALU.mult, ALU.mult)
                    k.mm(pl[0:8, :W], wr[:, kc, :], hf[:, :], kc == 0, kc == KC - 1)
                k.act(lgT[0:8, lc:lc + W], pl[0:8, :W], AF.Identity)
        if is_moe:
            for kc in range(KC):
                k.dma("sp", h2d[kc * 128:(kc + 1) * 128, :], h2T[kc][:, :])
        if is_moe:
            for blk in range(TH // 128):
                sl = slice(blk * 128, (blk + 1) * 128)
                smal = smalp.next()
                lg, m8, msk, nl1 = smal[:, 0:8], smal[:, 8:16], smal[:, 16:24], smal[:, 24:25]
                ex, e2, den, rden, gw = smal[:, 32:40], smal[:, 25:26], smal[:, 26:27], smal[:, 27:28], smal[:, 40:48]
                pl = k.bank()
                k.mm(pl[:, 0:8], lgT[0:8, sl], c["ident"][0:8, 0:8], True, True)
                k.act(lg, pl[:, 0:8], AF.Identity)
                k.vmax(m8, lg)
                k.ts(msk, lg, m8[:, 1:2], None, ALU.is_ge)
                k.ts(nl1, m8[:, 0:1], -1.0, None, ALU.mult)
                k.act(ex, lg, AF.Exp, bias=nl1)
                k.act(e2, m8[:, 1:2], AF.Exp, bias=nl1)
                k.ts(den, e2, 1.0, None, ALU.add)
                k.recip(rden, den)
                k.stt(gw, ex, rden, msk, ALU.mult, ALU.mult)
                pt = k.bank()
                k.mm(pt[0:8, 0:128], gw, c["ident"][:, :], True, True)
                k.act(gwT[0:8, sl], pt[0:8, 0:128], AF.Identity)

        k.barrier()
        k.ptr = base
        wgp = Rot([k.alloc(f"wg{i}", [128, KC, 256], BF16) for i in range(2)])
        wup = Rot([k.alloc(f"wu{i}", [128, KC, 256], BF16) for i in range(2)])
        wdp = Rot([k.alloc(f"wd{i}", [128, 4, D], BF16) for i in range(2)])
        actT = [k.alloc(f"actT{i}", [128, TH], BF16) for i in range(4)]
        sgp = Rot([k.alloc(f"sg{i}", [128, W], F32) for i in range(2)])
        tmpp = Rot([k.alloc(f"tm{i}", [128, W], F32) for i in range(2)])
        if is_moe:
            gwBp = Rot([k.alloc(f"gwB{i}", [128, TH], F32) for i in range(2)])
        for ex_i in range(NE):
            if is_moe:
                wg_d, wu_d, wd_d = (V(dw[n], dw[n].ap[ex_i]) for n in ("wg", "wu", "wd"))
                gwB = gwBp.next()
                for tt in range(NTH):
                    pb = k.bank()
                    k.mm(pb[:, :W], c["sel"][0:8, ex_i * 128:(ex_i + 1) * 128], gwT[0:8, tt * W:(tt + 1) * W],
                         True, True)
                    k.act(gwB[:, tt * W:(tt + 1) * W], pb[:, :W], AF.Identity)
                for kc in range(KC):
                    if ex_i > 0:
                        k.dma("sp", h2T[kc][:, :], h2d[kc * 128:(kc + 1) * 128, :])
                    k.stt(h2T[kc][:, :], gwB[:, :], 0.0, h2T[kc][:, :], ALU.is_gt, ALU.mult)
            else:
                wg_d, wu_d, wd_d = (dw[n].v() for n in ("wg", "wu", "wd"))
            for g in range(F // 512):
                wd_t = wdp.next()
                k.dma("pool", wd_t.v(), wd_d[g * 512:(g + 1) * 512, :].re("(f p) d -> p f d", p=128))
                for pr in range(2):
                    wg_t, wu_t = wgp.next(), wup.next()
                    f0 = g * 512 + pr * 256
                    k.dma("pool", wg_t.v(), wg_d[:, f0:f0 + 256].re("(kc p) f -> p kc f", p=128))
                    k.dma("pool", wu_t.v(), wu_d[:, f0:f0 + 256].re("(kc p) f -> p kc f", p=128))
                    for fc2 in range(2):
                        fc = pr * 2 + fc2
                        for tt in range(NTH):
                            cs = slice(tt * W, (tt + 1) * W)
                            pg_, pu_ = k.bank(), k.bank()
                            for kc in range(KC):
                                k.mm(pg_[:, :W], wg_t[:, kc, fc2 * 128:(fc2 + 1) * 128], h2T[kc][:, cs], kc == 0,
                                     kc == KC - 1)
                            for kc in range(KC):
                                k.mm(pu_[:, :W], wu_t[:, kc, fc2 * 128:(fc2 + 1) * 128], h2T[kc][:, cs], kc == 0,
                                     kc == KC - 1)
                            sg = sgp.next()
                            k.act(sg[:, :], pg_[:, :W], AF.Silu)
                            if is_moe:
                                tm = tmpp.next()
                                k.tt(tm[:, :], pu_[:, :W], sg[:, :], ALU.mult)
                                k.tt(actT[fc][:, cs], tm[:, :], gwB[:, cs], ALU.mult)
                            else:
                                k.tt(actT[fc][:, cs], pu_[:, :W], sg[:, :], ALU.mult)
                for d in range(KC):
                    for tt in range(NTH):
                        cs = slice(tt * W, (tt + 1) * W)
                        pd = k.bank()
                        for fc in range(4):
                            k.mm(pd[:, :W], wd_t[:, fc, d * 128:(d + 1) * 128], actT[fc][:, cs], fc == 0, fc == 3)
                        k.tt(acc[d][:, cs], pd[:, :W], acc[d][:, cs], ALU.add)
        if not is_last:
            for d in range(KC):
                k.dma("sp", xout[d * 128:(d + 1) * 128, hh * TH:(hh + 1) * TH], acc[d][:, :])
                if hh == 1:
                    k.dma("sp", xlast[d * 128:(d + 1) * 128, :], acc[d][:, TH - HALO:TH])
        else:
            k.barrier()
            k.ptr = base
            sqf = Rot([k.alloc(f"sqf{i}", [128, W], BF16) for i in range(2)])
            otp = Rot([k.alloc(f"ot{i}", [128, W], F32) for i in range(2)])
            rtf = k.alloc("rtf", [128, W], F32)
            rsf = k.alloc("rsf", [128, W], F32)
            for tt in range(NTH):
                cs = slice(tt * W, (tt + 1) * W)
                for d in range(KC):
                    sq = sqf.next()
                    k.act(sq[:, :], acc[d][:, cs], AF.Square)
                    k.mm(ps_ss[:, :W], ones, sq[:, :], d == 0, d == KC - 1)
                k.act(rtf[:, :], ps_ss[:, :W], AF.Sqrt, bias=EPS, scale=1.0 / D)
                k.recip(rsf[:, :], rtf[:, :])
                for d in range(KC):
                    ot = otp.next()
                    k.stt(ot[:, :], acc[d][:, cs], vcol("finn", d), rsf[:, :], ALU.mult, ALU.mult)
                    k.dma("sp", outT[d * 128:(d + 1) * 128, hh * TH + tt * W: hh * TH + (tt + 1) * W], ot[:, :])


ARENA_WORDS = 52800


def build_program(cfg, stages):
    nc = bass.Bass("TRN2", target_bir_lowering=False)
    D, KC, NJ, T, CW = cfg["D"], cfg["KC"], cfg["NJ"], cfg["T"], cfg["CW"]
    fused = len(stages) == 4
    groups = [[0, 1, 2, 3], [4, 5, 6, 7]]
    final_bufs = []
    with ExitStack() as st:
        k = K(nc, st, ARENA_WORDS)

        def ext_in(name, shape, dtype=F32):
            return k.dram(name, shape, dtype, "ExternalInput")

        def inter(name, shape, dtype, producer_stage, consumer_stages):
            if producer_stage in stages:
                if all(s in stages for s in consumer_stages):
                    b = k.dram(name, shape, dtype, "Internal")
                else:
                    b = k.dram(name, shape, dtype, "ExternalOutput")
                    final_bufs.append(b)
                return b
            if any(s in stages for s in consumer_stages):
                return ext_in(name, shape, dtype)
            return None

        din = {"ident": ext_in("ident", [128, 128]), "sel": ext_in("sel", [8, 1024]),
               "flags": ext_in("flags", [128, 8])}
        c = setup_consts(k, cfg, din)
        xTin = ext_in("xTin", [D, HALO + T]) if ("A0" in stages or "B0" in stages) else None
        t = {}
        for l in (0, 1):
            t[f"yT{l}"] = inter(f"yT{l}", [CW, T], BF16, f"A{l}", [f"B{l}"])
            t[f"YL{l}"] = inter(f"YL{l}", [CW, T], F32, f"A{l}", [f"B{l}"])
            t[f"PG{l}"] = inter(f"PG{l}", [CW, T], F32, f"A{l}", [f"B{l}"])
            if fused:
                t[f"cst{l}"] = k.dram(f"cst{l}", [256, NJ], F32, "Internal")
                t[f"carr{l}"] = k.dram(f"carr{l}", [1024, NJ], F32, "Internal")
            else:
                t[f"cst{l}"] = inter(f"cst{l}", [256, NJ], F32, f"A{l}", ["host"])
                t[f"carr{l}"] = ext_in(f"carr{l}", [1024, NJ]) if f"B{l}" in stages else None
        t["xT1"] = inter("xT1", [D, T], F32, "B0", ["A1", "B1"])
        if fused:
            t["xlast"] = k.dram("xlast", [D, HALO], F32, "Internal")
            t["xh1"] = k.dram("xhall", [4 * D, HALO], F32, "Internal")
        else:
            t["xlast"] = inter("xlast", [D, HALO], F32, "B0", ["host"])
            t["xh1"] = ext_in("xh1", [D, HALO]) if "A1" in stages else None
        if "B1" in stages:
            outT = k.dram("outT", [D, T], F32, "ExternalOutput")
            final_bufs.append(outT)

        def layer_w(l, names):
            return {n: ext_in(f"{n}{l}", shp, F32) for n, shp in names}

        for sname in stages:
            l = int(sname[1])
            k.barrier()
            if sname[0] == "A":
                dw = layer_w(l, [("vecs", [128, cfg["NV"]]), ("w_in", [D, cfg["EIN"]]),
                                 ("wa", [NJ, 128, 128]), ("wx", [NJ, 128, 128])])
                if l == 0:
                    stage_A(k, cfg, c, dw, xTin, HALO, xTin, 0, t["yT0"], t["YL0"], t["PG0"], t["cst0"])
                else:
                    stage_A(k, cfg, c, dw, t["xT1"], 0, t["xh1"], 0, t["yT1"], t["YL1"], t["PG1"], t["cst1"],
                            halo_gathered=fused)
                if fused:
                    k.collective(t[f"carr{l}"], t[f"cst{l}"], groups)
            else:
                names = [("vecsB", [128, cfg["NV"]]), ("w_out", [D, D])]
                if l == 0:
                    names += [("wg", [D, cfg["F"]]), ("wu", [D, cfg["F"]]), ("wd", [cfg["F"], D])]
                else:
                    names += [("w_router", [D, 8]), ("wg", [cfg["NE"], D, cfg["FE"]]),
                              ("wu", [cfg["NE"], D, cfg["FE"]]), ("wd", [cfg["NE"], cfg["FE"], D])]
                dw = layer_w(l, names)
                dw["vecs"] = dw["vecsB"]
                if l == 0:
                    stage_B(k, cfg, c, dw, xTin, HALO, t["yT0"], t["YL0"], t["PG0"], t["carr0"],
                            t["xT1"], t["xlast"], None, False, False)
                    if fused:
                        k.collective(t["xh1"], t["xlast"], groups)
                else:
                    stage_B(k, cfg, c, dw, t["xT1"], 0, t["yT1"], t["YL1"], t["PG1"], t["carr1"],
                            None, None, outT, True, True)
        k.emit(final_bufs)
        build_program.last_peak = k.peak
    return nc


def vec2d(v, n):
    return np.ascontiguousarray(np.asarray(v, np.float32).reshape(n, 128).T)


def pack_vecs(cfg, inp, l):
    KC, NJ = cfg["KC"], cfg["NJ"]
    cw = np.asarray(inp["conv_w"][l], np.float32)
    lw = np.asarray(inp["lru_conv_w"][l], np.float32)
    parts = [vec2d(inp["mix_norm"][l], KC), vec2d(inp["ffn_norm"][l], KC), vec2d(inp["final_norm"], KC),
             vec2d(inp["conv_b"][l], NJ), vec2d(inp["conv_ln_g"][l], NJ), vec2d(inp["conv_ln_b"][l], NJ),
             vec2d(inp["lru_conv_b"][l], NJ), vec2d(inp["lru_ba"][l], NJ), vec2d(inp["lru_bx"][l], NJ),
             vec2d(inp["lru_lambda"][l], NJ),
             cw.reshape(CK, NJ, 128).transpose(2, 1, 0).reshape(128, NJ * CK),
             lw.reshape(LK, NJ, 128).transpose(2, 1, 0).reshape(128, NJ * LK)]
    return np.ascontiguousarray(np.concatenate(parts, axis=1).astype(np.float32))


def host_consts():
    ident = np.eye(128, dtype=np.float32)
    sel = np.zeros((8, 8, 128), np.float32)
    for e in range(8):
        sel[e, e, :] = 1.0
    return ident, sel.reshape(8, 1024)


def core_flags(s):
    f = np.zeros((128, 8), np.float32)
    for j in range(3):
        f[:, j] = 1.0 if j < s else 0.0
    f[:, 3] = 1.0 if s == 0 else 0.0
    f[:, 4] = 0.0 if s == 0 else 1.0
    for j in range(3):
        f[:, 5 + j] = 1.0 if j == s - 1 else 0.0
    return f


_PROG_CACHE = {}


def get_prog(cfg, stages):
    key = (cfg["D"], cfg["T"], cfg["F"], cfg["FE"], cfg["W"], tuple(stages))
    if key not in _PROG_CACHE:
        _PROG_CACHE[key] = build_program(cfg, list(stages))
    return _PROG_CACHE[key]


def run_unfused(cfg, inp, n_cores=8):
    D, T, NJ = cfg["D"], cfg["T"], cfg["NJ"]
    x = np.asarray(inp["x"], np.float32)
    B, S, _ = x.shape
    nseg = S // T
    assert B * nseg == n_cores and nseg == 4
    ident, sel = host_consts()
    common = []
    for cidx in range(n_cores):
        b, s = divmod(cidx, nseg)
        xT = np.zeros((D, HALO + T), np.float32)
        lo = s * T - HALO
        if s == 0:
            xT[:, HALO:] = x[b, 0:T, :].T
        else:
            xT[:, :] = x[b, lo:lo + HALO + T, :].T
        common.append({"ident": ident, "sel": sel, "flags": core_flags(s), "xTin": xT})
    vecs = [pack_vecs(cfg, inp, l) for l in (0, 1)]
    f32 = lambda a: np.ascontiguousarray(np.asarray(a, np.float32))

    def wA(l):
        return {f"vecs{l}": vecs[l], f"w_in{l}": f32(inp["w_in"][l]), f"wa{l}": f32(inp["lru_wa"][l]),
                f"wx{l}": f32(inp["lru_wx"][l])}

    def wB(l):
        d = {f"vecsB{l}": vecs[l], f"w_out{l}": f32(inp["w_out"][l])}
        if l == 0:
            d.update({"wg0": f32(inp["dense_wg"][0]), "wu0": f32(inp["dense_wu"][0]), "wd0": f32(inp["dense_wd"][0])})
        else:
            d.update({"w_router1": f32(inp["w_router"][0]), "wg1": f32(inp["moe_wg"][0]),
                      "wu1": f32(inp["moe_wu"][0]), "wd1": f32(inp["moe_wd"][0])})
        return d

    def launch(stage, maps):
        nc = get_prog(cfg, [stage])
        res = run_bass_kernel_spmd(nc, maps, core_ids=list(range(n_cores)))
        return res.results

    def gather_carry(res, l):
        out = []
        for cidx in range(n_cores):
            b = cidx // nseg
            out.append(np.ascontiguousarray(np.concatenate([res[b * nseg + j][f"cst{l}"] for j in range(nseg)], 0)))
        return out

    keepA = ("ident", "sel", "flags")
    rA0 = launch("A0", [dict(common[i], **wA(0)) for i in range(n_cores)])
    carr0 = gather_carry(rA0, 0)
    mB0 = [dict(common[i], **wB(0), yT0=rA0[i]["yT0"], YL0=rA0[i]["YL0"], PG0=rA0[i]["PG0"], carr0=carr0[i])
           for i in range(n_cores)]
    rB0 = launch("B0", mB0)
    mA1 = []
    for i in range(n_cores):
        s = i % nseg
        xh = rB0[i - 1]["xlast"] if s > 0 else np.zeros((D, HALO), np.float32)
        m = {kk: common[i][kk] for kk in keepA}
        m.update(wA(1))
        m.update(xT1=rB0[i]["xT1"], xh1=np.ascontiguousarray(xh))
        mA1.append(m)
    rA1 = launch("A1", mA1)
    carr1 = gather_carry(rA1, 1)
    mB1 = []
    for i in range(n_cores):
        m = {kk: common[i][kk] for kk in keepA}
        m.update(wB(1))
        m.update(xT1=rB0[i]["xT1"], yT1=rA1[i]["yT1"], YL1=rA1[i]["YL1"], PG1=rA1[i]["PG1"], carr1=carr1[i])
        mB1.append(m)
    rB1 = launch("B1", mB1)
    out = np.empty((B, S, D), np.float32)
    for i in range(n_cores):
        b, s = divmod(i, nseg)
        out[b, s * T:(s + 1) * T, :] = rB1[i]["outT"].T
    return out


def run_fused(cfg, inp, n_cores=8):
    D, T = cfg["D"], cfg["T"]
    x = np.asarray(inp["x"], np.float32)
    B, S, _ = x.shape
    nseg = S // T
    assert B * nseg == n_cores and nseg == 4
    ident, sel = host_consts()
    f32 = lambda a: np.ascontiguousarray(np.asarray(a, np.float32))
    vecs = [pack_vecs(cfg, inp, l) for l in (0, 1)]
    shared = {"ident": ident, "sel": sel}
    for l in (0, 1):
        shared.update({f"vecs{l}": vecs[l], f"vecsB{l}": vecs[l], f"w_in{l}": f32(inp["w_in"][l]),
                       f"wa{l}": f32(inp["lru_wa"][l]), f"wx{l}": f32(inp["lru_wx"][l]),
                       f"w_out{l}": f32(inp["w_out"][l])})
    shared.update({"wg0": f32(inp["dense_wg"][0]), "wu0": f32(inp["dense_wu"][0]), "wd0": f32(inp["dense_wd"][0]),
                   "w_router1": f32(inp["w_router"][0]), "wg1": f32(inp["moe_wg"][0]),
                   "wu1": f32(inp["moe_wu"][0]), "wd1": f32(inp["moe_wd"][0])})
    maps = []
    for cidx in range(n_cores):
        b, s = divmod(cidx, nseg)
        xT = np.zeros((D, HALO + T), np.float32)
        if s == 0:
            xT[:, HALO:] = x[b, 0:T, :].T
        else:
            xT[:, :] = x[b, s * T - HALO:(s + 1) * T, :].T
        maps.append(dict(shared, flags=core_flags(s), xTin=xT))
    nc = get_prog(cfg, ["A0", "B0", "A1", "B1"])
    res = run_bass_kernel_spmd(nc, maps, core_ids=list(range(n_cores))).results
    out = np.empty((B, S, D), np.float32)
    for i in range(n_cores):
        b, s = divmod(i, nseg)
        out[b, s * T:(s + 1) * T, :] = res[i]["outT"].T
    return out


FULL_CFG = make_cfg()


def kernel(**inputs):
    return run_fused(FULL_CFG, inputs)
```
